# Optimizing a Trainium2 kernel written in Bass

```python
import jax, jax.numpy as jnp
from jax import lax
import numpy as np

D_MODEL = 2048
BATCH = 16
SEQ = 2048
DEPTH = 4

CHUNK = 64
N_EVEN = (DEPTH + 1) // 2
N_ODD = DEPTH // 2

SB_HEAD_DIM = 64
SB_WIDTH = D_MODEL // 2
SB_HEADS = SB_WIDTH // SB_HEAD_DIM
SB_BLOCK = 128

RW_HEAD_DIM = 64
RW_WIDTH = D_MODEL // 2
RW_HEADS = RW_WIDTH // RW_HEAD_DIM
DECAY_LORA = 64
ICL_LORA = 64
GATE_LORA = 160
LNX_EPS = 64e-5
RW_SHIFT_WIDTH = 3 * RW_WIDTH + DECAY_LORA + ICL_LORA + GATE_LORA

EVEN_IN_WIDTH = 3 * SB_WIDTH + RW_SHIFT_WIDTH
EVEN_OUT_WIDTH = SB_WIDTH + RW_WIDTH

SGU_WIDTH = D_MODEL
SGU_GROUPS = 8
SGU_BLOCK = 128
LN_EPS = 1e-5

FFN_HIDDEN = 4 * D_MODEL
RMS_EPS = 1e-6

kernel_name = "hybrid_stickbreak_rwkv7_sgu_stream_encoder"

F32 = jnp.float32


def rms_norm(x, g):
    x32 = x.astype(F32)
    y = x32 * lax.rsqrt(jnp.mean(x32 * x32, axis=-1, keepdims=True) + RMS_EPS)
    return (y * g.astype(F32)).astype(x.dtype)


def token_shift(z, mu):
    prev = jnp.pad(z[:, :-1], ((0, 0), (1, 0), (0, 0)))
    return z + (prev - z) * mu


def stick_breaking_attention(q, k, v):
    T, Dh = q.shape[2], q.shape[3]
    scale = Dh ** -0.5
    outs = []
    for blk in range(T // SB_BLOCK):
        q0 = blk * SB_BLOCK
        kv_len = q0 + SB_BLOCK
        qb = q[:, :, q0:kv_len].astype(F32)
        kb = k[:, :, :kv_len].astype(F32)
        vb = v[:, :, :kv_len].astype(F32)
        z = jnp.einsum('bhtd,bhsd->bhts', qb, kb) * scale
        t_pos = q0 + jnp.arange(SB_BLOCK)
        s_pos = jnp.arange(kv_len)
        visible = s_pos[None, :] < t_pos[:, None]
        log_keep = jnp.where(visible, jax.nn.log_sigmoid(-z), 0.0)
        tail = lax.cumsum(log_keep, axis=3, reverse=True)
        tail_excl = jnp.concatenate([tail[..., 1:], jnp.zeros_like(tail[..., :1])], axis=-1)
        w = jnp.where(visible, jnp.exp(jax.nn.log_sigmoid(z) + tail_excl), 0.0)
        outs.append(jnp.einsum('bhts,bhsd->bhtd', w, vb))
    return jnp.concatenate(outs, axis=2).astype(q.dtype)


def rwkv7_time_mix(zb, mu, w_up, w0, a_up, a0, g_up, k_k, k_a, r_k, lnx_w, lnx_b):
    B, T, _ = zb.shape
    zs = token_shift(zb, mu)
    idx = [RW_WIDTH, 2 * RW_WIDTH, 3 * RW_WIDTH, 3 * RW_WIDTH + DECAY_LORA,
           3 * RW_WIDTH + DECAY_LORA + ICL_LORA]
    r, k, v, xw, xa, xg = jnp.split(zs, idx, axis=-1)

    w_log = -jax.nn.softplus(-(w0 + jnp.tanh(xw) @ w_up).astype(F32)) - 0.5
    decay = jnp.exp(-jnp.exp(w_log))
    a = jax.nn.sigmoid((a0 + xa @ a_up).astype(F32))
    g = jax.nn.sigmoid(xg) @ g_up

    def heads(t):
        return t.reshape(B, T, RW_HEADS, RW_HEAD_DIM)

    r32 = r.astype(F32)
    k32 = k.astype(F32)
    v32 = heads(v.astype(F32))
    kk = heads(k32 * k_k.astype(F32))
    kk = kk / jnp.maximum(jnp.sqrt(jnp.sum(kk * kk, axis=-1, keepdims=True)), 1e-12)
    k_mod = heads(k32 * (1.0 + (a - 1.0) * k_a.astype(F32)))
    r_h = heads(r32)
    a_h = heads(a)
    w_h = heads(decay)

    def step(S, inp):
        r_t, w_t, k_t, v_t, kk_t, a_t = inp
        sa = jnp.einsum('bhvk,bhk->bhv', S, -kk_t)
        S = (S * w_t[:, :, None, :] + sa[..., None] * (kk_t * a_t)[:, :, None, :]
             + v_t[..., None] * k_t[:, :, None, :])
        y = jnp.einsum('bhvk,bhk->bhv', S, r_t)
        return S, y

    xs = tuple(jnp.moveaxis(t, 1, 0) for t in (r_h, w_h, k_mod, v32, kk, a_h))
    S0 = jnp.zeros((B, RW_HEADS, RW_HEAD_DIM, RW_HEAD_DIM), F32)
    _, y = lax.scan(step, S0, xs)
    y = jnp.moveaxis(y, 0, 1)

    mean = jnp.mean(y, axis=-1, keepdims=True)
    var = jnp.mean(jnp.square(y - mean), axis=-1, keepdims=True)
    y = ((y - mean) * lax.rsqrt(var + LNX_EPS)).reshape(B, T, RW_WIDTH)
    y = y * lnx_w.astype(F32) + lnx_b.astype(F32)
    bonus = jnp.sum(r_h * k_mod * r_k.astype(F32), axis=-1, keepdims=True) * v32
    y = (y + bonus.reshape(B, T, RW_WIDTH)) * g.astype(F32)
    return y.astype(zb.dtype)


def even_mixer(h, w_in, mu, w_up, w0, a_up, a0, g_up, k_k, k_a, r_k, lnx_w, lnx_b, w_out):
    B, T, _ = h.shape
    z = h @ w_in
    z_sb, z_rw = z[..., :3 * SB_WIDTH], z[..., 3 * SB_WIDTH:]
    q, k, v = jnp.split(z_sb, 3, axis=-1)

    def sb_heads(t):
        return t.reshape(B, T, SB_HEADS, SB_HEAD_DIM).transpose(0, 2, 1, 3)

    o_sb = stick_breaking_attention(sb_heads(q), sb_heads(k), sb_heads(v))
    o_sb = o_sb.transpose(0, 2, 1, 3).reshape(B, T, SB_WIDTH)
    o_rw = rwkv7_time_mix(z_rw, mu, w_up, w0, a_up, a0, g_up, k_k, k_a, r_k, lnx_w, lnx_b)
    return jnp.concatenate([o_sb.astype(h.dtype), o_rw.astype(h.dtype)], axis=-1) @ w_out


def odd_mixer(h, w_in, ln_g, ln_b, w_s, b_s, w_out):
    B, T, _ = h.shape
    zc = jax.nn.gelu(h @ w_in, approximate=False)
    u, v = jnp.split(zc, 2, axis=-1)
    v32 = v.astype(F32)
    mean = jnp.mean(v32, axis=-1, keepdims=True)
    var = jnp.mean(jnp.square(v32 - mean), axis=-1, keepdims=True)
    v32 = (v32 - mean) * lax.rsqrt(var + LN_EPS) * ln_g.astype(F32) + ln_b.astype(F32)
    nb = T // SGU_BLOCK
    v32 = v32.reshape(B, nb, SGU_BLOCK, SGU_GROUPS, SGU_WIDTH // SGU_GROUPS)
    chunk_id = jnp.arange(SGU_BLOCK) // CHUNK
    mask = chunk_id[None, :] <= chunk_id[:, None]
    ws = jnp.where(mask[None], w_s.astype(F32), 0.0)
    mixed = jnp.einsum('gts,bnsgc->bntgc', ws, v32) + b_s.astype(F32).T[None, None, :, :, None]
    y = u * mixed.reshape(B, T, SGU_WIDTH).astype(u.dtype)
    return y @ w_out


def squared_relu_mlp(h, w1, w2):
    return jnp.square(jax.nn.relu(h @ w1)) @ w2


def setup_inputs(seed: int = 0) -> dict:
    key = jax.random.key(seed)
    ks = jax.random.split(key, 32)
    n = jax.random.normal
    D = D_MODEL
    inp = {}
    inp["x"] = n(ks[0], (BATCH, SEQ, D), F32)
    inp["norm_mix"] = 1.0 + 0.02 * n(ks[1], (DEPTH, D), F32)
    inp["norm_ffn"] = 1.0 + 0.02 * n(ks[2], (DEPTH, D), F32)
    inp["norm_final"] = 1.0 + 0.02 * n(ks[3], (D,), F32)
    inp["even_w_in"] = n(ks[4], (N_EVEN, D, EVEN_IN_WIDTH), F32) * D ** -0.5
    inp["even_mu"] = jax.random.uniform(ks[5], (N_EVEN, RW_SHIFT_WIDTH), F32)
    inp["even_w_up"] = n(ks[6], (N_EVEN, DECAY_LORA, RW_WIDTH), F32) * 0.5 * DECAY_LORA ** -0.5
    inp["even_w0"] = 0.5 * n(ks[7], (N_EVEN, RW_WIDTH), F32)
    inp["even_a_up"] = n(ks[8], (N_EVEN, ICL_LORA, RW_WIDTH), F32) * ICL_LORA ** -0.5
    inp["even_a0"] = 0.1 * n(ks[9], (N_EVEN, RW_WIDTH), F32)
    inp["even_g_up"] = n(ks[10], (N_EVEN, GATE_LORA, RW_WIDTH), F32) * GATE_LORA ** -0.5
    inp["even_k_k"] = 0.85 + 0.02 * n(ks[11], (N_EVEN, RW_WIDTH), F32)
    inp["even_k_a"] = 1.0 + 0.02 * n(ks[12], (N_EVEN, RW_WIDTH), F32)
    inp["even_r_k"] = 0.1 * n(ks[13], (N_EVEN, RW_HEADS, RW_HEAD_DIM), F32)
    inp["even_lnx_w"] = 1.0 + 0.02 * n(ks[14], (N_EVEN, RW_WIDTH), F32)
    inp["even_lnx_b"] = 0.02 * n(ks[15], (N_EVEN, RW_WIDTH), F32)
    inp["even_w_out"] = n(ks[16], (N_EVEN, EVEN_OUT_WIDTH, D), F32) * EVEN_OUT_WIDTH ** -0.5
    inp["odd_w_in"] = n(ks[17], (N_ODD, D, 2 * SGU_WIDTH), F32) * D ** -0.5
    inp["odd_ln_g"] = 1.0 + 0.02 * n(ks[18], (N_ODD, SGU_WIDTH), F32)
    inp["odd_ln_b"] = 0.02 * n(ks[19], (N_ODD, SGU_WIDTH), F32)
    inp["odd_w_s"] = n(ks[20], (N_ODD, SGU_GROUPS, SGU_BLOCK, SGU_BLOCK), F32) * SGU_BLOCK ** -0.5
    inp["odd_b_s"] = 1.0 + 0.1 * n(ks[21], (N_ODD, SGU_GROUPS, SGU_BLOCK), F32)
    inp["odd_w_out"] = n(ks[22], (N_ODD, SGU_WIDTH, D), F32) * SGU_WIDTH ** -0.5
    inp["ffn_w1"] = n(ks[23], (DEPTH, D, FFN_HIDDEN), F32) * D ** -0.5
    inp["ffn_w2"] = n(ks[24], (DEPTH, FFN_HIDDEN, D), F32) * FFN_HIDDEN ** -0.5
    return inp


def reference(x, norm_mix, norm_ffn, norm_final,
              even_w_in, even_mu, even_w_up, even_w0, even_a_up, even_a0, even_g_up,
              even_k_k, even_k_a, even_r_k, even_lnx_w, even_lnx_b, even_w_out,
              odd_w_in, odd_ln_g, odd_ln_b, odd_w_s, odd_b_s, odd_w_out,
              ffn_w1, ffn_w2):
    for layer in range(DEPTH):
        i = layer // 2
        h = rms_norm(x, norm_mix[layer])
        if layer % 2 == 0:
            mix = even_mixer(h, even_w_in[i], even_mu[i], even_w_up[i], even_w0[i],
                             even_a_up[i], even_a0[i], even_g_up[i], even_k_k[i],
                             even_k_a[i], even_r_k[i], even_lnx_w[i], even_lnx_b[i],
                             even_w_out[i])
        else:
            mix = odd_mixer(h, odd_w_in[i], odd_ln_g[i], odd_ln_b[i], odd_w_s[i],
                            odd_b_s[i], odd_w_out[i])
        x = x + mix.astype(x.dtype)
        h = rms_norm(x, norm_ffn[layer])
        x = x + squared_relu_mlp(h, ffn_w1[layer], ffn_w2[layer]).astype(x.dtype)
    return rms_norm(x, norm_final)
```

```python
import contextlib
import numpy as np
import concourse.bass as bass
import concourse.mybir as mybir
from concourse.bass_utils import run_bass_kernel_spmd

F32 = mybir.dt.float32
BF16 = mybir.dt.bfloat16
AF = mybir.ActivationFunctionType
ALU = mybir.AluOpType

D = 2048
SEQ = 2048
NB_CORE = 2
NT = NB_CORE * SEQ
TB = 512
NBLK = NT // TB
DEPTH = 4
HID = 8192
NC_D = D // 128
NJ = HID // 128
RWW = 3360
EIN = 6432
RMS_EPS = 1e-6
LN_EPS = 1e-5
LNX_EPS = 64e-5
C0 = float(np.exp(-0.5))


class Sem:
    def __init__(self, h):
        self.h = h
        self.n = 0


class Tile:
    def __init__(self, t, name):
        self.t = t
        self.name = name
        self.w = []
        self.r = []
        self.dsem = None

    def __getitem__(self, idx):
        return self.t[idx]


class Eng:
    def __init__(self, name, e, sem):
        self.name = name
        self.e = e
        self.sem = sem
        self.seen = {}


class K:
    def __init__(self, nc):
        self.nc = nc
        self.engs = {}
        for name, e in (("pe", nc.tensor), ("act", nc.scalar), ("dve", nc.vector),
                        ("pool", nc.gpsimd), ("sp", nc.sync)):
            self.engs[name] = Eng(name, e, Sem(nc.alloc_semaphore("p_" + name)))
        self.free_dsems = [Sem(nc.alloc_semaphore("d%d" % i)) for i in range(80)]
        self.live_dsems = []
        self.stack = None
        self.tiles = []
        self.uid = 0

    @contextlib.contextmanager
    def phase(self):
        self.stack = contextlib.ExitStack()
        self.tiles = []
        try:
            yield self
        finally:
            pass
        self.barrier()
        self.stack.close()
        self.stack = None
        self.free_dsems.extend(self.live_dsems)
        self.live_dsems = []
        self.tiles = []

    def sb(self, shape, dtype, name=None):
        self.uid += 1
        name = "%s_%d" % (name or "t", self.uid)
        t = self.stack.enter_context(self.nc.sbuf_tensor(name, list(shape), dtype))
        tl = Tile(t, name)
        self.tiles.append(tl)
        return tl

    def ps(self, name=None, dtype=F32, shape=(128, 512)):
        self.uid += 1
        name = "%s_%d" % (name or "ps", self.uid)
        t = self.stack.enter_context(self.nc.psum_tensor(name, list(shape), dtype))
        tl = Tile(t, name)
        self.tiles.append(tl)
        return tl

    def _wait(self, eng, toks):
        best = {}
        for (s, v) in toks:
            if v > best.get(id(s), (s, 0))[1]:
                best[id(s)] = (s, v)
        for (s, v) in best.values():
            if eng.seen.get(id(s), 0) >= v:
                continue
            eng.e.wait_ge(s.h, v)
            eng.seen[id(s)] = v

    def _deps(self, eng, reads, writes):
        toks = []
        for t in reads:
            for tk in t.w:
                if tk[0] is eng.sem and eng.name == "pe":
                    continue
                toks.append(tk)
        for t in writes:
            for tk in t.w + t.r:
                if tk[0] is eng.sem:
                    continue
                toks.append(tk)
        return toks

    @staticmethod
    def _note_read(t, tok):
        t.r = [x for x in t.r if x[0] is not tok[0]] + [tok]

    def op(self, en, reads, writes, fn, inc=True):
        eng = self.engs[en]
        self._wait(eng, self._deps(eng, reads, writes))
        ins = fn(eng.e)
        if inc:
            eng.sem.n += 1
            ins.then_inc(eng.sem.h, 1)
            tok = (eng.sem, eng.sem.n)
        else:
            tok = (eng.sem, eng.sem.n + 1)
        for t in writes:
            t.w = [tok]
            t.r = []
        for t in reads:
            if t not in writes:
                self._note_read(t, tok)
        return ins

    def _dsem(self, t):
        if t.dsem is None:
            t.dsem = self.free_dsems.pop()
            self.live_dsems.append(t.dsem)
        return t.dsem

    def dma(self, q, out, in_, reads=(), writes=(), owner=None):
        eng = self.engs[q]
        self._wait(eng, self._deps(eng, list(reads), list(writes)))
        owner = owner or (writes[0] if writes else reads[0])
        s = self._dsem(owner)
        s.n += 16
        eng.e.dma_start(out=out, in_=in_).then_inc(s.h, 16)
        tok = (s, s.n)
        for t in writes:
            t.w = [tok]
            t.r = []
        for t in reads:
            self._note_read(t, tok)
        return tok

    def barrier(self):
        toks = [(e.sem, e.sem.n) for e in self.engs.values()]
        toks += [(s, s.n) for s in self.live_dsems]
        for e in self.engs.values():
            self._wait(e, [tk for tk in toks if tk[0] is not e.sem and tk[1] > 0])
        for t in self.tiles:
            t.w = []
            t.r = []


def tile_w(W, bw):
    Kd, N = W.shape
    return np.ascontiguousarray(W.reshape(Kd // 128, 128, N // bw, bw).transpose(2, 1, 0, 3))


class WPack:
    def __init__(self):
        self.parts = []
        self.off = {}
        self.rows = 0

    def add(self, name, arr):
        a = np.ascontiguousarray(arr, dtype=np.float32).reshape(-1)
        assert a.size % 2048 == 0, (name, a.size)
        self.off[name] = self.rows
        self.parts.append(a.reshape(-1, 2048))
        self.rows += a.size // 2048

    def build(self):
        return np.concatenate(self.parts, axis=0)


def wpack_layout():
    off = {}
    rows = []
    for l in range(DEPTH):
        r = 0
        if l % 2 == 0:
            off["ein_qk%d" % l] = r; r += 16 * 128 * 16 * 128 // 2048
            off["ein_v%d" % l] = r; r += 2 * 128 * 16 * 512 // 2048
            off["ein_rw%d" % l] = r; r += 27 * 128 * 16 * 128 // 2048
            off["eout%d" % l] = r; r += 16 * 128 * 16 * 128 // 2048
        else:
            off["oin_u%d" % l] = r; r += 16 * 128 * 16 * 128 // 2048
            off["oin_v%d" % l] = r; r += 4 * 128 * 16 * 512 // 2048
            off["oout%d" % l] = r; r += 16 * 128 * 16 * 128 // 2048
        off["w1_%d" % l] = r; r += 64 * 128 * 16 * 128 // 2048
        off["w2_%d" % l] = r; r += 16 * 128 * 64 * 128 // 2048
        rows.append(r)
    return off, rows


def pack_weights(inp):
    off, rows = wpack_layout()
    out = []
    for l in range(DEPTH):
        wp = WPack()
        i = l // 2
        if l % 2 == 0:
            win = inp["even_w_in"][i]
            wp.add("ein_qk%d" % l, tile_w(win[:, :2048], 128))
            wp.add("ein_v%d" % l, tile_w(win[:, 2048:3072], 512))
            wrw = np.zeros((D, 27 * 128), np.float32)
            wrw[:, :RWW] = win[:, 3072:]
            wp.add("ein_rw%d" % l, tile_w(wrw, 128))
            wp.add("eout%d" % l, tile_w(inp["even_w_out"][i], 128))
        else:
            win = inp["odd_w_in"][i]
            wp.add("oin_u%d" % l, tile_w(win[:, :2048], 128))
            wp.add("oin_v%d" % l, tile_w(win[:, 2048:], 512))
            wp.add("oout%d" % l, tile_w(inp["odd_w_out"][i], 128))
        wp.add("w1_%d" % l, tile_w(inp["ffn_w1"][l], 128))
        w2t = tile_w(inp["ffn_w2"][l], 128).reshape(16, 128, 2, 32, 128).transpose(0, 2, 1, 3, 4)
        wp.add("w2_%d" % l, w2t)
        for k_, v in wp.off.items():
            assert off[k_] == v
        assert wp.rows == rows[l]
        out.append(wp.build())
    return out


def wview(wbf, row0, F, n=1):
    a = F // 2048
    v = wbf[row0:row0 + n * 128 * a, :]
    return v.rearrange("(n p a) c -> p n (a c)", n=n, p=128, a=a)


class Prog:
    def __init__(self, nc, k, dr):
        self.nc = nc
        self.k = k
        self.dr = dr

    def load_x(self, xb, blk, src=None):
        k = self.k
        xT = src if src is not None else self.dr["xT"]
        for g in range(4):
            src = xT[g * 512:(g + 1) * 512, blk * TB:(blk + 1) * TB].rearrange("(c p) t -> p c t", p=128)
            k.dma("sp", xb[:, g * 4:(g + 1) * 4, :], src, writes=[xb])

    def store_x(self, xb, blk, dst=None):
        k = self.k
        xT = dst if dst is not None else self.dr["xT"]
        for g in range(4):
            d = xT[g * 512:(g + 1) * 512, blk * TB:(blk + 1) * TB].rearrange("(c p) t -> p c t", p=128)
            k.dma("pool", d, xb[:, g * 4:(g + 1) * 4, :], reads=[xb])

    def rms(self, xb, hT, gcol, ps_ss, sqs, rstd, tmp, onesb, eps_t):
        k = self.k
        for c in range(NC_D):
            sq = sqs[c % len(sqs)]
            k.op("act", [xb], [sq], lambda e, c=c, sq=sq: e.activation(out=sq[:, :], in_=xb[:, c, :], func=AF.Square))
            k.op("pe", [sq, onesb], [ps_ss], lambda e, c=c, sq=sq: e.matmul(
                ps_ss[:, :], lhsT=onesb[:, :], rhs=sq[:, :], start=(c == 0), stop=(c == NC_D - 1)),
                inc=True)
        k.op("act", [ps_ss, eps_t], [tmp], lambda e: e.activation(
            out=tmp[:, :], in_=ps_ss[:, :], func=AF.Sqrt, scale=1.0 / D, bias=eps_t[:, 0:1]))
        k.op("dve", [tmp], [rstd], lambda e: e.reciprocal(out=rstd[:, :], in_=tmp[:, :]))
        for c in range(NC_D):
            k.op("dve", [xb, rstd, gcol], [hT], lambda e, c=c: e.scalar_tensor_tensor(
                out=hT[:, c, :], in0=xb[:, c, :], scalar=gcol[:, c:c + 1], op0=ALU.mult,
                in1=rstd[:, :], op1=ALU.mult))

    def ffn_stage1(self, l, hf, hT, hid, w1b, ps_h, rl, cnt):
        k = self.k
        wbf = self.dr["wbf%d" % l]
        o1 = self.off["w1_%d" % l]
        for jg in range(hf * 16, hf * 16 + 16):
            wt = w1b[cnt[0] % len(w1b)]; cnt[0] += 1
            k.dma("sp", wt[:, :, :], wview(wbf, o1 + jg * 2 * 128, 2048, 2), writes=[wt])
            for jj in range(2):
                j = jg * 2 + jj
                ph = ps_h[j % len(ps_h)]
                for c in range(NC_D):
                    k.op("pe", [wt, hT], [ph], lambda e, c=c, jj=jj, wt=wt, ph=ph: e.matmul(
                        ph[:, :], lhsT=wt[:, jj, c * 128:(c + 1) * 128], rhs=hT[:, c, :],
                        start=(c == 0), stop=(c == NC_D - 1)), inc=(c == NC_D - 1))
                r = rl[j % len(rl)]
                k.op("act", [ph], [r], lambda e, ph=ph, r=r: e.activation(out=r[:, :], in_=ph[:, :], func=AF.Relu))
                k.op("dve", [r], [hid], lambda e, j=j, r=r: e.tensor_tensor(
                    out=hid[:, j - hf * 32, :], in0=r[:, :], in1=r[:, :], op=ALU.mult))

    def ffn_stage2(self, l, hf, crange, xb, hid, w2b, ps_o, cnt):
        k = self.k
        wbf = self.dr["wbf%d" % l]
        o2 = self.off["w2_%d" % l]
        for c in crange:
            po = ps_o[c % len(ps_o)]
            wt = w2b[cnt[0] % len(w2b)]; cnt[0] += 1
            k.dma("sp", wt[:, :, :], wview(wbf, o2 + c * 512 + hf * 256, 4096, 1), writes=[wt])
            for jj in range(32):
                k.op("pe", [wt, hid], [po], lambda e, jj=jj, wt=wt, po=po: e.matmul(
                    po[:, :], lhsT=wt[:, 0, jj * 128:(jj + 1) * 128], rhs=hid[:, jj, :],
                    start=(jj == 0), stop=(jj == 31)), inc=(jj == 31))
            k.op("dve", [po, xb], [xb], lambda e, c=c, po=po: e.tensor_tensor(
                out=xb[:, c, :], in0=xb[:, c, :], in1=po[:, :], op=ALU.add))

    def phase_copyx(self):
        k = self.k
        with k.phase():
            dummy = Tile(None, "cpdummy")
            k.tiles.append(dummy)
            for g in range(8):
                k.dma("sp", self.dr["xT"][g * 256:(g + 1) * 256, :], self.dr["xin"][g * 256:(g + 1) * 256, :], writes=[dummy])

    def phase_cast(self, l, part, bg=False):
        k = self.k
        if not hasattr(self, "cast_tok"):
            self.cast_tok = {}
        with k.phase():
            CH = 1024
            wsrc = self.dr["wflat%d" % l]
            wbf = self.dr["wbf%d" % l]
            split = self.off["w1_%d" % l]
            s1 = self.off["ein_v%d" % l] if l % 2 == 0 else split
            r, r1 = {"a1": (0, s1), "a2": (s1, split), "b": (split, self.wrows[l])}[part]
            sem = k.free_dsems.pop()
            if not bg:
                k.live_dsems.append(sem)
            while r < r1:
                n = min(CH, r1 - r)
                sem.n += 16
                k.engs["pool"].e.dma_start(out=wbf[r:r + n, :], in_=wsrc[r:r + n, :]).then_inc(sem.h, 16)
                r += n
            self.cast_tok[(l, part)] = (sem, sem.n)

    def wait_cast(self, l, part="a1"):
        toks = [t for (ll, pp), t in getattr(self, "cast_tok", {}).items() if ll == l and pp <= part]
        for e in self.k.engs.values():
            self.k._wait(e, toks)

    def consts(self, ph):
        k = self.k
        ones32 = k.sb([128, 128], F32, "ones32")
        k.dma("sp", ones32[:, :], self.dr["c_ones32"], writes=[ones32])
        gains = k.sb([128, 9 * NC_D], F32, "gains")
        k.dma("sp", gains[:, :], self.dr["gains"], writes=[gains])
        eps_t = k.sb([128, 4], F32, "eps")
        k.dma("sp", eps_t[:, :], self.dr["c_eps"], writes=[eps_t])
        self.onesb = k.sb([128, 128], BF16, "onesb16")
        k.op("dve", [], [self.onesb], lambda e: e.memset(self.onesb[:, :], 1.0))
        return ones32, gains, eps_t

    def phase_ffn(self, l):
        self.wait_cast(l, "b")
        k = self.k
        with k.phase() as ph:
            ones32, gains, eps_t = self.consts(ph)
            xbs = [k.sb([128, NC_D, TB], F32, "xb") for _ in range(2)]
            hTs = [k.sb([128, NC_D, TB], BF16, "hT") for _ in range(2)]
            hid = k.sb([128, NJ // 2, TB], BF16, "hid")
            w1b = [k.sb([128, 2, 2048], BF16, "w1") for _ in range(3)]
            w2b = [k.sb([128, 1, 4096], BF16, "w2") for _ in range(3)]
            sqs = [k.sb([128, TB], BF16, "sq") for _ in range(2)]
            rl = [k.sb([128, TB], F32, "rl") for _ in range(2)]
            rstd = k.sb([128, TB], F32, "rstd")
            tmp = k.sb([128, TB], F32, "tmp")
            ps_ss = k.ps("ss")
            ps_h = [k.ps("h") for _ in range(2)]
            ps_o = [k.ps("o") for _ in range(2)]
            g = _GV(gains, (4 + l) * NC_D)
            c1, c2 = [0], [0]
            self.load_x(xbs[0], 0)
            self.rms(xbs[0], hTs[0], g, ps_ss, sqs, rstd, tmp, self.onesb, eps_t)
            for blk in range(self.nblk):
                xb, hT = xbs[blk % 2], hTs[blk % 2]
                nxt = blk + 1 < self.nblk
                if nxt:
                    self.load_x(xbs[(blk + 1) % 2], blk + 1)
                self.ffn_stage1(l, 0, hT, hid, w1b, ps_h, rl, c1)
                self.ffn_stage2(l, 0, range(NC_D), xb, hid, w2b, ps_o, c2)
                self.ffn_stage1(l, 1, hT, hid, w1b, ps_h, rl, c1)
                self.ffn_stage2(l, 1, range(0, 8), xb, hid, w2b, ps_o, c2)
                if nxt:
                    self.rms(xbs[(blk + 1) % 2], hTs[(blk + 1) % 2], g, ps_ss, sqs, rstd, tmp, self.onesb, eps_t)
                self.ffn_stage2(l, 1, range(8, NC_D), xb, hid, w2b, ps_o, c2)
                self.store_x(xb, blk)

    def phase_odd(self, l):
        self.wait_cast(l, "a2")
        k = self.k
        i = l // 2
        wbf = self.dr["wbf%d" % l]
        xT = self.dr["xT"]
        ou, ov, oo = self.off["oin_u%d" % l], self.off["oin_v%d" % l], self.off["oout%d" % l]
        with k.phase() as ph:
            ones32, gains, eps_t = self.consts(ph)
            xb = k.sb([128, NC_D, TB], F32, "xb")
            hTs = [k.sb([128, NC_D, TB], BF16, "hT") for _ in range(2)]
            uT = k.sb([128, NC_D, TB], BF16, "uT")
            vt = k.sb([128, 4, D], F32, "vt")
            vn = k.sb([128, 4, D], BF16, "vn")
            wA = [k.sb([128, 2, 2048], BF16, "wA") for _ in range(2)]
            wv = [k.sb([128, NC_D, 512], BF16, "wv") for _ in range(2)]
            lng = k.sb([128, D], F32, "lng")
            lnb = k.sb([128, D], F32, "lnb")
            wsb = k.sb([128, 8, 128], BF16, "wsb")
            bsb = k.sb([1, 1024], BF16, "bsb")
            onesb = self.onesb
            xr = [k.sb([128, TB], F32, "xr") for _ in range(2)]
            sqs = [k.sb([128, TB], BF16, "sq") for _ in range(2)]
            rstd = k.sb([128, TB], F32, "rstd")
            tmp = k.sb([128, TB], F32, "tmp")
            st = k.sb([128, 8], F32, "st")
            ps_ss = k.ps("ss")
            ps_a = [k.ps("a") for _ in range(2)]
            ps_v = [k.ps("v") for _ in range(2)]
            ps_m = [k.ps("m") for _ in range(2)]
            k.dma("sp", lng[:, :], self.dr["odd_lng"][i], writes=[lng])
            k.dma("sp", lnb[:, :], self.dr["odd_lnb"][i], writes=[lnb])
            k.dma("pool", wsb[:, :, :], self.dr["odd_wsT"][i], writes=[wsb])
            k.dma("pool", bsb[:, :], self.dr["odd_bs"][i], writes=[bsb])
            k.op("dve", [wsb], [wsb], lambda e: e.memset(wsb[64:128, :, 0:64], 0.0))
            g = _GV(gains, l * NC_D)
            na = 0
            nx = 0
            self.load_x(xb, 0)
            self.rms(xb, hTs[0], g, ps_ss, sqs, rstd, tmp, self.onesb, eps_t)
            for blk in range(self.nblk):
                cols = slice(blk * TB, (blk + 1) * TB)
                hT = hTs[blk % 2]
                nxt = blk + 1 < self.nblk
                for nb in range(4):
                    wt = wv[nb % 2]
                    k.dma("sp", wt[:, :, :], wview(wbf, ov + nb * 512, 8192, 1)[:, 0, :].rearrange(
                        "p (c m) -> p c m", c=NC_D), writes=[wt])
                    for tt in range(4):
                        pv = ps_v[(nb * 4 + tt) % 2]
                        for dc in range(NC_D):
                            k.op("pe", [wt, hT], [pv], lambda e, dc=dc, tt=tt, wt=wt, pv=pv: e.matmul(
                                pv[:, :], lhsT=hT[:, dc, tt * 128:(tt + 1) * 128], rhs=wt[:, dc, :],
                                start=(dc == 0), stop=(dc == NC_D - 1)), inc=(dc == NC_D - 1))
                        k.op("act", [pv], [vt], lambda e, nb=nb, tt=tt, pv=pv: e.activation(
                            out=vt[:, tt, nb * 512:(nb + 1) * 512], in_=pv[:, :], func=AF.Gelu))
                if nxt:
                    self.load_x(xb, blk + 1)
                for tt in range(4):
                    k.op("act", [vt], [vn, st], lambda e, tt=tt: e.activation(
                        out=vn[:, tt, :], in_=vt[:, tt, :], func=AF.Identity, accum_out=st[:, 0:1]))
                    k.op("dve", [st], [st], lambda e: e.tensor_scalar(
                        out=st[:, 1:2], in0=st[:, 0:1], scalar1=-1.0 / D, scalar2=None, op0=ALU.mult))
                    k.op("act", [vt, st], [vt], lambda e, tt=tt: e.activation(
                        out=vt[:, tt, :], in_=vt[:, tt, :], func=AF.Identity, bias=st[:, 1:2]))
                    k.op("act", [vt], [vn, st], lambda e, tt=tt: e.activation(
                        out=vn[:, tt, :], in_=vt[:, tt, :], func=AF.Square, accum_out=st[:, 2:3]))
                    k.op("act", [st, eps_t], [st], lambda e: e.activation(
                        out=st[:, 3:4], in_=st[:, 2:3], func=AF.Sqrt, scale=1.0 / D, bias=eps_t[:, 1:2]))
                    k.op("dve", [st], [st], lambda e: e.reciprocal(out=st[:, 4:5], in_=st[:, 3:4]))
                    k.op("dve", [vt, st, lng], [vt], lambda e, tt=tt: e.scalar_tensor_tensor(
                        out=vt[:, tt, :], in0=vt[:, tt, :], scalar=st[:, 4:5], op0=ALU.mult,
                        in1=lng[:, :], op1=ALU.mult))
                    k.op("dve", [vt, lnb], [vn], lambda e, tt=tt: e.tensor_tensor(
                        out=vn[:, tt, :], in0=vt[:, tt, :], in1=lnb[:, :], op=ALU.add))
                for cp in range(8):
                    wt = wA[na % 2]; na += 1
                    k.dma("sp", wt[:, :, :], wview(wbf, ou + cp * 256, 2048, 2), writes=[wt])
                    for jj in range(2):
                        c = cp * 2 + jj
                        pa = ps_a[c % 2]
                        for dc in range(NC_D):
                            k.op("pe", [wt, hT], [pa], lambda e, dc=dc, jj=jj, wt=wt, pa=pa: e.matmul(
                                pa[:, :], lhsT=wt[:, jj, dc * 128:(dc + 1) * 128], rhs=hT[:, dc, :],
                                start=(dc == 0), stop=(dc == NC_D - 1)), inc=(dc == NC_D - 1))
                        k.op("act", [pa], [uT], lambda e, c=c, pa=pa: e.activation(
                            out=uT[:, c, :], in_=pa[:, :], func=AF.Gelu))
                if nxt:
                    self.rms(xb, hTs[(blk + 1) % 2], g, ps_ss, sqs, rstd, tmp, self.onesb, eps_t)
                for c in range(NC_D):
                    gI = c // 2
                    pm = ps_m[c % 2]
                    for tt in range(4):
                        k.op("pe", [vn, wsb], [pm], lambda e, c=c, tt=tt, gI=gI, pm=pm: e.matmul(
                            pm[:, tt * 128:(tt + 1) * 128], lhsT=vn[:, tt, c * 128:(c + 1) * 128],
                            rhs=wsb[:, gI, :], start=True, stop=False), inc=False)
                        k.op("pe", [onesb, bsb], [pm], lambda e, tt=tt, gI=gI, pm=pm: e.matmul(
                            pm[:, tt * 128:(tt + 1) * 128], lhsT=onesb[0:1, :],
                            rhs=bsb[0:1, gI * 128:(gI + 1) * 128], start=False, stop=True), inc=(tt == 3))
                    k.op("dve", [pm, uT], [uT], lambda e, c=c, pm=pm: e.tensor_tensor(
                        out=uT[:, c, :], in0=pm[:, :], in1=uT[:, c, :], op=ALU.mult))
                for cp in range(8):
                    wt = wA[na % 2]; na += 1
                    k.dma("sp", wt[:, :, :], wview(wbf, oo + cp * 256, 2048, 2), writes=[wt])
                    for jj in range(2):
                        c = cp * 2 + jj
                        pa = ps_a[c % 2]
                        xc = xr[nx % 2]; nx += 1
                        k.dma("sp", xc[:, :], xT[c * 128:(c + 1) * 128, cols], writes=[xc])
                        for cc in range(NC_D):
                            k.op("pe", [wt, uT], [pa], lambda e, cc=cc, jj=jj, wt=wt, pa=pa: e.matmul(
                                pa[:, :], lhsT=wt[:, jj, cc * 128:(cc + 1) * 128], rhs=uT[:, cc, :],
                                start=(cc == 0), stop=(cc == NC_D - 1)), inc=(cc == NC_D - 1))
                        k.op("dve", [pa, xc], [xc], lambda e, xc=xc, pa=pa: e.tensor_tensor(
                            out=xc[:, :], in0=xc[:, :], in1=pa[:, :], op=ALU.add))
                        k.dma("pool", xT[c * 128:(c + 1) * 128, cols], xc[:, :], reads=[xc])

    def phase_e1(self, l):
        self.wait_cast(l)
        k = self.k
        wbf = self.dr["wbf%d" % l]
        oq, ov, orw = self.off["ein_qk%d" % l], self.off["ein_v%d" % l], self.off["ein_rw%d" % l]
        qkT, vtok, zrwT = self.dr["qkT"], self.dr["vtok"], self.dr["zrwT"]
        with k.phase() as ph:
            ones32, gains, eps_t = self.consts(ph)
            xbs = [k.sb([128, NC_D, TB], F32, "xb") for _ in range(2)]
            hTs = [k.sb([128, NC_D, TB], BF16, "hT") for _ in range(2)]
            wA = [k.sb([128, 2, 2048], BF16, "wA") for _ in range(3)]
            wv = [k.sb([128, NC_D, 512], BF16, "wv") for _ in range(2)]
            sqk = [k.sb([128, 2, TB], BF16, "sqk") for _ in range(3)]
            sv = [k.sb([128, 4, 512], BF16, "sv") for _ in range(2)]
            srw = [k.sb([128, 2, TB], F32, "srw") for _ in range(3)]
            sqs = [k.sb([128, TB], BF16, "sq") for _ in range(2)]
            rstd = k.sb([128, TB], F32, "rstd")
            tmp = k.sb([128, TB], F32, "tmp")
            ps_ss = k.ps("ss")
            ps_a = [k.ps("a") for _ in range(3)]
            ps_v = [k.ps("v") for _ in range(2)]
            g = _GV(gains, l * NC_D)
            na = 0
            ncp = 0
            xsrc = self.dr["xin"] if l == 0 else None
            self.load_x(xbs[0], 0, src=xsrc)
            self.rms(xbs[0], hTs[0], g, ps_ss, sqs, rstd, tmp, self.onesb, eps_t)
            for blk in range(self.nblk):
                cols = slice(blk * TB, (blk + 1) * TB)
                xb, hT = xbs[blk % 2], hTs[blk % 2]
                nxt = blk + 1 < self.nblk
                if nxt:
                    self.load_x(xbs[(blk + 1) % 2], blk + 1, src=xsrc)
                jobs = [("qk", cp) for cp in range(8)] + [("rw", cp) for cp in range(14)]
                for kind, cp in jobs:
                    if blk == 0 and kind == "rw" and cp == 0:
                        self.wait_cast(l, "a2")
                    wt = wA[na % 3]; na += 1
                    n = 2 if not (kind == "rw" and cp == 13) else 1
                    base = (oq if kind == "qk" else orw) + cp * 256
                    k.dma("sp", wt[:, 0:n, :], wview(wbf, base, 2048, n), writes=[wt])
                    stg = (sqk if kind == "qk" else srw)[ncp % 3]
                    for jj in range(n):
                        pa = ps_a[ncp % 3]
                        for dc in range(NC_D):
                            k.op("pe", [wt, hT], [pa], lambda e, dc=dc, jj=jj, wt=wt, pa=pa: e.matmul(
                                pa[:, :], lhsT=wt[:, jj, dc * 128:(dc + 1) * 128], rhs=hT[:, dc, :],
                                start=(dc == 0), stop=(dc == NC_D - 1)), inc=(dc == NC_D - 1))
                        if (cp + jj) % 2 == 0:
                            k.op("act", [pa], [stg], lambda e, jj=jj, pa=pa, stg=stg: e.activation(
                                out=stg[:, jj, :], in_=pa[:, :], func=AF.Copy))
                        else:
                            k.op("dve", [pa], [stg], lambda e, jj=jj, pa=pa, stg=stg: e.tensor_copy(
                                out=stg[:, jj, :], in_=pa[:, :]))
                        ncp += 1 if jj == n - 1 else 0
                        if jj < n - 1:
                            ncp += 0
                    dst = (qkT if kind == "qk" else zrwT)[cp * 256:cp * 256 + n * 128, cols].rearrange(
                        "(j p) t -> p j t", p=128)
                    k.dma("pool", dst, stg[:, 0:n, :], reads=[stg])
                if nxt:
                    self.rms(xbs[(blk + 1) % 2], hTs[(blk + 1) % 2], g, ps_ss, sqs, rstd, tmp, self.onesb, eps_t)
                for nb in range(2):
                    wt = wv[nb % 2]
                    k.dma("sp", wt[:, :, :], wview(wbf, ov + nb * 512, 8192, 1)[:, 0, :].rearrange(
                        "p (c m) -> p c m", c=NC_D), writes=[wt])
                    stg = sv[nb % 2]
                    for tt in range(4):
                        pv = ps_v[tt % 2]
                        for dc in range(NC_D):
                            k.op("pe", [wt, hT], [pv], lambda e, dc=dc, tt=tt, wt=wt, pv=pv: e.matmul(
                                pv[:, :], lhsT=hT[:, dc, tt * 128:(tt + 1) * 128], rhs=wt[:, dc, :],
                                start=(dc == 0), stop=(dc == NC_D - 1)), inc=(dc == NC_D - 1))
                        k.op("act", [pv], [stg], lambda e, tt=tt, pv=pv, stg=stg: e.activation(
                            out=stg[:, tt, :], in_=pv[:, :], func=AF.Copy))
                    dst = vtok[cols, nb * 512:(nb + 1) * 512].rearrange("(t p) c -> p t c", p=128)
                    k.dma("pool", dst, stg[:, :, :], reads=[stg])

    def phase_sb(self, l):
        k = self.k
        qkT, vtok, oT = self.dr["qkT"], self.dr["vtok"], self.dr["oT"]
        nseq = max(1, self.nblk * TB // SEQ)
        L = min(SEQ, self.nblk * TB)
        nqg = L // 512
        with k.phase() as ph:
            c32 = k.sb([128, 6, 512], F32, "c32")
            k.dma("sp", c32[:, :, :], self.dr["c_sb"], writes=[c32])
            cb = k.sb([128, 6, 512], BF16, "cb")
            k.op("dve", [c32], [cb], lambda e: e.tensor_copy(out=cb[:, :, :], in_=c32[:, :, :]))
            one_t = k.sb([128, 1], F32, "one")
            k.op("dve", [], [one_t], lambda e: e.memset(one_t[:, :], 1.0))
            qs = [k.sb([64, SEQ], BF16, "q") for _ in range(2)]
            ks = [k.sb([64, SEQ], BF16, "k") for _ in range(2)]
            nks = [None, None]
            xcs = [k.sb([128, 512], F32, "xc") for _ in range(2)]
            vs = [k.sb([128, 16, 64], BF16, "v") for _ in range(2)]
            ohs = [k.sb([64, SEQ], BF16, "oh") for _ in range(2)]
            e32 = [k.sb([128, 512], F32, "e32") for _ in range(2)]
            sps = [k.sb([128, 512], BF16, "sp") for _ in range(2)]
            A32 = k.sb([128, 512], F32, "A32")
            Abf = [k.sb([128, 512], BF16, "Abf") for _ in range(2)]
            ws = [k.sb([128, 512], BF16, "w") for _ in range(3)]
            ps_z = [k.ps("z") for _ in range(2)]
            ps_c = [k.ps("c") for _ in range(2)]
            ps_o = [k.ps("o") for _ in range(2)]
            Abf3 = Abf + [k.sb([128, 512], BF16, "Abf")]
            its = []
            hi = 0
            for b in range(nseq):
                for h in range(16):
                    for qg in range(nqg):
                        kbs = list(range(4 * qg + 3, -1, -1))
                        for idx, kb in enumerate(kbs):
                            its.append(dict(b=b, h=h, hi=hi, qg=qg, idx=idx, kb=kb, nkb=len(kbs),
                                            gq=hi * nqg + qg))
                    hi += 1

            def head_tiles(hi_):
                return qs[hi_ % 2], ks[hi_ % 2], nks[hi_ % 2], vs[hi_ % 2], ohs[hi_ % 2]

            def head_load(itd):
                b, h = itd["b"], itd["h"]
                q, kk, nk, v, oh = head_tiles(itd["hi"])
                tc_ = slice(b * SEQ, b * SEQ + L)
                k.dma("sp", q[:, 0:L], qkT[h * 64:(h + 1) * 64, tc_], writes=[q])
                k.dma("sp", kk[:, 0:L], qkT[1024 + h * 64:1024 + (h + 1) * 64, tc_], writes=[kk])
                k.dma("sp", v[:, 0:L // 128, :], vtok[tc_, h * 64:(h + 1) * 64].rearrange(
                    "(s p) c -> p s c", p=128), writes=[v])

            def stage1(n, itd):
                if itd["qg"] == 0 and itd["idx"] == 0:
                    head_load(itd)
                q, kk, nk, v, oh = head_tiles(itd["hi"])
                qg, kb, idx = itd["qg"], itd["kb"], itd["idx"]
                i = kb - 4 * qg
                qsl = slice(qg * 512, (qg + 1) * 512)
                ksl = slice(kb * 128, (kb + 1) * 128)
                pz, e_, sp = ps_z[n % 2], e32[n % 2], sps[n % 2]
                k.op("pe", [kk, q], [pz], lambda e: e.matmul(
                    pz[:, :], lhsT=kk[:, ksl], rhs=q[:, qsl], start=True, stop=True))
                k.op("act", [pz], [e_], lambda e: e.activation(out=e_[:, :], in_=pz[:, :], func=AF.Exp, scale=0.125))
                k.op("act", [e_, one_t], [sp], lambda e: e.activation(
                    out=sp[:, :], in_=e_[:, :], func=AF.Ln, bias=one_t[:, 0:1]))
                if i >= 0:
                    k.op("dve", [sp, cb], [sp], lambda e: e.tensor_tensor(
                        out=sp[:, :], in0=sp[:, :], in1=cb[:, 2 + i, :], op=ALU.mult))
                if idx < itd["nkb"] - 1:
                    abn = Abf3[(n + 1) % 3]
                    if idx == 0:
                        k.op("dve", [sp], [abn], lambda e: e.tensor_copy(out=abn[:, :], in_=sp[:, :]))
                    else:
                        abc = Abf3[n % 3]
                        k.op("dve", [sp, abc], [abn], lambda e: e.tensor_tensor(
                            out=abn[:, :], in0=abc[:, :], in1=sp[:, :], op=ALU.add))

            def stage2(n, itd):
                q, kk, nk, v, oh = head_tiles(itd["hi"])
                qg, kb, idx = itd["qg"], itd["kb"], itd["idx"]
                i = kb - 4 * qg
                qsl = slice(qg * 512, (qg + 1) * 512)
                ksl = slice(kb * 128, (kb + 1) * 128)
                pc, sp, w = ps_c[n % 2], sps[n % 2], ws[n % 3]
                first = idx == 0
                last = idx == itd["nkb"] - 1
                ab = Abf3[n % 3]
                k.op("pe", [cb, sp], [pc], lambda e: e.matmul(
                    pc[:, :], lhsT=cb[:, 0, 0:128], rhs=sp[:, :], start=True, stop=first), inc=first)
                if not first:
                    k.op("pe", [cb, ab], [pc], lambda e: e.matmul(
                        pc[:, :], lhsT=cb[:, 1, 0:128], rhs=ab[:, :], start=False, stop=True))
                xc_, e_ = xcs[n % 2], e32[n % 2]
                k.op("act", [pc], [xc_], lambda e: e.activation(out=xc_[:, :], in_=pc[:, :], func=AF.Exp, scale=-1.0))
                k.op("dve", [xc_, e_], [w], lambda e: e.tensor_tensor(out=w[:, :], in0=xc_[:, :], in1=e_[:, :], op=ALU.mult))
                if i >= 0:
                    k.op("dve", [w, cb], [w], lambda e: e.tensor_tensor(
                        out=w[:, :], in0=w[:, :], in1=cb[:, 2 + i, :], op=ALU.mult))

            def stage3(n, itd):
                q, kk, nk, v, oh = head_tiles(itd["hi"])
                qg, kb, idx = itd["qg"], itd["kb"], itd["idx"]
                qsl = slice(qg * 512, (qg + 1) * 512)
                w = ws[n % 3]
                po = ps_o[itd["gq"] % 2]
                first = idx == 0
                last = idx == itd["nkb"] - 1
                k.op("pe", [v, w], [po], lambda e: e.matmul(
                    po[0:64, :], lhsT=v[:, kb, :], rhs=w[:, :], start=first, stop=last), inc=last)
                if last:
                    k.op("dve", [po], [oh], lambda e: e.tensor_copy(out=oh[:, qsl], in_=po[0:64, :]))
                    if qg == nqg - 1:
                        b, h = itd["b"], itd["h"]
                        tc_ = slice(b * SEQ, b * SEQ + L)
                        k.dma("pool", oT[h * 64:(h + 1) * 64, tc_], oh[:, 0:L], reads=[oh])

            stage1(0, its[0])
            N_ = len(its)
            for n in range(N_ + 1):
                if n + 1 < N_:
                    stage1(n + 1, its[n + 1])
                if n < N_:
                    stage2(n, its[n])
                if n >= 1:
                    stage3(n - 1, its[n - 1])

    def phase_rw(self, l):
        k = self.k
        i = l // 2
        zrwT, oT = self.dr["zrwT"], self.dr["oT"]
        nseq = max(1, self.nblk * TB // SEQ)
        L = min(SEQ, self.nblk * TB)
        nq = L // 512
        NP = 12
        HH = (slice(0, 64), slice(64, 128))
        with k.phase() as ph:
            ones32, gains, eps_t = self.consts(ph)
            crw = k.sb([128, 5, 512], F32, "crw")
            k.dma("sp", crw[:, :, :], self.dr["c_rw"], writes=[crw])
            bd = k.sb([128, 128], F32, "bd")
            k.dma("sp", bd[:, :], self.dr["c_bd"], writes=[bd])
            par = k.sb([128, 8, NP], F32, "par")
            k.dma("sp", par[:, :, :], self.dr["rw_par"][i], writes=[par])
            k.op("dve", [par], [par], lambda e: e.tensor_scalar(
                out=par[:, :, 10:11], in0=par[:, :, 6:7], scalar1=-1.0, scalar2=1.0, op0=ALU.mult, op1=ALU.add))
            mux = k.sb([128, 4], F32, "mux")
            k.dma("sp", mux[:, :], self.dr["rw_mux"][i], writes=[mux])
            wup = k.sb([64, 1024], F32, "wup")
            aup = k.sb([64, 1024], F32, "aup")
            g0 = k.sb([128, 1024], F32, "g0")
            g1 = k.sb([32, 1024], F32, "g1")
            k.dma("sp", wup[:, :], self.dr["rw_wup"][i], writes=[wup])
            k.dma("sp", aup[:, :], self.dr["rw_aup"][i], writes=[aup])
            k.dma("sp", g0[:, :], self.dr["rw_gup"][i, 0:128, :], writes=[g0])
            k.dma("sp", g1[:, :], self.dr["rw_gup"][i, 128:160, :], writes=[g1])
            Sall = k.sb([128, 16, 64], F32, "Sall")
            k.op("dve", [], [Sall], lambda e: e.memset(Sall[:, :, :], 0.0))
            raws = [k.sb([128, 513], F32, "raw") for _ in range(4)]
            nraw = [0]
            SU, SL, IU, IDT, RST = 0, 1, 2, 3, 4
            tmpA = k.sb([128, 512], F32, "tmpA")

            def load_shift(dst, rows, row0, mucol, b, q, reads_mu):
                raw = raws[nraw[0] % 4]; nraw[0] += 1
                t0 = b * SEQ + q * 512
                if q == 0:
                    k.op("dve", [], [raw], lambda e: e.memset(raw[0:rows, 0:1], 0.0))
                    k.dma("sp", raw[0:rows, 1:513], zrwT[row0:row0 + rows, t0:t0 + 512], writes=[raw])
                else:
                    k.dma("sp", raw[0:rows, 0:513], zrwT[row0:row0 + rows, t0 - 1:t0 + 512], writes=[raw])
                T = tmpA
                k.op("dve", [raw], [T], lambda e: e.tensor_tensor(
                    out=T[0:rows, :], in0=raw[0:rows, 0:512], in1=raw[0:rows, 1:513], op=ALU.subtract))
                k.op("dve", [T, raw, reads_mu], [dst], lambda e: e.scalar_tensor_tensor(
                    out=dst[0:rows, 0:512], in0=T[0:rows, :], scalar=mucol(rows), op0=ALU.mult,
                    in1=raw[0:rows, 1:513], op1=ALU.add))

            sh = []
            for b in range(2):
                sh.append(dict(txw=k.sb([64, 512], F32, "txw"), xas=k.sb([64, 512], F32, "xas"),
                               sg0=k.sb([128, 512], F32, "sg0"), sg1=k.sb([32, 512], F32, "sg1")))
            names32 = ["rs", "ks", "vs", "sig", "cum", "gi", "ginv", "ge", "a", "kap", "kmod",
                       "kt", "rt", "bv", "T1", "T2"]
            names16 = ["kt16", "rt16", "vs16", "bt", "ktl",
                       "Nm", "NTm", "Mkk", "Mbr", "Mkr", "X0", "X1", "P0", "P1", "PT0", "PT1",
                       "Btm", "Ktm", "Vtm"]
            id16 = k.sb([128, 64], BF16, "id16")
            k.op("dve", [crw], [id16], lambda e: e.tensor_copy(out=id16[:, :], in_=crw[:, 3, 0:64]))
            ch = []
            for b in range(2):
                C = {}
                for nm in names32:
                    C[nm] = k.sb([128, 512], F32, nm)
                for nm in names16:
                    C[nm] = k.sb([128, 512], BF16, nm)
                C["Yq"] = C["kmod"]
                C["nP"] = [k.sb([128, 64], BF16, "nP") for _ in range(2)]
                C["UT"] = [k.sb([128, 64], BF16, "UT") for _ in range(2)]
                C["Sg"] = [k.sb([128, 64], F32, "Sg") for _ in range(2)]
                C["obf"] = [k.sb([128, 512], BF16, "obf") for _ in range(2)]
                C["pA"] = k.ps("pA")
                C["pB"] = k.ps("pB")
                C["pS"] = k.ps("pS")
                C["pY"] = k.ps("pY")
                C["no"] = 0
                ch.append(C)

            def mm(ps, out_ap, lhsT, rhs, reads, start, stop, inc):
                k.op("pe", reads, [ps], lambda e: e.matmul(out_ap, lhsT=lhsT, rhs=rhs, start=start, stop=stop), inc=inc)

            def dve(reads, writes, fn):
                k.op("dve", reads, writes, fn)

            def pool_(reads, writes, fn):
                k.op("pool", reads, writes, fn)

            def act(reads, writes, fn):
                k.op("act", reads, writes, fn)

            def unit_prep(q, hp, b, C, S):
                hc = slice(hp * 128, (hp + 1) * 128)
                pA, pB = C["pA"], C["pB"]
                rs, ks, vs = C["rs"], C["ks"], C["vs"]
                T1, T2 = C["T1"], C["T2"]
                load_shift(rs, 128, hp * 128, lambda r: par[0:r, hp, 0:1], b, q, par)
                load_shift(ks, 128, 1024 + hp * 128, lambda r: par[0:r, hp, 1:2], b, q, par)
                load_shift(vs, 128, 2048 + hp * 128, lambda r: par[0:r, hp, 2:3], b, q, par)
                yield
                sig, cum, gi, ginv, ge, a = C["sig"], C["cum"], C["gi"], C["ginv"], C["ge"], C["a"]
                kap, kmod, kt, bt, ktl, rt, bv = C["kap"], C["kmod"], C["kt"], C["bt"], C["ktl"], C["rt"], C["bv"]
                mm(pA, pA[:, :], wup[:, hc], S["txw"][:, :], [wup, S["txw"]], True, True, True)
                act([pA, par], [sig], lambda e: e.activation(out=sig[:, :], in_=pA[:, :], func=AF.Sigmoid,
                                                             bias=par[:, hp, 3:4]))
                dve([sig, crw], [cum], lambda e: e.tensor_tensor_scan(
                    out=cum[:, :], data0=crw[:, RST, :], data1=sig[:, :], initial=0.0, op0=ALU.mult, op1=ALU.add))
                act([cum], [gi], lambda e: e.activation(out=gi[:, :], in_=cum[:, :], func=AF.Exp, scale=-C0))
                act([cum], [ginv], lambda e: e.activation(out=ginv[:, :], in_=cum[:, :], func=AF.Exp, scale=C0))
                dve([cum, sig], [T1], lambda e: e.tensor_tensor(out=T1[:, :], in0=cum[:, :], in1=sig[:, :], op=ALU.subtract))
                act([T1], [ge], lambda e: e.activation(out=ge[:, :], in_=T1[:, :], func=AF.Exp, scale=-C0))
                yield
                mm(pB, pB[:, :], aup[:, hc], S["xas"][:, :], [aup, S["xas"]], True, True, True)
                act([pB, par], [a], lambda e: e.activation(out=a[:, :], in_=pB[:, :], func=AF.Sigmoid,
                                                           bias=par[:, hp, 4:5]))
                yield
                dve([ks, par], [T1], lambda e: e.tensor_scalar(out=T1[:, :], in0=ks[:, :], scalar1=par[:, hp, 5:6],
                                                               scalar2=None, op0=ALU.mult))
                dve([T1], [T2], lambda e: e.tensor_tensor(out=T2[:, :], in0=T1[:, :], in1=T1[:, :], op=ALU.mult))
                mm(pA, pA[:, :], bd[:, :], T2[:, :], [bd, T2], True, True, True)
                act([pA], [T2], lambda e: e.activation(out=T2[:, :], in_=pA[:, :], func=AF.Sqrt))
                dve([T2], [T2], lambda e: e.tensor_scalar_max(out=T2[:, :], in0=T2[:, :], scalar1=1e-12))
                dve([T2], [T2], lambda e: e.reciprocal(out=T2[:, :], in_=T2[:, :]))
                dve([T1, T2], [kap], lambda e: e.tensor_tensor(out=kap[:, :], in0=T1[:, :], in1=T2[:, :], op=ALU.mult))
                yield
                dve([a, par], [T1], lambda e: e.tensor_scalar(out=T1[:, :], in0=a[:, :], scalar1=par[:, hp, 6:7],
                                                              scalar2=par[:, hp, 10:11], op0=ALU.mult, op1=ALU.add))
                dve([ks, T1], [kmod], lambda e: e.tensor_tensor(out=kmod[:, :], in0=ks[:, :], in1=T1[:, :], op=ALU.mult))
                pool_([kap, ge], [kt], lambda e: e.tensor_tensor(out=kt[:, :], in0=kap[:, :], in1=ge[:, :], op=ALU.mult))
                kt16, rt16, vs16 = C["kt16"], C["rt16"], C["vs16"]
                k.op("pool", [kt], [kt16], lambda e: e.tensor_copy(out=kt16[:, :], in_=kt[:, :]))
                k.op("pool", [vs], [vs16], lambda e: e.tensor_copy(out=vs16[:, :], in_=vs[:, :]))
                pool_([kap, a], [T1], lambda e: e.tensor_tensor(out=T1[:, :], in0=kap[:, :], in1=a[:, :], op=ALU.mult))
                dve([T1, ginv], [bt], lambda e: e.tensor_tensor(out=bt[:, :], in0=T1[:, :], in1=ginv[:, :], op=ALU.mult))
                pool_([kmod, ginv], [ktl], lambda e: e.tensor_tensor(out=ktl[:, :], in0=kmod[:, :], in1=ginv[:, :], op=ALU.mult))
                pool_([rs, gi], [rt], lambda e: e.tensor_tensor(out=rt[:, :], in0=rs[:, :], in1=gi[:, :], op=ALU.mult))
                k.op("pool", [rt], [rt16], lambda e: e.tensor_copy(out=rt16[:, :], in_=rt[:, :]))
                yield
                dve([rs, kmod, par], [T1], lambda e: e.scalar_tensor_tensor(
                    out=T1[:, :], in0=rs[:, :], scalar=par[:, hp, 7:8], op0=ALU.mult, in1=kmod[:, :], op1=ALU.mult))
                mm(pB, pB[:, :], bd[:, :], T1[:, :], [bd, T1], True, True, True)
                dve([pB, vs], [bv], lambda e: e.tensor_tensor(out=bv[:, :], in0=pB[:, :], in1=vs[:, :], op=ALU.mult))
                yield
                specs = [("Nm", bt, kt16, SU, pA), ("NTm", kt16, bt, SL, pB), ("Mkk", ktl, kt16, SU, pA),
                         ("Mbr", bt, rt16, IU, pB), ("Mkr", ktl, rt16, IU, pA)]
                for nm, Lt, Rt, mk, ps in specs:
                    dst = C[nm]
                    for c in range(8):
                        cs = slice(c * 64, (c + 1) * 64)
                        for hh, pr in enumerate(HH):
                            mm(ps, ps[pr, cs], Lt[pr, cs], Rt[pr, cs], [Lt, Rt], True, True, c == 7 and hh == 1)
                    dve([ps, crw], [dst], lambda e, ps=ps, dst=dst, mk=mk: e.tensor_tensor(
                        out=dst[:, :], in0=ps[:, :], in1=crw[:, mk, :], op=ALU.mult))
                    yield
                X = C["X0"]
                dve([crw, C["Nm"]], [X], lambda e: e.tensor_tensor(out=X[:, :], in0=crw[:, IDT, :], in1=C["Nm"][:, :],
                                                                   op=ALU.subtract))
                P, PT = C["Nm"], C["NTm"]
                Pb = [C["P0"], C["P1"]]
                PTb = [C["PT0"], C["PT1"]]
                Xb = [C["X1"], C["X0"]]
                for j in range(1, 6):
                    Pn, PTn, Xn = Pb[j % 2], PTb[j % 2], Xb[(j - 1) % 2]
                    if j < 5:
                        for c in range(8):
                            cs = slice(c * 64, (c + 1) * 64)
                            for hh, pr in enumerate(HH):
                                mm(pA, pA[pr, cs], PT[pr, cs], P[pr, cs], [PT, P], True, True, c == 7 and hh == 1)
                    for c in range(8):
                        cs = slice(c * 64, (c + 1) * 64)
                        for hh, pr in enumerate(HH):
                            mm(pB, pB[pr, cs], P[pr, cs], PT[pr, cs], [PT, P], True, True, c == 7 and hh == 1)
                    if j < 5:
                        act([pA], [Pn], lambda e, Pn=Pn: e.activation(out=Pn[:, :], in_=pA[:, :], func=AF.Copy))
                    act([pB], [PTn], lambda e, PTn=PTn: e.activation(out=PTn[:, :], in_=pB[:, :], func=AF.Copy))
                    yield
                    for c in range(8):
                        cs = slice(c * 64, (c + 1) * 64)
                        for hh, pr in enumerate(HH):
                            mm(pA, pA[pr, cs], PTn[pr, cs], X[pr, cs], [PTn, X], True, True, c == 7 and hh == 1)
                    dve([pA, X], [Xn], lambda e, X=X, Xn=Xn: e.tensor_tensor(
                        out=Xn[:, :], in0=pA[:, :], in1=X[:, :], op=ALU.add))
                    P, PT, X = Pn, PTn, Xn
                    yield
                C["X"] = X
                for src, dn, ps in ((bt, "Btm", pA), (ktl, "Ktm", pB), (vs16, "Vtm", pA)):
                    dst = C[dn]
                    for c in range(8):
                        cs = slice(c * 64, (c + 1) * 64)
                        for hh, pr in enumerate(HH):
                            mm(ps, ps[pr, cs], src[pr, cs], id16[pr, :], [src, id16], True, True,
                               c == 7 and hh == 1)
                    if dn != "Vtm":
                        act([ps], [dst], lambda e, ps=ps, dst=dst: e.activation(out=dst[:, :], in_=ps[:, :], func=AF.Copy))
                    else:
                        dve([ps], [dst], lambda e, ps=ps, dst=dst: e.tensor_copy(out=dst[:, :], in_=ps[:, :]))
                    yield

            def chunk_a(c, hp, b, C):
                cs = slice(c * 64, (c + 1) * 64)
                pS = C["pS"]
                kt, Mkk, Vtm = C["kt"], C["Mkk"], C["Vtm"]
                si = b * 8 + hp
                nP = C["nP"][c % 2]
                for hh, pr in enumerate(HH):
                    mm(pS, pS[pr, 0:64], kt[pr, cs], Sall[pr, si, :], [kt, Sall], True, False, False)
                    mm(pS, pS[pr, 0:64], Mkk[pr, cs], Vtm[pr, cs], [Mkk, Vtm], False, True, hh == 1)
                act([pS], [nP], lambda e: e.activation(out=nP[:, :], in_=pS[:, 0:64], func=AF.Copy, scale=-1.0))

            def chunk_b(c, hp, b, C):
                cs = slice(c * 64, (c + 1) * 64)
                pS, X = C["pS"], C["X"]
                nP, UT = C["nP"][c % 2], C["UT"][c % 2]
                for hh, pr in enumerate(HH):
                    mm(pS, pS[pr, 64:128], X[pr, cs], nP[pr, :], [X, nP], True, True, hh == 1)
                act([pS], [UT], lambda e: e.activation(out=UT[:, :], in_=pS[:, 64:128], func=AF.Copy))

            def chunk_c(c, hp, b, C):
                cs = slice(c * 64, (c + 1) * 64)
                pS, pY = C["pS"], C["pY"]
                rt, Mbr, Mkr, Btm, Ktm, Vtm, gi = C["rt"], C["Mbr"], C["Mkr"], C["Btm"], C["Ktm"], C["Vtm"], C["gi"]
                si = b * 8 + hp
                UT, Sg = C["UT"][c % 2], C["Sg"][c % 2]
                gcol = gi[:, c * 64 + 63:c * 64 + 64]
                for hh, pr in enumerate(HH):
                    mm(pY, pY[pr, cs], Sall[pr, si, :], rt[pr, cs], [Sall, rt], True, False, False)
                    mm(pY, pY[pr, cs], UT[pr, :], Mbr[pr, cs], [UT, Mbr], False, False, False)
                    mm(pY, pY[pr, cs], Vtm[pr, cs], Mkr[pr, cs], [Vtm, Mkr], False, True, hh == 1)
                for hh, pr in enumerate(HH):
                    mm(pS, pS[pr, 128:192], Btm[pr, cs], UT[pr, :], [Btm, UT], True, False, False)
                    mm(pS, pS[pr, 128:192], Ktm[pr, cs], Vtm[pr, cs], [Ktm, Vtm], False, True, hh == 1)
                dve([Sall, gi], [Sg], lambda e: e.tensor_scalar(out=Sg[:, :], in0=Sall[:, si, :], scalar1=gcol,
                                                                scalar2=None, op0=ALU.mult))
                dve([pS, Sg, gi], [Sall], lambda e: e.scalar_tensor_tensor(
                    out=Sall[:, si, :], in0=pS[:, 128:192], scalar=gcol, op0=ALU.mult, in1=Sg[:, :], op1=ALU.add))

            def unit_post(q, hp, b, C, S):
                hc = slice(hp * 128, (hp + 1) * 128)
                pA, pB, pY = C["pA"], C["pB"], C["pY"]
                Yq, T1, T2, bv = C["Yq"], C["T1"], C["T2"], C["bv"]
                act([pY], [Yq], lambda e: e.activation(out=Yq[:, :], in_=pY[:, :], func=AF.Copy))
                mm(pA, pA[:, :], bd[:, :], Yq[:, :], [bd, Yq], True, True, True)
                dve([pA, Yq], [T1], lambda e: e.scalar_tensor_tensor(
                    out=T1[:, :], in0=pA[:, :], scalar=-1.0 / 64, op0=ALU.mult, in1=Yq[:, :], op1=ALU.add))
                dve([T1], [T2], lambda e: e.tensor_tensor(out=T2[:, :], in0=T1[:, :], in1=T1[:, :], op=ALU.mult))
                mm(pB, pB[:, :], bd[:, :], T2[:, :], [bd, T2], True, True, True)
                act([pB, eps_t], [T2], lambda e: e.activation(out=T2[:, :], in_=pB[:, :], func=AF.Sqrt,
                                                              scale=1.0 / 64, bias=eps_t[:, 2:3]))
                yield
                dve([T2], [T2], lambda e: e.reciprocal(out=T2[:, :], in_=T2[:, :]))
                dve([T1, T2], [T1], lambda e: e.tensor_tensor(out=T1[:, :], in0=T1[:, :], in1=T2[:, :], op=ALU.mult))
                dve([T1, par], [T1], lambda e: e.tensor_scalar(out=T1[:, :], in0=T1[:, :], scalar1=par[:, hp, 8:9],
                                                               scalar2=par[:, hp, 9:10], op0=ALU.mult, op1=ALU.add))
                dve([T1, bv], [T1], lambda e: e.tensor_tensor(out=T1[:, :], in0=T1[:, :], in1=bv[:, :], op=ALU.add))
                mm(pA, pA[:, :], g0[:, hc], S["sg0"][:, :], [g0, S["sg0"]], True, False, False)
                mm(pA, pA[:, :], g1[:, hc], S["sg1"][:, :], [g1, S["sg1"]], False, True, True)
                ob = C["obf"][C["no"] % 2]; C["no"] += 1
                dve([T1, pA], [ob], lambda e: e.tensor_tensor(out=ob[:, :], in0=T1[:, :], in1=pA[:, :], op=ALU.mult))
                t0 = b * SEQ + q * 512
                k.dma("pool", oT[1024 + hp * 128:1024 + (hp + 1) * 128, t0:t0 + 512], ob[:, :], reads=[ob])

            def chain(b):
                C, S = ch[b], sh[b]
                for q in range(nq):
                    load_shift(S["txw"], 64, 3072, lambda r: mux[0:r, 0:1], b, q, mux)
                    act([S["txw"]], [S["txw"]], lambda e: e.activation(out=S["txw"][:, :], in_=S["txw"][:, :], func=AF.Tanh))
                    load_shift(S["xas"], 64, 3136, lambda r: mux[0:r, 1:2], b, q, mux)
                    load_shift(S["sg0"], 128, 3200, lambda r: mux[0:r, 2:3], b, q, mux)
                    act([S["sg0"]], [S["sg0"]], lambda e: e.activation(out=S["sg0"][:, :], in_=S["sg0"][:, :], func=AF.Sigmoid))
                    load_shift(S["sg1"], 32, 3328, lambda r: mux[0:r, 3:4], b, q, mux)
                    act([S["sg1"]], [S["sg1"]], lambda e: e.activation(out=S["sg1"][:, :], in_=S["sg1"][:, :], func=AF.Sigmoid))
                    yield
                    for hp in range(8):
                        yield from unit_prep(q, hp, b, C, S)
                        for c in range(8):
                            chunk_a(c, hp, b, C)
                            yield
                            chunk_b(c, hp, b, C)
                            yield
                            chunk_c(c, hp, b, C)
                            yield
                        yield from unit_post(q, hp, b, C, S)

            streams = [chain(b) for b in range(nseq)]
            live = [True] * nseq
            for _ in range(30):
                next(streams[0])
            while any(live):
                for b in range(nseq):
                    if live[b]:
                        try:
                            next(streams[b])
                        except StopIteration:
                            live[b] = False

    def phase_eout(self, l):
        self.wait_cast(l, "a2")
        k = self.k
        wbf = self.dr["wbf%d" % l]
        oo = self.off["eout%d" % l]
        oT = self.dr["oT"]
        with k.phase() as ph:
            xbs = [k.sb([128, NC_D, TB], F32, "xb") for _ in range(2)]
            obs = [k.sb([128, NC_D, TB], BF16, "ob") for _ in range(2)]
            wA = [k.sb([128, 2, 2048], BF16, "wA") for _ in range(3)]
            ps_a = [k.ps("a") for _ in range(2)]
            na = 0
            xsrc = self.dr["xin"] if l == 0 else None

            def ld(blk):
                cols = slice(blk * TB, (blk + 1) * TB)
                self.load_x(xbs[blk % 2], blk, src=xsrc)
                for g4 in range(4):
                    k.dma("sp", obs[blk % 2][:, g4 * 4:(g4 + 1) * 4, :], oT[g4 * 512:(g4 + 1) * 512, cols].rearrange(
                        "(c p) t -> p c t", p=128), writes=[obs[blk % 2]])
            ld(0)
            for blk in range(self.nblk):
                xb, ob = xbs[blk % 2], obs[blk % 2]
                if blk + 1 < self.nblk:
                    ld(blk + 1)
                for cp in range(8):
                    wt = wA[na % 3]; na += 1
                    k.dma("sp", wt[:, :, :], wview(wbf, oo + cp * 256, 2048, 2), writes=[wt])
                    for jj in range(2):
                        c = cp * 2 + jj
                        pa = ps_a[c % 2]
                        for cc in range(NC_D):
                            k.op("pe", [wt, ob], [pa], lambda e, cc=cc, jj=jj, wt=wt, pa=pa: e.matmul(
                                pa[:, :], lhsT=wt[:, jj, cc * 128:(cc + 1) * 128], rhs=ob[:, cc, :],
                                start=(cc == 0), stop=(cc == NC_D - 1)), inc=(cc == NC_D - 1))
                        k.op("dve", [pa, xb], [xb], lambda e, c=c, pa=pa: e.tensor_tensor(
                            out=xb[:, c, :], in0=xb[:, c, :], in1=pa[:, :], op=ALU.add))
                self.store_x(xb, blk)

    def phase_zero_orw(self):
        k = self.k
        with k.phase():
            z = k.sb([128, 8, TB], BF16, "z")
            k.op("dve", [], [z], lambda e: e.memset(z[:, :, :], 0.0))
            for blk in range(self.nblk):
                k.dma("sp", self.dr["oT"][1024:2048, blk * TB:(blk + 1) * TB].rearrange(
                    "(c p) t -> p c t", p=128), z[:, :, :], reads=[z])

    def phase_final(self):
        k = self.k
        with k.phase() as ph:
            ones32, gains, eps_t = self.consts(ph)
            xbs = [k.sb([128, NC_D, TB], F32, "xb") for _ in range(2)]
            obs = [k.sb([128, NC_D, TB], F32, "ob") for _ in range(2)]
            sqs = [k.sb([128, TB], BF16, "sq") for _ in range(2)]
            rstd = k.sb([128, TB], F32, "rstd")
            tmp = k.sb([128, TB], F32, "tmp")
            ps_ss = k.ps("ss")
            g = _GV(gains, 8 * NC_D)
            for blk in range(self.nblk):
                xb = xbs[blk % 2]
                ob = obs[blk % 2]
                self.load_x(xb, blk)
                self.rms(xb, ob, g, ps_ss, sqs, rstd, tmp, self.onesb, eps_t)
                self.store_x(ob, blk, dst=self.dr["outT"])


class _GV:
    def __init__(self, base, off):
        self.base = base
        self.off = off
        self.t = base.t

    @property
    def w(self):
        return self.base.w

    @w.setter
    def w(self, v):
        self.base.w = v

    @property
    def r(self):
        return self.base.r

    @r.setter
    def r(self, v):
        self.base.r = v

    def __getitem__(self, idx):
        p, c = idx
        if isinstance(c, slice):
            c = slice(c.start + self.off, c.stop + self.off)
        else:
            c = c + self.off
        return self.t[p, c]


def host_consts():
    c = {}
    c["c_ones32"] = np.ones((128, 128), np.float32)
    sb = np.zeros((128, 6, 512), np.float32)
    jj = np.arange(128)[:, None]
    ss = np.arange(128)[None, :]
    sb[:, 0, :128] = (jj >= ss)
    sb[:, 1, :128] = 1.0
    tq = np.arange(512)[None, :]
    for i in range(4):
        sb[:, 2 + i, :] = (tq > jj + 128 * i)
    c["c_sb"] = sb
    rw = np.zeros((128, 5, 512), np.float32)
    s_ = (np.arange(128) % 64)[:, None]
    t_ = np.arange(64)[None, :]
    rw[:, 0, :] = np.tile((s_ < t_), (1, 8))
    rw[:, 1, :] = np.tile((s_ > t_), (1, 8))
    rw[:, 2, :] = np.tile((s_ <= t_), (1, 8))
    rw[:, 3, :] = np.tile((s_ == t_), (1, 8))
    rw[:, 4, :] = (np.arange(512) % 64 != 0)[None, :]
    c["c_rw"] = rw
    bdm = np.zeros((128, 128), np.float32)
    bdm[:64, :64] = 1.0
    bdm[64:, 64:] = 1.0
    c["c_bd"] = bdm
    c["c_eps"] = np.tile(np.array([[RMS_EPS, LN_EPS, LNX_EPS, 1.0]], np.float32), (128, 1))
    return c


def build(plan, nblk=NBLK, debug_out=()):
    nc = bass.Bass("TRN2", target_bir_lowering=False)
    off, wrows = wpack_layout()
    dr = {}
    dr["xin"] = nc.dram_tensor("xin", [D, NT], F32, kind="ExternalInput").ap()
    for l in range(DEPTH):
        dr["wflat%d" % l] = nc.dram_tensor("wflat%d" % l, [wrows[l], 2048], F32, kind="ExternalInput").ap()
        dr["wbf%d" % l] = nc.dram_tensor("wbf%d" % l, [wrows[l], 2048], BF16, kind="Internal").ap()
    dr["gains"] = nc.dram_tensor("gains", [128, 9 * NC_D], F32, kind="ExternalInput").ap()
    for name, arr in host_consts().items():
        dr[name] = nc.dram_tensor(name, list(arr.shape), F32, kind="ExternalInput").ap()
    dr["odd_lng"] = nc.dram_tensor("odd_lng", [2, 128, D], F32, kind="ExternalInput").ap()
    dr["odd_lnb"] = nc.dram_tensor("odd_lnb", [2, 128, D], F32, kind="ExternalInput").ap()
    dr["odd_wsT"] = nc.dram_tensor("odd_wsT", [2, 128, 8, 128], F32, kind="ExternalInput").ap()
    dr["odd_bs"] = nc.dram_tensor("odd_bs", [2, 1, 1024], F32, kind="ExternalInput").ap()
    def _scr(name, shape, dt):
        return nc.dram_tensor(name, shape, dt, kind=("ExternalOutput" if name in debug_out else "Internal")).ap()
    dr["qkT"] = _scr("qkT", [2048, NT], BF16)
    dr["vtok"] = _scr("vtok", [NT, 1024], BF16)
    dr["zrwT"] = _scr("zrwT", [27 * 128, NT], F32)
    dr["oT"] = _scr("oT", [2048, NT], BF16)
    dr["rw_par"] = nc.dram_tensor("rw_par", [2, 128, 8, 12], F32, kind="ExternalInput").ap()
    dr["rw_mux"] = nc.dram_tensor("rw_mux", [2, 128, 4], F32, kind="ExternalInput").ap()
    dr["rw_wup"] = nc.dram_tensor("rw_wup", [2, 64, 1024], F32, kind="ExternalInput").ap()
    dr["rw_aup"] = nc.dram_tensor("rw_aup", [2, 64, 1024], F32, kind="ExternalInput").ap()
    dr["rw_gup"] = nc.dram_tensor("rw_gup", [2, 160, 1024], F32, kind="ExternalInput").ap()
    dr["outT"] = nc.dram_tensor("outT", [D, NT], F32, kind="ExternalOutput").ap()
    dr["xT"] = dr["xin"] if False else nc.dram_tensor("xT", [D, NT], F32, kind=("ExternalOutput" if "xT" in debug_out else "Internal")).ap()
    k = K(nc)
    p = Prog(nc, k, dr)
    p.off = off
    p.wrows = wrows
    p.nblk = nblk
    for step in plan:
        if step[0] == "castbg":
            p.phase_cast(step[1], step[2], bg=True)
        elif step[0] == "cast":
            p.phase_cast(step[1], step[2])
        elif step[0] == "copyx":
            p.phase_copyx()
        elif step[0] == "ffn":
            p.phase_ffn(step[1])
        elif step[0] == "odd":
            p.phase_odd(step[1])
        elif step[0] == "e1":
            p.phase_e1(step[1])
        elif step[0] == "sb":
            p.phase_sb(step[1])
        elif step[0] == "eout":
            p.phase_eout(step[1])
        elif step[0] == "zero_orw":
            p.phase_zero_orw()
        elif step[0] == "rw":
            p.phase_rw(step[1])
        elif step[0] == "final":
            p.phase_final()
        else:
            raise ValueError(step)
    return nc


def full_plan():
    def cb(l):
        return [("castbg", l, "a1"), ("castbg", l, "a2"), ("castbg", l, "b")]
    plan = [("cast", 0, "a1"), ("castbg", 0, "a2")]
    plan += [("e1", 0), ("castbg", 0, "b")] + cb(1) + [("sb", 0)] + cb(2) + [("rw", 0), ("eout", 0), ("ffn", 0)]
    plan += [("odd", 1), ("ffn", 1)]
    plan += [("e1", 2)] + cb(3) + [("sb", 2), ("rw", 2), ("eout", 2), ("ffn", 2)]
    plan += [("odd", 3), ("ffn", 3)]
    plan += [("final",)]
    return plan


def host_inputs(inp):
    x = np.asarray(inp["x"], np.float32)
    gains = np.concatenate([np.asarray(inp["norm_mix"], np.float32),
                            np.asarray(inp["norm_ffn"], np.float32),
                            np.asarray(inp["norm_final"], np.float32)[None]], axis=0)
    gains = np.ascontiguousarray(gains.reshape(9, NC_D, 128).transpose(2, 0, 1).reshape(128, 9 * NC_D))
    wflat = pack_weights({k_: np.asarray(v, np.float32) for k_, v in inp.items()})
    common = {"gains": gains}
    f = lambda a: np.asarray(a, np.float32)
    par = np.zeros((2, 128, 8, 12), np.float32)
    mu = f(inp["even_mu"])
    def hd(a):
        return a.reshape(2, 8, 128).transpose(0, 2, 1)
    par[:, :, :, 0] = hd(mu[:, 0:1024]); par[:, :, :, 1] = hd(mu[:, 1024:2048]); par[:, :, :, 2] = hd(mu[:, 2048:3072])
    par[:, :, :, 3] = hd(f(inp["even_w0"])); par[:, :, :, 4] = hd(f(inp["even_a0"]))
    par[:, :, :, 5] = hd(f(inp["even_k_k"])); par[:, :, :, 6] = hd(f(inp["even_k_a"]))
    par[:, :, :, 7] = hd(f(inp["even_r_k"]).reshape(2, 1024))
    par[:, :, :, 8] = hd(f(inp["even_lnx_w"])); par[:, :, :, 9] = hd(f(inp["even_lnx_b"]))
    common["rw_par"] = par
    mux = np.zeros((2, 128, 4), np.float32)
    mux[:, :64, 0] = mu[:, 3072:3136]; mux[:, :64, 1] = mu[:, 3136:3200]
    mux[:, :, 2] = mu[:, 3200:3328]; mux[:, :32, 3] = mu[:, 3328:3360]
    common["rw_mux"] = mux
    common["rw_wup"] = f(inp["even_w_up"]); common["rw_aup"] = f(inp["even_a_up"]); common["rw_gup"] = f(inp["even_g_up"])
    common["odd_lng"] = np.ascontiguousarray(np.broadcast_to(f(inp["odd_ln_g"])[:, None, :], (2, 128, D)))
    common["odd_lnb"] = np.ascontiguousarray(np.broadcast_to(f(inp["odd_ln_b"])[:, None, :], (2, 128, D)))
    common["odd_wsT"] = np.ascontiguousarray(f(inp["odd_w_s"]).transpose(0, 3, 1, 2))
    common["odd_bs"] = np.ascontiguousarray(f(inp["odd_b_s"]).reshape(2, 1, 1024))
    for l in range(DEPTH):
        common["wflat%d" % l] = wflat[l]
    common.update(host_consts())
    maps = []
    for c in range(8):
        xs = x[c * NB_CORE:(c + 1) * NB_CORE].reshape(NT, D)
        m = dict(common)
        m["xin"] = np.ascontiguousarray(xs.T)
        maps.append(m)
    return maps


def kernel(**inputs):
    maps = host_inputs(inputs)
    nc = build(full_plan())
    res = run_bass_kernel_spmd(nc, maps, core_ids=list(range(8)))
    out = np.empty((8 * NB_CORE, SEQ, D), np.float32)
    for c in range(8):
        oT = np.asarray(res.results[c]["outT"], np.float32)
        out[c * NB_CORE:(c + 1) * NB_CORE] = oT.T.reshape(NB_CORE, SEQ, D)
    return out
```

```python
import contextlib
import numpy as np
import concourse.bass as bass
import concourse.mybir as mybir
from concourse.bass_utils import run_bass_kernel_spmd

F32 = mybir.dt.float32
BF16 = mybir.dt.bfloat16
AF = mybir.ActivationFunctionType
ALU = mybir.AluOpType

D = 2048
SEQ = 2048
NB_CORE = 2
NT = NB_CORE * SEQ
TB = 512
NBLK = NT // TB
DEPTH = 4
HID = 8192
NC_D = D // 128
NJ = HID // 128
RWW = 3360
EIN = 6432
RMS_EPS = 1e-6
LN_EPS = 1e-5
LNX_EPS = 64e-5
C0 = float(np.exp(-0.5))


class Sem:
    def __init__(self, h):
        self.h = h
        self.n = 0


class Tile:
    def __init__(self, t, name):
        self.t = t
        self.name = name
        self.w = []
        self.r = []
        self.dsem = None

    def __getitem__(self, idx):
        return self.t[idx]


class Eng:
    def __init__(self, name, e, sem):
        self.name = name
        self.e = e
        self.sem = sem
        self.seen = {}


class K:
    def __init__(self, nc):
        self.nc = nc
        self.engs = {}
        for name, e in (("pe", nc.tensor), ("act", nc.scalar), ("dve", nc.vector),
                        ("pool", nc.gpsimd), ("sp", nc.sync)):
            self.engs[name] = Eng(name, e, Sem(nc.alloc_semaphore("p_" + name)))
        self.free_dsems = [Sem(nc.alloc_semaphore("d%d" % i)) for i in range(80)]
        self.live_dsems = []
        self.stack = None
        self.tiles = []
        self.uid = 0

    @contextlib.contextmanager
    def phase(self):
        self.stack = contextlib.ExitStack()
        self.tiles = []
        try:
            yield self
        finally:
            pass
        self.barrier()
        self.stack.close()
        self.stack = None
        self.free_dsems.extend(self.live_dsems)
        self.live_dsems = []
        self.tiles = []

    def sb(self, shape, dtype, name=None):
        self.uid += 1
        name = "%s_%d" % (name or "t", self.uid)
        t = self.stack.enter_context(self.nc.sbuf_tensor(name, list(shape), dtype))
        tl = Tile(t, name)
        self.tiles.append(tl)
        return tl

    def ps(self, name=None, dtype=F32, shape=(128, 512)):
        self.uid += 1
        name = "%s_%d" % (name or "ps", self.uid)
        t = self.stack.enter_context(self.nc.psum_tensor(name, list(shape), dtype))
        tl = Tile(t, name)
        self.tiles.append(tl)
        return tl

    def _wait(self, eng, toks):
        best = {}
        for (s, v) in toks:
            if v > best.get(id(s), (s, 0))[1]:
                best[id(s)] = (s, v)
        for (s, v) in best.values():
            if eng.seen.get(id(s), 0) >= v:
                continue
            eng.e.wait_ge(s.h, v)
            eng.seen[id(s)] = v

    def _deps(self, eng, reads, writes):
        toks = []
        for t in reads:
            for tk in t.w:
                if tk[0] is eng.sem and eng.name == "pe":
                    continue
                toks.append(tk)
        for t in writes:
            for tk in t.w + t.r:
                if tk[0] is eng.sem:
                    continue
                toks.append(tk)
        return toks

    @staticmethod
    def _note_read(t, tok):
        t.r = [x for x in t.r if x[0] is not tok[0]] + [tok]

    def op(self, en, reads, writes, fn, inc=True):
        eng = self.engs[en]
        self._wait(eng, self._deps(eng, reads, writes))
        ins = fn(eng.e)
        if inc:
            eng.sem.n += 1
            ins.then_inc(eng.sem.h, 1)
            tok = (eng.sem, eng.sem.n)
        else:
            tok = (eng.sem, eng.sem.n + 1)
        for t in writes:
            t.w = [tok]
            t.r = []
        for t in reads:
            if t not in writes:
                self._note_read(t, tok)
        return ins

    def _dsem(self, t):
        if t.dsem is None:
            t.dsem = self.free_dsems.pop()
            self.live_dsems.append(t.dsem)
        return t.dsem

    def dma(self, q, out, in_, reads=(), writes=(), owner=None):
        eng = self.engs[q]
        self._wait(eng, self._deps(eng, list(reads), list(writes)))
        owner = owner or (writes[0] if writes else reads[0])
        s = self._dsem(owner)
        s.n += 16
        eng.e.dma_start(out=out, in_=in_).then_inc(s.h, 16)
        tok = (s, s.n)
        for t in writes:
            t.w = [tok]
            t.r = []
        for t in reads:
            self._note_read(t, tok)
        return tok

    def barrier(self):
        toks = [(e.sem, e.sem.n) for e in self.engs.values()]
        toks += [(s, s.n) for s in self.live_dsems]
        for e in self.engs.values():
            self._wait(e, [tk for tk in toks if tk[0] is not e.sem and tk[1] > 0])
        for t in self.tiles:
            t.w = []
            t.r = []


def tile_w(W, bw):
    Kd, N = W.shape
    return np.ascontiguousarray(W.reshape(Kd // 128, 128, N // bw, bw).transpose(2, 1, 0, 3))


class WPack:
    def __init__(self):
        self.parts = []
        self.off = {}
        self.rows = 0

    def add(self, name, arr):
        a = np.ascontiguousarray(arr, dtype=np.float32).reshape(-1)
        assert a.size % 2048 == 0, (name, a.size)
        self.off[name] = self.rows
        self.parts.append(a.reshape(-1, 2048))
        self.rows += a.size // 2048

    def build(self):
        return np.concatenate(self.parts, axis=0)


def wpack_layout():
    off = {}
    rows = []
    for l in range(DEPTH):
        r = 0
        if l % 2 == 0:
            off["ein_qk%d" % l] = r; r += 16 * 128 * 16 * 128 // 2048
            off["ein_v%d" % l] = r; r += 2 * 128 * 16 * 512 // 2048
            off["ein_rw%d" % l] = r; r += 27 * 128 * 16 * 128 // 2048
            off["eout%d" % l] = r; r += 16 * 128 * 16 * 128 // 2048
        else:
            off["oin_u%d" % l] = r; r += 16 * 128 * 16 * 128 // 2048
            off["oin_v%d" % l] = r; r += 4 * 128 * 16 * 512 // 2048
            off["oout%d" % l] = r; r += 16 * 128 * 16 * 128 // 2048
        off["w1_%d" % l] = r; r += 64 * 128 * 16 * 128 // 2048
        off["w2_%d" % l] = r; r += 16 * 128 * 64 * 128 // 2048
        rows.append(r)
    return off, rows


def pack_weights(inp):
    off, rows = wpack_layout()
    out = []
    for l in range(DEPTH):
        wp = WPack()
        i = l // 2
        if l % 2 == 0:
            win = inp["even_w_in"][i]
            wp.add("ein_qk%d" % l, tile_w(win[:, :2048], 128))
            wp.add("ein_v%d" % l, tile_w(win[:, 2048:3072], 512))
            wrw = np.zeros((D, 27 * 128), np.float32)
            wrw[:, :RWW] = win[:, 3072:]
            wp.add("ein_rw%d" % l, tile_w(wrw, 128))
            wp.add("eout%d" % l, tile_w(inp["even_w_out"][i], 128))
        else:
            win = inp["odd_w_in"][i]
            wp.add("oin_u%d" % l, tile_w(win[:, :2048], 128))
            wp.add("oin_v%d" % l, tile_w(win[:, 2048:], 512))
            wp.add("oout%d" % l, tile_w(inp["odd_w_out"][i], 128))
        wp.add("w1_%d" % l, tile_w(inp["ffn_w1"][l], 128))
        w2t = tile_w(inp["ffn_w2"][l], 128).reshape(16, 128, 2, 32, 128).transpose(0, 2, 1, 3, 4)
        wp.add("w2_%d" % l, w2t)
        for k_, v in wp.off.items():
            assert off[k_] == v
        assert wp.rows == rows[l]
        out.append(wp.build())
    return out


def wview(wbf, row0, F, n=1):
    a = F // 2048
    v = wbf[row0:row0 + n * 128 * a, :]
    return v.rearrange("(n p a) c -> p n (a c)", n=n, p=128, a=a)


class Prog:
    def __init__(self, nc, k, dr):
        self.nc = nc
        self.k = k
        self.dr = dr

    def load_x(self, xb, blk, src=None):
        k = self.k
        xT = src if src is not None else self.dr["xT"]
        for g in range(4):
            src = xT[g * 512:(g + 1) * 512, blk * TB:(blk + 1) * TB].rearrange("(c p) t -> p c t", p=128)
            k.dma("sp", xb[:, g * 4:(g + 1) * 4, :], src, writes=[xb])

    def store_x(self, xb, blk, dst=None):
        k = self.k
        xT = dst if dst is not None else self.dr["xT"]
        for g in range(4):
            d = xT[g * 512:(g + 1) * 512, blk * TB:(blk + 1) * TB].rearrange("(c p) t -> p c t", p=128)
            k.dma("pool", d, xb[:, g * 4:(g + 1) * 4, :], reads=[xb])

    def rms(self, xb, hT, gcol, ps_ss, sqs, rstd, tmp, onesb, eps_t):
        k = self.k
        for c in range(NC_D):
            sq = sqs[c % len(sqs)]
            k.op("act", [xb], [sq], lambda e, c=c, sq=sq: e.activation(out=sq[:, :], in_=xb[:, c, :], func=AF.Square))
            k.op("pe", [sq, onesb], [ps_ss], lambda e, c=c, sq=sq: e.matmul(
                ps_ss[:, :], lhsT=onesb[:, :], rhs=sq[:, :], start=(c == 0), stop=(c == NC_D - 1)),
                inc=True)
        k.op("act", [ps_ss, eps_t], [tmp], lambda e: e.activation(
            out=tmp[:, :], in_=ps_ss[:, :], func=AF.Sqrt, scale=1.0 / D, bias=eps_t[:, 0:1]))
        k.op("dve", [tmp], [rstd], lambda e: e.reciprocal(out=rstd[:, :], in_=tmp[:, :]))
        for c in range(NC_D):
            k.op("dve", [xb, rstd, gcol], [hT], lambda e, c=c: e.scalar_tensor_tensor(
                out=hT[:, c, :], in0=xb[:, c, :], scalar=gcol[:, c:c + 1], op0=ALU.mult,
                in1=rstd[:, :], op1=ALU.mult))

    def ffn_stage1(self, l, hf, hT, hid, w1b, ps_h, rl, cnt):
        k = self.k
        wbf = self.dr["wbf%d" % l]
        o1 = self.off["w1_%d" % l]
        for jg in range(hf * 16, hf * 16 + 16):
            wt = w1b[cnt[0] % len(w1b)]; cnt[0] += 1
            k.dma("sp", wt[:, :, :], wview(wbf, o1 + jg * 2 * 128, 2048, 2), writes=[wt])
            for jj in range(2):
                j = jg * 2 + jj
                ph = ps_h[j % len(ps_h)]
                for c in range(NC_D):
                    k.op("pe", [wt, hT], [ph], lambda e, c=c, jj=jj, wt=wt, ph=ph: e.matmul(
                        ph[:, :], lhsT=wt[:, jj, c * 128:(c + 1) * 128], rhs=hT[:, c, :],
                        start=(c == 0), stop=(c == NC_D - 1)), inc=(c == NC_D - 1))
                r = rl[j % len(rl)]
                k.op("act", [ph], [r], lambda e, ph=ph, r=r: e.activation(out=r[:, :], in_=ph[:, :], func=AF.Relu))
                k.op("dve", [r], [hid], lambda e, j=j, r=r: e.tensor_tensor(
                    out=hid[:, j - hf * 32, :], in0=r[:, :], in1=r[:, :], op=ALU.mult))

    def ffn_stage2(self, l, hf, crange, xb, hid, w2b, ps_o, cnt):
        k = self.k
        wbf = self.dr["wbf%d" % l]
        o2 = self.off["w2_%d" % l]
        for c in crange:
            po = ps_o[c % len(ps_o)]
            wt = w2b[cnt[0] % len(w2b)]; cnt[0] += 1
            k.dma("sp", wt[:, :, :], wview(wbf, o2 + c * 512 + hf * 256, 4096, 1), writes=[wt])
            for jj in range(32):
                k.op("pe", [wt, hid], [po], lambda e, jj=jj, wt=wt, po=po: e.matmul(
                    po[:, :], lhsT=wt[:, 0, jj * 128:(jj + 1) * 128], rhs=hid[:, jj, :],
                    start=(jj == 0), stop=(jj == 31)), inc=(jj == 31))
            k.op("dve", [po, xb], [xb], lambda e, c=c, po=po: e.tensor_tensor(
                out=xb[:, c, :], in0=xb[:, c, :], in1=po[:, :], op=ALU.add))

    def phase_copyx(self):
        k = self.k
        with k.phase():
            dummy = Tile(None, "cpdummy")
            k.tiles.append(dummy)
            for g in range(8):
                k.dma("sp", self.dr["xT"][g * 256:(g + 1) * 256, :], self.dr["xin"][g * 256:(g + 1) * 256, :], writes=[dummy])

    def phase_cast(self, l, part, bg=False):
        k = self.k
        if not hasattr(self, "cast_tok"):
            self.cast_tok = {}
        with k.phase():
            CH = 1024
            wsrc = self.dr["wflat%d" % l]
            wbf = self.dr["wbf%d" % l]
            split = self.off["w1_%d" % l]
            s1 = self.off["ein_v%d" % l] if l % 2 == 0 else split
            r, r1 = {"a1": (0, s1), "a2": (s1, split), "b": (split, self.wrows[l])}[part]
            sem = k.free_dsems.pop()
            if not bg:
                k.live_dsems.append(sem)
            while r < r1:
                n = min(CH, r1 - r)
                sem.n += 16
                k.engs["pool"].e.dma_start(out=wbf[r:r + n, :], in_=wsrc[r:r + n, :]).then_inc(sem.h, 16)
                r += n
            self.cast_tok[(l, part)] = (sem, sem.n)

    def cast_queue(self, l, part):
        if not hasattr(self, "bg_jobs"):
            self.bg_jobs = []
            self.bg_sems = {}
        split = self.off["w1_%d" % l]
        s1 = self.off["ein_v%d" % l] if l % 2 == 0 else split
        r, r1 = {"a1": (0, s1), "a2": (s1, split), "b": (split, self.wrows[l])}[part]
        while r < r1:
            n = min(1024, r1 - r)
            self.bg_jobs.append((l, part, r, n))
            r += n

    def bg_pump(self, njobs, only=None):
        k = self.k
        if not hasattr(self, "cast_tok"):
            self.cast_tok = {}
        jobs = getattr(self, "bg_jobs", [])
        done = 0
        i = 0
        while i < len(jobs) and done < njobs:
            l, part, r, n = jobs[i]
            if only is not None and (l, part) not in only:
                i += 1
                continue
            jobs.pop(i)
            sem = self.bg_sems.get((l, part))
            if sem is None:
                sem = self.bg_sems[(l, part)] = k.free_dsems.pop()
            sem.n += 16
            k.engs["pool"].e.dma_start(out=self.dr["wbf%d" % l][r:r + n, :],
                                       in_=self.dr["wflat%d" % l][r:r + n, :]).then_inc(sem.h, 16)
            self.cast_tok[(l, part)] = (sem, sem.n)
            done += 1

    def wait_cast(self, l, part="a1"):
        need = [(l, pp) for pp in ("a1", "a2", "b") if pp <= part]
        self.bg_pump(10 ** 6, only=need)
        toks = [t for (ll, pp), t in getattr(self, "cast_tok", {}).items() if ll == l and pp <= part]
        for e in self.k.engs.values():
            self.k._wait(e, toks)

    def consts(self, ph):
        k = self.k
        ones32 = k.sb([128, 128], F32, "ones32")
        k.dma("sp", ones32[:, :], self.dr["c_ones32"], writes=[ones32])
        gains = k.sb([128, 9 * NC_D], F32, "gains")
        k.dma("sp", gains[:, :], self.dr["gains"], writes=[gains])
        eps_t = k.sb([128, 4], F32, "eps")
        k.dma("sp", eps_t[:, :], self.dr["c_eps"], writes=[eps_t])
        self.onesb = k.sb([128, 128], BF16, "onesb16")
        k.op("dve", [], [self.onesb], lambda e: e.memset(self.onesb[:, :], 1.0))
        return ones32, gains, eps_t

    def phase_ffn(self, l):
        self.wait_cast(l, "b")
        k = self.k
        with k.phase() as ph:
            ones32, gains, eps_t = self.consts(ph)
            xbs = [k.sb([128, NC_D, TB], F32, "xb") for _ in range(2)]
            hTs = [k.sb([128, NC_D, TB], BF16, "hT") for _ in range(2)]
            hid = k.sb([128, NJ // 2, TB], BF16, "hid")
            w1b = [k.sb([128, 2, 2048], BF16, "w1") for _ in range(3)]
            w2b = [k.sb([128, 1, 4096], BF16, "w2") for _ in range(3)]
            sqs = [k.sb([128, TB], BF16, "sq") for _ in range(2)]
            rl = [k.sb([128, TB], F32, "rl") for _ in range(2)]
            rstd = k.sb([128, TB], F32, "rstd")
            tmp = k.sb([128, TB], F32, "tmp")
            ps_ss = k.ps("ss")
            ps_h = [k.ps("h") for _ in range(2)]
            ps_o = [k.ps("o") for _ in range(2)]
            g = _GV(gains, (4 + l) * NC_D)
            c1, c2 = [0], [0]
            self.load_x(xbs[0], 0)
            self.rms(xbs[0], hTs[0], g, ps_ss, sqs, rstd, tmp, self.onesb, eps_t)
            for blk in range(self.nblk):
                xb, hT = xbs[blk % 2], hTs[blk % 2]
                nxt = blk + 1 < self.nblk
                if nxt:
                    self.load_x(xbs[(blk + 1) % 2], blk + 1)
                self.ffn_stage1(l, 0, hT, hid, w1b, ps_h, rl, c1)
                self.ffn_stage2(l, 0, range(NC_D), xb, hid, w2b, ps_o, c2)
                self.ffn_stage1(l, 1, hT, hid, w1b, ps_h, rl, c1)
                self.ffn_stage2(l, 1, range(0, 8), xb, hid, w2b, ps_o, c2)
                if nxt:
                    self.rms(xbs[(blk + 1) % 2], hTs[(blk + 1) % 2], g, ps_ss, sqs, rstd, tmp, self.onesb, eps_t)
                self.ffn_stage2(l, 1, range(8, NC_D), xb, hid, w2b, ps_o, c2)
                self.store_x(xb, blk)

    def phase_odd(self, l):
        self.wait_cast(l, "a2")
        k = self.k
        i = l // 2
        wbf = self.dr["wbf%d" % l]
        xT = self.dr["xT"]
        ou, ov, oo = self.off["oin_u%d" % l], self.off["oin_v%d" % l], self.off["oout%d" % l]
        with k.phase() as ph:
            ones32, gains, eps_t = self.consts(ph)
            xb = k.sb([128, NC_D, TB], F32, "xb")
            hTs = [k.sb([128, NC_D, TB], BF16, "hT") for _ in range(2)]
            uT = k.sb([128, NC_D, TB], BF16, "uT")
            vt = k.sb([128, 4, D], F32, "vt")
            vn = k.sb([128, 4, D], BF16, "vn")
            wA = [k.sb([128, 2, 2048], BF16, "wA") for _ in range(2)]
            wv = [k.sb([128, NC_D, 512], BF16, "wv") for _ in range(2)]
            lng = k.sb([128, D], F32, "lng")
            lnb = k.sb([128, D], F32, "lnb")
            wsb = k.sb([128, 8, 128], BF16, "wsb")
            bsb = k.sb([1, 1024], BF16, "bsb")
            onesb = self.onesb
            xr = [k.sb([128, TB], F32, "xr") for _ in range(2)]
            sqs = [k.sb([128, TB], BF16, "sq") for _ in range(2)]
            rstd = k.sb([128, TB], F32, "rstd")
            tmp = k.sb([128, TB], F32, "tmp")
            st = k.sb([128, 8], F32, "st")
            ps_ss = k.ps("ss")
            ps_a = [k.ps("a") for _ in range(2)]
            ps_v = [k.ps("v") for _ in range(2)]
            ps_m = [k.ps("m") for _ in range(2)]
            k.dma("sp", lng[:, :], self.dr["odd_lng"][i], writes=[lng])
            k.dma("sp", lnb[:, :], self.dr["odd_lnb"][i], writes=[lnb])
            k.dma("pool", wsb[:, :, :], self.dr["odd_wsT"][i], writes=[wsb])
            k.dma("pool", bsb[:, :], self.dr["odd_bs"][i], writes=[bsb])
            k.op("dve", [wsb], [wsb], lambda e: e.memset(wsb[64:128, :, 0:64], 0.0))
            g = _GV(gains, l * NC_D)
            na = 0
            nx = 0
            self.load_x(xb, 0)
            self.rms(xb, hTs[0], g, ps_ss, sqs, rstd, tmp, self.onesb, eps_t)
            for blk in range(self.nblk):
                cols = slice(blk * TB, (blk + 1) * TB)
                hT = hTs[blk % 2]
                nxt = blk + 1 < self.nblk
                for nb in range(4):
                    wt = wv[nb % 2]
                    k.dma("sp", wt[:, :, :], wview(wbf, ov + nb * 512, 8192, 1)[:, 0, :].rearrange(
                        "p (c m) -> p c m", c=NC_D), writes=[wt])
                    for tt in range(4):
                        pv = ps_v[(nb * 4 + tt) % 2]
                        for dc in range(NC_D):
                            k.op("pe", [wt, hT], [pv], lambda e, dc=dc, tt=tt, wt=wt, pv=pv: e.matmul(
                                pv[:, :], lhsT=hT[:, dc, tt * 128:(tt + 1) * 128], rhs=wt[:, dc, :],
                                start=(dc == 0), stop=(dc == NC_D - 1)), inc=(dc == NC_D - 1))
                        k.op("act", [pv], [vt], lambda e, nb=nb, tt=tt, pv=pv: e.activation(
                            out=vt[:, tt, nb * 512:(nb + 1) * 512], in_=pv[:, :], func=AF.Gelu))
                if nxt:
                    self.load_x(xb, blk + 1)
                for tt in range(4):
                    k.op("act", [vt], [vn, st], lambda e, tt=tt: e.activation(
                        out=vn[:, tt, :], in_=vt[:, tt, :], func=AF.Identity, accum_out=st[:, 0:1]))
                    k.op("dve", [st], [st], lambda e: e.tensor_scalar(
                        out=st[:, 1:2], in0=st[:, 0:1], scalar1=-1.0 / D, scalar2=None, op0=ALU.mult))
                    k.op("act", [vt, st], [vt], lambda e, tt=tt: e.activation(
                        out=vt[:, tt, :], in_=vt[:, tt, :], func=AF.Identity, bias=st[:, 1:2]))
                    k.op("act", [vt], [vn, st], lambda e, tt=tt: e.activation(
                        out=vn[:, tt, :], in_=vt[:, tt, :], func=AF.Square, accum_out=st[:, 2:3]))
                    k.op("act", [st, eps_t], [st], lambda e: e.activation(
                        out=st[:, 3:4], in_=st[:, 2:3], func=AF.Sqrt, scale=1.0 / D, bias=eps_t[:, 1:2]))
                    k.op("dve", [st], [st], lambda e: e.reciprocal(out=st[:, 4:5], in_=st[:, 3:4]))
                    k.op("dve", [vt, st, lng], [vt], lambda e, tt=tt: e.scalar_tensor_tensor(
                        out=vt[:, tt, :], in0=vt[:, tt, :], scalar=st[:, 4:5], op0=ALU.mult,
                        in1=lng[:, :], op1=ALU.mult))
                    k.op("dve", [vt, lnb], [vn], lambda e, tt=tt: e.tensor_tensor(
                        out=vn[:, tt, :], in0=vt[:, tt, :], in1=lnb[:, :], op=ALU.add))
                for cp in range(8):
                    wt = wA[na % 2]; na += 1
                    k.dma("sp", wt[:, :, :], wview(wbf, ou + cp * 256, 2048, 2), writes=[wt])
                    for jj in range(2):
                        c = cp * 2 + jj
                        pa = ps_a[c % 2]
                        for dc in range(NC_D):
                            k.op("pe", [wt, hT], [pa], lambda e, dc=dc, jj=jj, wt=wt, pa=pa: e.matmul(
                                pa[:, :], lhsT=wt[:, jj, dc * 128:(dc + 1) * 128], rhs=hT[:, dc, :],
                                start=(dc == 0), stop=(dc == NC_D - 1)), inc=(dc == NC_D - 1))
                        k.op("act", [pa], [uT], lambda e, c=c, pa=pa: e.activation(
                            out=uT[:, c, :], in_=pa[:, :], func=AF.Gelu))
                if nxt:
                    self.rms(xb, hTs[(blk + 1) % 2], g, ps_ss, sqs, rstd, tmp, self.onesb, eps_t)
                for c in range(NC_D):
                    gI = c // 2
                    pm = ps_m[c % 2]
                    for tt in range(4):
                        k.op("pe", [vn, wsb], [pm], lambda e, c=c, tt=tt, gI=gI, pm=pm: e.matmul(
                            pm[:, tt * 128:(tt + 1) * 128], lhsT=vn[:, tt, c * 128:(c + 1) * 128],
                            rhs=wsb[:, gI, :], start=True, stop=False), inc=False)
                        k.op("pe", [onesb, bsb], [pm], lambda e, tt=tt, gI=gI, pm=pm: e.matmul(
                            pm[:, tt * 128:(tt + 1) * 128], lhsT=onesb[0:1, :],
                            rhs=bsb[0:1, gI * 128:(gI + 1) * 128], start=False, stop=True), inc=(tt == 3))
                    k.op("dve", [pm, uT], [uT], lambda e, c=c, pm=pm: e.tensor_tensor(
                        out=uT[:, c, :], in0=pm[:, :], in1=uT[:, c, :], op=ALU.mult))
                for cp in range(8):
                    wt = wA[na % 2]; na += 1
                    k.dma("sp", wt[:, :, :], wview(wbf, oo + cp * 256, 2048, 2), writes=[wt])
                    for jj in range(2):
                        c = cp * 2 + jj
                        pa = ps_a[c % 2]
                        xc = xr[nx % 2]; nx += 1
                        k.dma("sp", xc[:, :], xT[c * 128:(c + 1) * 128, cols], writes=[xc])
                        for cc in range(NC_D):
                            k.op("pe", [wt, uT], [pa], lambda e, cc=cc, jj=jj, wt=wt, pa=pa: e.matmul(
                                pa[:, :], lhsT=wt[:, jj, cc * 128:(cc + 1) * 128], rhs=uT[:, cc, :],
                                start=(cc == 0), stop=(cc == NC_D - 1)), inc=(cc == NC_D - 1))
                        k.op("dve", [pa, xc], [xc], lambda e, xc=xc, pa=pa: e.tensor_tensor(
                            out=xc[:, :], in0=xc[:, :], in1=pa[:, :], op=ALU.add))
                        k.dma("pool", xT[c * 128:(c + 1) * 128, cols], xc[:, :], reads=[xc])

    def phase_e1(self, l):
        self.wait_cast(l)
        k = self.k
        wbf = self.dr["wbf%d" % l]
        oq, ov, orw = self.off["ein_qk%d" % l], self.off["ein_v%d" % l], self.off["ein_rw%d" % l]
        qkT, vtok, zrwT = self.dr["qkT"], self.dr["vtok"], self.dr["zrwT"]
        with k.phase() as ph:
            ones32, gains, eps_t = self.consts(ph)
            xbs = [k.sb([128, NC_D, TB], F32, "xb") for _ in range(2)]
            hTs = [k.sb([128, NC_D, TB], BF16, "hT") for _ in range(2)]
            wA = [k.sb([128, 2, 2048], BF16, "wA") for _ in range(3)]
            wv = [k.sb([128, NC_D, 512], BF16, "wv") for _ in range(2)]
            sqk = [k.sb([128, 2, TB], BF16, "sqk") for _ in range(3)]
            sv = [k.sb([128, 4, 512], BF16, "sv") for _ in range(2)]
            srw = [k.sb([128, 2, TB], F32, "srw") for _ in range(3)]
            sqs = [k.sb([128, TB], BF16, "sq") for _ in range(2)]
            rstd = k.sb([128, TB], F32, "rstd")
            tmp = k.sb([128, TB], F32, "tmp")
            ps_ss = k.ps("ss")
            ps_a = [k.ps("a") for _ in range(3)]
            ps_v = [k.ps("v") for _ in range(2)]
            g = _GV(gains, l * NC_D)
            na = 0
            ncp = 0
            xsrc = self.dr["xin"] if l == 0 else None
            self.load_x(xbs[0], 0, src=xsrc)
            self.rms(xbs[0], hTs[0], g, ps_ss, sqs, rstd, tmp, self.onesb, eps_t)
            for blk in range(self.nblk):
                cols = slice(blk * TB, (blk + 1) * TB)
                xb, hT = xbs[blk % 2], hTs[blk % 2]
                nxt = blk + 1 < self.nblk
                if nxt:
                    self.load_x(xbs[(blk + 1) % 2], blk + 1, src=xsrc)
                jobs = [("qk", cp) for cp in range(8)] + [("rw", cp) for cp in range(14)]
                for kind, cp in jobs:
                    if blk == 0 and kind == "rw" and cp == 0:
                        self.wait_cast(l, "a2")
                    wt = wA[na % 3]; na += 1
                    n = 2 if not (kind == "rw" and cp == 13) else 1
                    base = (oq if kind == "qk" else orw) + cp * 256
                    k.dma("sp", wt[:, 0:n, :], wview(wbf, base, 2048, n), writes=[wt])
                    stg = (sqk if kind == "qk" else srw)[ncp % 3]
                    for jj in range(n):
                        pa = ps_a[ncp % 3]
                        for dc in range(NC_D):
                            k.op("pe", [wt, hT], [pa], lambda e, dc=dc, jj=jj, wt=wt, pa=pa: e.matmul(
                                pa[:, :], lhsT=wt[:, jj, dc * 128:(dc + 1) * 128], rhs=hT[:, dc, :],
                                start=(dc == 0), stop=(dc == NC_D - 1)), inc=(dc == NC_D - 1))
                        if (cp + jj) % 2 == 0:
                            k.op("act", [pa], [stg], lambda e, jj=jj, pa=pa, stg=stg: e.activation(
                                out=stg[:, jj, :], in_=pa[:, :], func=AF.Copy))
                        else:
                            k.op("dve", [pa], [stg], lambda e, jj=jj, pa=pa, stg=stg: e.tensor_copy(
                                out=stg[:, jj, :], in_=pa[:, :]))
                        ncp += 1 if jj == n - 1 else 0
                        if jj < n - 1:
                            ncp += 0
                    dst = (qkT if kind == "qk" else zrwT)[cp * 256:cp * 256 + n * 128, cols].rearrange(
                        "(j p) t -> p j t", p=128)
                    k.dma("pool", dst, stg[:, 0:n, :], reads=[stg])
                if nxt:
                    self.rms(xbs[(blk + 1) % 2], hTs[(blk + 1) % 2], g, ps_ss, sqs, rstd, tmp, self.onesb, eps_t)
                for nb in range(2):
                    wt = wv[nb % 2]
                    k.dma("sp", wt[:, :, :], wview(wbf, ov + nb * 512, 8192, 1)[:, 0, :].rearrange(
                        "p (c m) -> p c m", c=NC_D), writes=[wt])
                    stg = sv[nb % 2]
                    for tt in range(4):
                        pv = ps_v[tt % 2]
                        for dc in range(NC_D):
                            k.op("pe", [wt, hT], [pv], lambda e, dc=dc, tt=tt, wt=wt, pv=pv: e.matmul(
                                pv[:, :], lhsT=hT[:, dc, tt * 128:(tt + 1) * 128], rhs=wt[:, dc, :],
                                start=(dc == 0), stop=(dc == NC_D - 1)), inc=(dc == NC_D - 1))
                        k.op("act", [pv], [stg], lambda e, tt=tt, pv=pv, stg=stg: e.activation(
                            out=stg[:, tt, :], in_=pv[:, :], func=AF.Copy))
                    dst = vtok[cols, nb * 512:(nb + 1) * 512].rearrange("(t p) c -> p t c", p=128)
                    k.dma("pool", dst, stg[:, :, :], reads=[stg])

    def phase_sb(self, l):
        k = self.k
        qkT, vtok, oT = self.dr["qkT"], self.dr["vtok"], self.dr["oT"]
        nseq = max(1, self.nblk * TB // SEQ)
        L = min(SEQ, self.nblk * TB)
        nqg = L // 512
        with k.phase() as ph:
            c32 = k.sb([128, 6, 512], F32, "c32")
            k.dma("sp", c32[:, :, :], self.dr["c_sb"], writes=[c32])
            cb = k.sb([128, 6, 512], BF16, "cb")
            k.op("dve", [c32], [cb], lambda e: e.tensor_copy(out=cb[:, :, :], in_=c32[:, :, :]))
            one_t = k.sb([128, 1], F32, "one")
            k.op("dve", [], [one_t], lambda e: e.memset(one_t[:, :], 1.0))
            qs = [k.sb([64, SEQ], BF16, "q") for _ in range(2)]
            ks = [k.sb([64, SEQ], BF16, "k") for _ in range(2)]
            nks = [None, None]
            xcs = [k.sb([128, 512], F32, "xc") for _ in range(2)]
            vs = [k.sb([128, 16, 64], BF16, "v") for _ in range(2)]
            ohs = [k.sb([64, SEQ], BF16, "oh") for _ in range(2)]
            e32 = [k.sb([128, 512], F32, "e32") for _ in range(2)]
            sps = [k.sb([128, 512], BF16, "sp") for _ in range(2)]
            A32 = k.sb([128, 512], F32, "A32")
            Abf = [k.sb([128, 512], BF16, "Abf") for _ in range(2)]
            ws = [k.sb([128, 512], BF16, "w") for _ in range(3)]
            ps_z = [k.ps("z") for _ in range(2)]
            ps_c = [k.ps("c") for _ in range(2)]
            ps_o = [k.ps("o") for _ in range(2)]
            Abf3 = Abf + [k.sb([128, 512], BF16, "Abf")]
            its = []
            hi = 0
            for b in range(nseq):
                for h in range(16):
                    for qg in range(nqg):
                        kbs = list(range(4 * qg + 3, -1, -1))
                        for idx, kb in enumerate(kbs):
                            its.append(dict(b=b, h=h, hi=hi, qg=qg, idx=idx, kb=kb, nkb=len(kbs),
                                            gq=hi * nqg + qg))
                    hi += 1

            def head_tiles(hi_):
                return qs[hi_ % 2], ks[hi_ % 2], nks[hi_ % 2], vs[hi_ % 2], ohs[hi_ % 2]

            def head_load(itd):
                self.bg_pump(2)
                b, h = itd["b"], itd["h"]
                q, kk, nk, v, oh = head_tiles(itd["hi"])
                tc_ = slice(b * SEQ, b * SEQ + L)
                k.dma("sp", q[:, 0:L], qkT[h * 64:(h + 1) * 64, tc_], writes=[q])
                k.dma("sp", kk[:, 0:L], qkT[1024 + h * 64:1024 + (h + 1) * 64, tc_], writes=[kk])
                k.dma("sp", v[:, 0:L // 128, :], vtok[tc_, h * 64:(h + 1) * 64].rearrange(
                    "(s p) c -> p s c", p=128), writes=[v])

            def stage1(n, itd):
                if itd["qg"] == 0 and itd["idx"] == 0:
                    head_load(itd)
                q, kk, nk, v, oh = head_tiles(itd["hi"])
                qg, kb, idx = itd["qg"], itd["kb"], itd["idx"]
                i = kb - 4 * qg
                qsl = slice(qg * 512, (qg + 1) * 512)
                ksl = slice(kb * 128, (kb + 1) * 128)
                pz, e_, sp = ps_z[n % 2], e32[n % 2], sps[n % 2]
                k.op("pe", [kk, q], [pz], lambda e: e.matmul(
                    pz[:, :], lhsT=kk[:, ksl], rhs=q[:, qsl], start=True, stop=True))
                k.op("act", [pz], [e_], lambda e: e.activation(out=e_[:, :], in_=pz[:, :], func=AF.Exp, scale=0.125))
                k.op("act", [e_, one_t], [sp], lambda e: e.activation(
                    out=sp[:, :], in_=e_[:, :], func=AF.Ln, bias=one_t[:, 0:1]))
                if i >= 0:
                    k.op("dve", [sp, cb], [sp], lambda e: e.tensor_tensor(
                        out=sp[:, :], in0=sp[:, :], in1=cb[:, 2 + i, :], op=ALU.mult))
                if idx < itd["nkb"] - 1:
                    abn = Abf3[(n + 1) % 3]
                    if idx == 0:
                        k.op("dve", [sp], [abn], lambda e: e.tensor_copy(out=abn[:, :], in_=sp[:, :]))
                    else:
                        abc = Abf3[n % 3]
                        k.op("dve", [sp, abc], [abn], lambda e: e.tensor_tensor(
                            out=abn[:, :], in0=abc[:, :], in1=sp[:, :], op=ALU.add))

            def stage2(n, itd):
                q, kk, nk, v, oh = head_tiles(itd["hi"])
                qg, kb, idx = itd["qg"], itd["kb"], itd["idx"]
                i = kb - 4 * qg
                qsl = slice(qg * 512, (qg + 1) * 512)
                ksl = slice(kb * 128, (kb + 1) * 128)
                pc, sp, w = ps_c[n % 2], sps[n % 2], ws[n % 3]
                first = idx == 0
                last = idx == itd["nkb"] - 1
                ab = Abf3[n % 3]
                k.op("pe", [cb, sp], [pc], lambda e: e.matmul(
                    pc[:, :], lhsT=cb[:, 0, 0:128], rhs=sp[:, :], start=True, stop=first), inc=first)
                if not first:
                    k.op("pe", [cb, ab], [pc], lambda e: e.matmul(
                        pc[:, :], lhsT=cb[:, 1, 0:128], rhs=ab[:, :], start=False, stop=True))
                xc_, e_ = xcs[n % 2], e32[n % 2]
                k.op("act", [pc], [xc_], lambda e: e.activation(out=xc_[:, :], in_=pc[:, :], func=AF.Exp, scale=-1.0))
                k.op("dve", [xc_, e_], [w], lambda e: e.tensor_tensor(out=w[:, :], in0=xc_[:, :], in1=e_[:, :], op=ALU.mult))
                if i >= 0:
                    k.op("dve", [w, cb], [w], lambda e: e.tensor_tensor(
                        out=w[:, :], in0=w[:, :], in1=cb[:, 2 + i, :], op=ALU.mult))

            def stage3(n, itd):
                q, kk, nk, v, oh = head_tiles(itd["hi"])
                qg, kb, idx = itd["qg"], itd["kb"], itd["idx"]
                qsl = slice(qg * 512, (qg + 1) * 512)
                w = ws[n % 3]
                po = ps_o[itd["gq"] % 2]
                first = idx == 0
                last = idx == itd["nkb"] - 1
                k.op("pe", [v, w], [po], lambda e: e.matmul(
                    po[0:64, :], lhsT=v[:, kb, :], rhs=w[:, :], start=first, stop=last), inc=last)
                if last:
                    k.op("dve", [po], [oh], lambda e: e.tensor_copy(out=oh[:, qsl], in_=po[0:64, :]))
                    if qg == nqg - 1:
                        b, h = itd["b"], itd["h"]
                        tc_ = slice(b * SEQ, b * SEQ + L)
                        k.dma("pool", oT[h * 64:(h + 1) * 64, tc_], oh[:, 0:L], reads=[oh])

            stage1(0, its[0])
            N_ = len(its)
            for n in range(N_ + 1):
                if n + 1 < N_:
                    stage1(n + 1, its[n + 1])
                if n < N_:
                    stage2(n, its[n])
                if n >= 1:
                    stage3(n - 1, its[n - 1])

    def phase_rw(self, l):
        k = self.k
        i = l // 2
        zrwT, oT = self.dr["zrwT"], self.dr["oT"]
        nseq = max(1, self.nblk * TB // SEQ)
        L = min(SEQ, self.nblk * TB)
        nq = L // 512
        NP = 12
        HH = (slice(0, 64), slice(64, 128))
        with k.phase() as ph:
            ones32, gains, eps_t = self.consts(ph)
            crw = k.sb([128, 5, 512], F32, "crw")
            k.dma("sp", crw[:, :, :], self.dr["c_rw"], writes=[crw])
            bd = k.sb([128, 128], F32, "bd")
            k.dma("sp", bd[:, :], self.dr["c_bd"], writes=[bd])
            par = k.sb([128, 8, NP], F32, "par")
            k.dma("sp", par[:, :, :], self.dr["rw_par"][i], writes=[par])
            k.op("dve", [par], [par], lambda e: e.tensor_scalar(
                out=par[:, :, 10:11], in0=par[:, :, 6:7], scalar1=-1.0, scalar2=1.0, op0=ALU.mult, op1=ALU.add))
            mux = k.sb([128, 4], F32, "mux")
            k.dma("sp", mux[:, :], self.dr["rw_mux"][i], writes=[mux])
            wup = k.sb([64, 1024], F32, "wup")
            aup = k.sb([64, 1024], F32, "aup")
            g0 = k.sb([128, 1024], F32, "g0")
            g1 = k.sb([32, 1024], F32, "g1")
            k.dma("sp", wup[:, :], self.dr["rw_wup"][i], writes=[wup])
            k.dma("sp", aup[:, :], self.dr["rw_aup"][i], writes=[aup])
            k.dma("sp", g0[:, :], self.dr["rw_gup"][i, 0:128, :], writes=[g0])
            k.dma("sp", g1[:, :], self.dr["rw_gup"][i, 128:160, :], writes=[g1])
            Sall = k.sb([128, 16, 64], F32, "Sall")
            k.op("dve", [], [Sall], lambda e: e.memset(Sall[:, :, :], 0.0))
            raws = [k.sb([128, 513], F32, "raw") for _ in range(4)]
            nraw = [0]
            SU, SL, IU, IDT, RST = 0, 1, 2, 3, 4
            tmpA = k.sb([128, 512], F32, "tmpA")

            def load_shift(dst, rows, row0, mucol, b, q, reads_mu):
                raw = raws[nraw[0] % 4]; nraw[0] += 1
                t0 = b * SEQ + q * 512
                if q == 0:
                    k.op("dve", [], [raw], lambda e: e.memset(raw[0:rows, 0:1], 0.0))
                    k.dma("sp", raw[0:rows, 1:513], zrwT[row0:row0 + rows, t0:t0 + 512], writes=[raw])
                else:
                    k.dma("sp", raw[0:rows, 0:513], zrwT[row0:row0 + rows, t0 - 1:t0 + 512], writes=[raw])
                T = tmpA
                k.op("dve", [raw], [T], lambda e: e.tensor_tensor(
                    out=T[0:rows, :], in0=raw[0:rows, 0:512], in1=raw[0:rows, 1:513], op=ALU.subtract))
                k.op("dve", [T, raw, reads_mu], [dst], lambda e: e.scalar_tensor_tensor(
                    out=dst[0:rows, 0:512], in0=T[0:rows, :], scalar=mucol(rows), op0=ALU.mult,
                    in1=raw[0:rows, 1:513], op1=ALU.add))

            sh = []
            for b in range(2):
                sh.append(dict(txw=k.sb([64, 512], F32, "txw"), xas=k.sb([64, 512], F32, "xas"),
                               sg0=k.sb([128, 512], F32, "sg0"), sg1=k.sb([32, 512], F32, "sg1")))
            names32 = ["rs", "ks", "vs", "sig", "cum", "gi", "ginv", "ge", "a", "kap", "kmod",
                       "kt", "rt", "bv", "T1", "T2"]
            names16 = ["kt16", "rt16", "vs16", "bt", "ktl",
                       "Nm", "NTm", "Mkk", "Mbr", "Mkr", "X0", "X1", "P0", "P1", "PT0", "PT1",
                       "Btm", "Ktm", "Vtm"]
            id16 = k.sb([128, 64], BF16, "id16")
            k.op("dve", [crw], [id16], lambda e: e.tensor_copy(out=id16[:, :], in_=crw[:, 3, 0:64]))
            ch = []
            for b in range(2):
                C = {}
                for nm in names32:
                    C[nm] = k.sb([128, 512], F32, nm)
                for nm in names16:
                    C[nm] = k.sb([128, 512], BF16, nm)
                C["Yq"] = C["kmod"]
                C["nP"] = [k.sb([128, 64], BF16, "nP") for _ in range(2)]
                C["UT"] = [k.sb([128, 64], BF16, "UT") for _ in range(2)]
                C["Sg"] = [k.sb([128, 64], F32, "Sg") for _ in range(2)]
                C["obf"] = [k.sb([128, 512], BF16, "obf") for _ in range(2)]
                C["pA"] = k.ps("pA")
                C["pB"] = k.ps("pB")
                C["pS"] = k.ps("pS")
                C["pY"] = k.ps("pY")
                C["no"] = 0
                ch.append(C)

            def mm(ps, out_ap, lhsT, rhs, reads, start, stop, inc):
                k.op("pe", reads, [ps], lambda e: e.matmul(out_ap, lhsT=lhsT, rhs=rhs, start=start, stop=stop), inc=inc)

            def dve(reads, writes, fn):
                k.op("dve", reads, writes, fn)

            def pool_(reads, writes, fn):
                k.op("pool", reads, writes, fn)

            def act(reads, writes, fn):
                k.op("act", reads, writes, fn)

            def unit_prep(q, hp, b, C, S):
                hc = slice(hp * 128, (hp + 1) * 128)
                pA, pB = C["pA"], C["pB"]
                rs, ks, vs = C["rs"], C["ks"], C["vs"]
                T1, T2 = C["T1"], C["T2"]
                load_shift(rs, 128, hp * 128, lambda r: par[0:r, hp, 0:1], b, q, par)
                load_shift(ks, 128, 1024 + hp * 128, lambda r: par[0:r, hp, 1:2], b, q, par)
                load_shift(vs, 128, 2048 + hp * 128, lambda r: par[0:r, hp, 2:3], b, q, par)
                yield
                sig, cum, gi, ginv, ge, a = C["sig"], C["cum"], C["gi"], C["ginv"], C["ge"], C["a"]
                kap, kmod, kt, bt, ktl, rt, bv = C["kap"], C["kmod"], C["kt"], C["bt"], C["ktl"], C["rt"], C["bv"]
                mm(pA, pA[:, :], wup[:, hc], S["txw"][:, :], [wup, S["txw"]], True, True, True)
                act([pA, par], [sig], lambda e: e.activation(out=sig[:, :], in_=pA[:, :], func=AF.Sigmoid,
                                                             bias=par[:, hp, 3:4]))
                dve([sig, crw], [cum], lambda e: e.tensor_tensor_scan(
                    out=cum[:, :], data0=crw[:, RST, :], data1=sig[:, :], initial=0.0, op0=ALU.mult, op1=ALU.add))
                act([cum], [gi], lambda e: e.activation(out=gi[:, :], in_=cum[:, :], func=AF.Exp, scale=-C0))
                act([cum], [ginv], lambda e: e.activation(out=ginv[:, :], in_=cum[:, :], func=AF.Exp, scale=C0))
                dve([cum, sig], [T1], lambda e: e.tensor_tensor(out=T1[:, :], in0=cum[:, :], in1=sig[:, :], op=ALU.subtract))
                act([T1], [ge], lambda e: e.activation(out=ge[:, :], in_=T1[:, :], func=AF.Exp, scale=-C0))
                yield
                mm(pB, pB[:, :], aup[:, hc], S["xas"][:, :], [aup, S["xas"]], True, True, True)
                act([pB, par], [a], lambda e: e.activation(out=a[:, :], in_=pB[:, :], func=AF.Sigmoid,
                                                           bias=par[:, hp, 4:5]))
                yield
                dve([ks, par], [T1], lambda e: e.tensor_scalar(out=T1[:, :], in0=ks[:, :], scalar1=par[:, hp, 5:6],
                                                               scalar2=None, op0=ALU.mult))
                dve([T1], [T2], lambda e: e.tensor_tensor(out=T2[:, :], in0=T1[:, :], in1=T1[:, :], op=ALU.mult))
                mm(pA, pA[:, :], bd[:, :], T2[:, :], [bd, T2], True, True, True)
                act([pA], [T2], lambda e: e.activation(out=T2[:, :], in_=pA[:, :], func=AF.Sqrt))
                dve([T2], [T2], lambda e: e.tensor_scalar_max(out=T2[:, :], in0=T2[:, :], scalar1=1e-12))
                dve([T2], [T2], lambda e: e.reciprocal(out=T2[:, :], in_=T2[:, :]))
                dve([T1, T2], [kap], lambda e: e.tensor_tensor(out=kap[:, :], in0=T1[:, :], in1=T2[:, :], op=ALU.mult))
                yield
                dve([a, par], [T1], lambda e: e.tensor_scalar(out=T1[:, :], in0=a[:, :], scalar1=par[:, hp, 6:7],
                                                              scalar2=par[:, hp, 10:11], op0=ALU.mult, op1=ALU.add))
                dve([ks, T1], [kmod], lambda e: e.tensor_tensor(out=kmod[:, :], in0=ks[:, :], in1=T1[:, :], op=ALU.mult))
                pool_([kap, ge], [kt], lambda e: e.tensor_tensor(out=kt[:, :], in0=kap[:, :], in1=ge[:, :], op=ALU.mult))
                kt16, rt16, vs16 = C["kt16"], C["rt16"], C["vs16"]
                k.op("pool", [kt], [kt16], lambda e: e.tensor_copy(out=kt16[:, :], in_=kt[:, :]))
                k.op("pool", [vs], [vs16], lambda e: e.tensor_copy(out=vs16[:, :], in_=vs[:, :]))
                pool_([kap, a], [T1], lambda e: e.tensor_tensor(out=T1[:, :], in0=kap[:, :], in1=a[:, :], op=ALU.mult))
                dve([T1, ginv], [bt], lambda e: e.tensor_tensor(out=bt[:, :], in0=T1[:, :], in1=ginv[:, :], op=ALU.mult))
                pool_([kmod, ginv], [ktl], lambda e: e.tensor_tensor(out=ktl[:, :], in0=kmod[:, :], in1=ginv[:, :], op=ALU.mult))
                pool_([rs, gi], [rt], lambda e: e.tensor_tensor(out=rt[:, :], in0=rs[:, :], in1=gi[:, :], op=ALU.mult))
                k.op("pool", [rt], [rt16], lambda e: e.tensor_copy(out=rt16[:, :], in_=rt[:, :]))
                yield
                dve([rs, kmod, par], [T1], lambda e: e.scalar_tensor_tensor(
                    out=T1[:, :], in0=rs[:, :], scalar=par[:, hp, 7:8], op0=ALU.mult, in1=kmod[:, :], op1=ALU.mult))
                mm(pB, pB[:, :], bd[:, :], T1[:, :], [bd, T1], True, True, True)
                dve([pB, vs], [bv], lambda e: e.tensor_tensor(out=bv[:, :], in0=pB[:, :], in1=vs[:, :], op=ALU.mult))
                yield
                specs = [("Nm", bt, kt16, SU, pA), ("NTm", kt16, bt, SL, pB), ("Mkk", ktl, kt16, SU, pA),
                         ("Mbr", bt, rt16, IU, pB), ("Mkr", ktl, rt16, IU, pA)]
                for nm, Lt, Rt, mk, ps in specs:
                    dst = C[nm]
                    for c in range(8):
                        cs = slice(c * 64, (c + 1) * 64)
                        for hh, pr in enumerate(HH):
                            mm(ps, ps[pr, cs], Lt[pr, cs], Rt[pr, cs], [Lt, Rt], True, True, c == 7 and hh == 1)
                    dve([ps, crw], [dst], lambda e, ps=ps, dst=dst, mk=mk: e.tensor_tensor(
                        out=dst[:, :], in0=ps[:, :], in1=crw[:, mk, :], op=ALU.mult))
                    yield
                X = C["X0"]
                dve([crw, C["Nm"]], [X], lambda e: e.tensor_tensor(out=X[:, :], in0=crw[:, IDT, :], in1=C["Nm"][:, :],
                                                                   op=ALU.subtract))
                P, PT = C["Nm"], C["NTm"]
                Pb = [C["P0"], C["P1"]]
                PTb = [C["PT0"], C["PT1"]]
                Xb = [C["X1"], C["X0"]]
                for j in range(1, 6):
                    Pn, PTn, Xn = Pb[j % 2], PTb[j % 2], Xb[(j - 1) % 2]
                    if j < 5:
                        for c in range(8):
                            cs = slice(c * 64, (c + 1) * 64)
                            for hh, pr in enumerate(HH):
                                mm(pA, pA[pr, cs], PT[pr, cs], P[pr, cs], [PT, P], True, True, c == 7 and hh == 1)
                    for c in range(8):
                        cs = slice(c * 64, (c + 1) * 64)
                        for hh, pr in enumerate(HH):
                            mm(pB, pB[pr, cs], P[pr, cs], PT[pr, cs], [PT, P], True, True, c == 7 and hh == 1)
                    if j < 5:
                        act([pA], [Pn], lambda e, Pn=Pn: e.activation(out=Pn[:, :], in_=pA[:, :], func=AF.Copy))
                    act([pB], [PTn], lambda e, PTn=PTn: e.activation(out=PTn[:, :], in_=pB[:, :], func=AF.Copy))
                    yield
                    for c in range(8):
                        cs = slice(c * 64, (c + 1) * 64)
                        for hh, pr in enumerate(HH):
                            mm(pA, pA[pr, cs], PTn[pr, cs], X[pr, cs], [PTn, X], True, True, c == 7 and hh == 1)
                    dve([pA, X], [Xn], lambda e, X=X, Xn=Xn: e.tensor_tensor(
                        out=Xn[:, :], in0=pA[:, :], in1=X[:, :], op=ALU.add))
                    P, PT, X = Pn, PTn, Xn
                    yield
                C["X"] = X
                for src, dn, ps in ((bt, "Btm", pA), (ktl, "Ktm", pB), (vs16, "Vtm", pA)):
                    dst = C[dn]
                    for c in range(8):
                        cs = slice(c * 64, (c + 1) * 64)
                        for hh, pr in enumerate(HH):
                            mm(ps, ps[pr, cs], src[pr, cs], id16[pr, :], [src, id16], True, True,
                               c == 7 and hh == 1)
                    if dn != "Vtm":
                        act([ps], [dst], lambda e, ps=ps, dst=dst: e.activation(out=dst[:, :], in_=ps[:, :], func=AF.Copy))
                    else:
                        dve([ps], [dst], lambda e, ps=ps, dst=dst: e.tensor_copy(out=dst[:, :], in_=ps[:, :]))
                    yield

            def chunk_a(c, hp, b, C):
                cs = slice(c * 64, (c + 1) * 64)
                pS = C["pS"]
                kt, Mkk, Vtm = C["kt"], C["Mkk"], C["Vtm"]
                si = b * 8 + hp
                nP = C["nP"][c % 2]
                for hh, pr in enumerate(HH):
                    mm(pS, pS[pr, 0:64], kt[pr, cs], Sall[pr, si, :], [kt, Sall], True, False, False)
                    mm(pS, pS[pr, 0:64], Mkk[pr, cs], Vtm[pr, cs], [Mkk, Vtm], False, True, hh == 1)
                act([pS], [nP], lambda e: e.activation(out=nP[:, :], in_=pS[:, 0:64], func=AF.Copy, scale=-1.0))

            def chunk_b(c, hp, b, C):
                cs = slice(c * 64, (c + 1) * 64)
                pS, X = C["pS"], C["X"]
                nP, UT = C["nP"][c % 2], C["UT"][c % 2]
                for hh, pr in enumerate(HH):
                    mm(pS, pS[pr, 64:128], X[pr, cs], nP[pr, :], [X, nP], True, True, hh == 1)
                act([pS], [UT], lambda e: e.activation(out=UT[:, :], in_=pS[:, 64:128], func=AF.Copy))

            def chunk_c(c, hp, b, C):
                cs = slice(c * 64, (c + 1) * 64)
                pS, pY = C["pS"], C["pY"]
                rt, Mbr, Mkr, Btm, Ktm, Vtm, gi = C["rt"], C["Mbr"], C["Mkr"], C["Btm"], C["Ktm"], C["Vtm"], C["gi"]
                si = b * 8 + hp
                UT, Sg = C["UT"][c % 2], C["Sg"][c % 2]
                gcol = gi[:, c * 64 + 63:c * 64 + 64]
                for hh, pr in enumerate(HH):
                    mm(pY, pY[pr, cs], Sall[pr, si, :], rt[pr, cs], [Sall, rt], True, False, False)
                    mm(pY, pY[pr, cs], UT[pr, :], Mbr[pr, cs], [UT, Mbr], False, False, False)
                    mm(pY, pY[pr, cs], Vtm[pr, cs], Mkr[pr, cs], [Vtm, Mkr], False, True, hh == 1)
                for hh, pr in enumerate(HH):
                    mm(pS, pS[pr, 128:192], Btm[pr, cs], UT[pr, :], [Btm, UT], True, False, False)
                    mm(pS, pS[pr, 128:192], Ktm[pr, cs], Vtm[pr, cs], [Ktm, Vtm], False, True, hh == 1)
                dve([Sall, gi], [Sg], lambda e: e.tensor_scalar(out=Sg[:, :], in0=Sall[:, si, :], scalar1=gcol,
                                                                scalar2=None, op0=ALU.mult))
                dve([pS, Sg, gi], [Sall], lambda e: e.scalar_tensor_tensor(
                    out=Sall[:, si, :], in0=pS[:, 128:192], scalar=gcol, op0=ALU.mult, in1=Sg[:, :], op1=ALU.add))

            def unit_post(q, hp, b, C, S):
                hc = slice(hp * 128, (hp + 1) * 128)
                pA, pB, pY = C["pA"], C["pB"], C["pY"]
                Yq, T1, T2, bv = C["Yq"], C["T1"], C["T2"], C["bv"]
                act([pY], [Yq], lambda e: e.activation(out=Yq[:, :], in_=pY[:, :], func=AF.Copy))
                mm(pA, pA[:, :], bd[:, :], Yq[:, :], [bd, Yq], True, True, True)
                dve([pA, Yq], [T1], lambda e: e.scalar_tensor_tensor(
                    out=T1[:, :], in0=pA[:, :], scalar=-1.0 / 64, op0=ALU.mult, in1=Yq[:, :], op1=ALU.add))
                dve([T1], [T2], lambda e: e.tensor_tensor(out=T2[:, :], in0=T1[:, :], in1=T1[:, :], op=ALU.mult))
                mm(pB, pB[:, :], bd[:, :], T2[:, :], [bd, T2], True, True, True)
                act([pB, eps_t], [T2], lambda e: e.activation(out=T2[:, :], in_=pB[:, :], func=AF.Sqrt,
                                                              scale=1.0 / 64, bias=eps_t[:, 2:3]))
                yield
                dve([T2], [T2], lambda e: e.reciprocal(out=T2[:, :], in_=T2[:, :]))
                dve([T1, T2], [T1], lambda e: e.tensor_tensor(out=T1[:, :], in0=T1[:, :], in1=T2[:, :], op=ALU.mult))
                dve([T1, par], [T1], lambda e: e.tensor_scalar(out=T1[:, :], in0=T1[:, :], scalar1=par[:, hp, 8:9],
                                                               scalar2=par[:, hp, 9:10], op0=ALU.mult, op1=ALU.add))
                dve([T1, bv], [T1], lambda e: e.tensor_tensor(out=T1[:, :], in0=T1[:, :], in1=bv[:, :], op=ALU.add))
                mm(pA, pA[:, :], g0[:, hc], S["sg0"][:, :], [g0, S["sg0"]], True, False, False)
                mm(pA, pA[:, :], g1[:, hc], S["sg1"][:, :], [g1, S["sg1"]], False, True, True)
                ob = C["obf"][C["no"] % 2]; C["no"] += 1
                dve([T1, pA], [ob], lambda e: e.tensor_tensor(out=ob[:, :], in0=T1[:, :], in1=pA[:, :], op=ALU.mult))
                t0 = b * SEQ + q * 512
                k.dma("pool", oT[1024 + hp * 128:1024 + (hp + 1) * 128, t0:t0 + 512], ob[:, :], reads=[ob])

            def chain(b):
                C, S = ch[b], sh[b]
                for q in range(nq):
                    load_shift(S["txw"], 64, 3072, lambda r: mux[0:r, 0:1], b, q, mux)
                    act([S["txw"]], [S["txw"]], lambda e: e.activation(out=S["txw"][:, :], in_=S["txw"][:, :], func=AF.Tanh))
                    load_shift(S["xas"], 64, 3136, lambda r: mux[0:r, 1:2], b, q, mux)
                    load_shift(S["sg0"], 128, 3200, lambda r: mux[0:r, 2:3], b, q, mux)
                    act([S["sg0"]], [S["sg0"]], lambda e: e.activation(out=S["sg0"][:, :], in_=S["sg0"][:, :], func=AF.Sigmoid))
                    load_shift(S["sg1"], 32, 3328, lambda r: mux[0:r, 3:4], b, q, mux)
                    act([S["sg1"]], [S["sg1"]], lambda e: e.activation(out=S["sg1"][:, :], in_=S["sg1"][:, :], func=AF.Sigmoid))
                    yield
                    for hp in range(8):
                        if b == 0:
                            self.bg_pump(1)
                        yield from unit_prep(q, hp, b, C, S)
                        for c in range(8):
                            chunk_a(c, hp, b, C)
                            yield
                            chunk_b(c, hp, b, C)
                            yield
                            chunk_c(c, hp, b, C)
                            yield
                        yield from unit_post(q, hp, b, C, S)

            streams = [chain(b) for b in range(nseq)]
            live = [True] * nseq
            for _ in range(30):
                next(streams[0])
            while any(live):
                for b in range(nseq):
                    if live[b]:
                        try:
                            next(streams[b])
                        except StopIteration:
                            live[b] = False

    def phase_eout(self, l):
        self.wait_cast(l, "a2")
        k = self.k
        wbf = self.dr["wbf%d" % l]
        oo = self.off["eout%d" % l]
        oT = self.dr["oT"]
        with k.phase() as ph:
            xbs = [k.sb([128, NC_D, TB], F32, "xb") for _ in range(2)]
            obs = [k.sb([128, NC_D, TB], BF16, "ob") for _ in range(2)]
            wA = [k.sb([128, 2, 2048], BF16, "wA") for _ in range(3)]
            ps_a = [k.ps("a") for _ in range(2)]
            na = 0
            xsrc = self.dr["xin"] if l == 0 else None

            def ld(blk):
                cols = slice(blk * TB, (blk + 1) * TB)
                self.load_x(xbs[blk % 2], blk, src=xsrc)
                for g4 in range(4):
                    k.dma("sp", obs[blk % 2][:, g4 * 4:(g4 + 1) * 4, :], oT[g4 * 512:(g4 + 1) * 512, cols].rearrange(
                        "(c p) t -> p c t", p=128), writes=[obs[blk % 2]])
            ld(0)
            for blk in range(self.nblk):
                xb, ob = xbs[blk % 2], obs[blk % 2]
                if blk + 1 < self.nblk:
                    ld(blk + 1)
                for cp in range(8):
                    wt = wA[na % 3]; na += 1
                    k.dma("sp", wt[:, :, :], wview(wbf, oo + cp * 256, 2048, 2), writes=[wt])
                    for jj in range(2):
                        c = cp * 2 + jj
                        pa = ps_a[c % 2]
                        for cc in range(NC_D):
                            k.op("pe", [wt, ob], [pa], lambda e, cc=cc, jj=jj, wt=wt, pa=pa: e.matmul(
                                pa[:, :], lhsT=wt[:, jj, cc * 128:(cc + 1) * 128], rhs=ob[:, cc, :],
                                start=(cc == 0), stop=(cc == NC_D - 1)), inc=(cc == NC_D - 1))
                        k.op("dve", [pa, xb], [xb], lambda e, c=c, pa=pa: e.tensor_tensor(
                            out=xb[:, c, :], in0=xb[:, c, :], in1=pa[:, :], op=ALU.add))
                self.store_x(xb, blk)

    def phase_zero_orw(self):
        k = self.k
        with k.phase():
            z = k.sb([128, 8, TB], BF16, "z")
            k.op("dve", [], [z], lambda e: e.memset(z[:, :, :], 0.0))
            for blk in range(self.nblk):
                k.dma("sp", self.dr["oT"][1024:2048, blk * TB:(blk + 1) * TB].rearrange(
                    "(c p) t -> p c t", p=128), z[:, :, :], reads=[z])

    def phase_final(self):
        k = self.k
        with k.phase() as ph:
            ones32, gains, eps_t = self.consts(ph)
            xbs = [k.sb([128, NC_D, TB], F32, "xb") for _ in range(2)]
            obs = [k.sb([128, NC_D, TB], F32, "ob") for _ in range(2)]
            sqs = [k.sb([128, TB], BF16, "sq") for _ in range(2)]
            rstd = k.sb([128, TB], F32, "rstd")
            tmp = k.sb([128, TB], F32, "tmp")
            ps_ss = k.ps("ss")
            g = _GV(gains, 8 * NC_D)
            for blk in range(self.nblk):
                xb = xbs[blk % 2]
                ob = obs[blk % 2]
                self.load_x(xb, blk)
                self.rms(xb, ob, g, ps_ss, sqs, rstd, tmp, self.onesb, eps_t)
                self.store_x(ob, blk, dst=self.dr["outT"])


class _GV:
    def __init__(self, base, off):
        self.base = base
        self.off = off
        self.t = base.t

    @property
    def w(self):
        return self.base.w

    @w.setter
    def w(self, v):
        self.base.w = v

    @property
    def r(self):
        return self.base.r

    @r.setter
    def r(self, v):
        self.base.r = v

    def __getitem__(self, idx):
        p, c = idx
        if isinstance(c, slice):
            c = slice(c.start + self.off, c.stop + self.off)
        else:
            c = c + self.off
        return self.t[p, c]


def host_consts():
    c = {}
    c["c_ones32"] = np.ones((128, 128), np.float32)
    sb = np.zeros((128, 6, 512), np.float32)
    jj = np.arange(128)[:, None]
    ss = np.arange(128)[None, :]
    sb[:, 0, :128] = (jj >= ss)
    sb[:, 1, :128] = 1.0
    tq = np.arange(512)[None, :]
    for i in range(4):
        sb[:, 2 + i, :] = (tq > jj + 128 * i)
    c["c_sb"] = sb
    rw = np.zeros((128, 5, 512), np.float32)
    s_ = (np.arange(128) % 64)[:, None]
    t_ = np.arange(64)[None, :]
    rw[:, 0, :] = np.tile((s_ < t_), (1, 8))
    rw[:, 1, :] = np.tile((s_ > t_), (1, 8))
    rw[:, 2, :] = np.tile((s_ <= t_), (1, 8))
    rw[:, 3, :] = np.tile((s_ == t_), (1, 8))
    rw[:, 4, :] = (np.arange(512) % 64 != 0)[None, :]
    c["c_rw"] = rw
    bdm = np.zeros((128, 128), np.float32)
    bdm[:64, :64] = 1.0
    bdm[64:, 64:] = 1.0
    c["c_bd"] = bdm
    c["c_eps"] = np.tile(np.array([[RMS_EPS, LN_EPS, LNX_EPS, 1.0]], np.float32), (128, 1))
    return c


def build(plan, nblk=NBLK, debug_out=()):
    nc = bass.Bass("TRN2", target_bir_lowering=False)
    off, wrows = wpack_layout()
    dr = {}
    dr["xin"] = nc.dram_tensor("xin", [D, NT], F32, kind="ExternalInput").ap()
    for l in range(DEPTH):
        dr["wflat%d" % l] = nc.dram_tensor("wflat%d" % l, [wrows[l], 2048], F32, kind="ExternalInput").ap()
        dr["wbf%d" % l] = nc.dram_tensor("wbf%d" % l, [wrows[l], 2048], BF16, kind="Internal").ap()
    dr["gains"] = nc.dram_tensor("gains", [128, 9 * NC_D], F32, kind="ExternalInput").ap()
    for name, arr in host_consts().items():
        dr[name] = nc.dram_tensor(name, list(arr.shape), F32, kind="ExternalInput").ap()
    dr["odd_lng"] = nc.dram_tensor("odd_lng", [2, 128, D], F32, kind="ExternalInput").ap()
    dr["odd_lnb"] = nc.dram_tensor("odd_lnb", [2, 128, D], F32, kind="ExternalInput").ap()
    dr["odd_wsT"] = nc.dram_tensor("odd_wsT", [2, 128, 8, 128], F32, kind="ExternalInput").ap()
    dr["odd_bs"] = nc.dram_tensor("odd_bs", [2, 1, 1024], F32, kind="ExternalInput").ap()
    def _scr(name, shape, dt):
        return nc.dram_tensor(name, shape, dt, kind=("ExternalOutput" if name in debug_out else "Internal")).ap()
    dr["qkT"] = _scr("qkT", [2048, NT], BF16)
    dr["vtok"] = _scr("vtok", [NT, 1024], BF16)
    dr["zrwT"] = _scr("zrwT", [27 * 128, NT], F32)
    dr["oT"] = _scr("oT", [2048, NT], BF16)
    dr["rw_par"] = nc.dram_tensor("rw_par", [2, 128, 8, 12], F32, kind="ExternalInput").ap()
    dr["rw_mux"] = nc.dram_tensor("rw_mux", [2, 128, 4], F32, kind="ExternalInput").ap()
    dr["rw_wup"] = nc.dram_tensor("rw_wup", [2, 64, 1024], F32, kind="ExternalInput").ap()
    dr["rw_aup"] = nc.dram_tensor("rw_aup", [2, 64, 1024], F32, kind="ExternalInput").ap()
    dr["rw_gup"] = nc.dram_tensor("rw_gup", [2, 160, 1024], F32, kind="ExternalInput").ap()
    dr["outT"] = nc.dram_tensor("outT", [D, NT], F32, kind="ExternalOutput").ap()
    dr["xT"] = dr["xin"] if False else nc.dram_tensor("xT", [D, NT], F32, kind=("ExternalOutput" if "xT" in debug_out else "Internal")).ap()
    k = K(nc)
    p = Prog(nc, k, dr)
    p.off = off
    p.wrows = wrows
    p.nblk = nblk
    for step in plan:
        if step[0] == "castq":
            p.cast_queue(step[1], step[2])
        elif step[0] == "castbg":
            p.phase_cast(step[1], step[2], bg=True)
        elif step[0] == "cast":
            p.phase_cast(step[1], step[2])
        elif step[0] == "copyx":
            p.phase_copyx()
        elif step[0] == "ffn":
            p.phase_ffn(step[1])
        elif step[0] == "odd":
            p.phase_odd(step[1])
        elif step[0] == "e1":
            p.phase_e1(step[1])
        elif step[0] == "sb":
            p.phase_sb(step[1])
        elif step[0] == "eout":
            p.phase_eout(step[1])
        elif step[0] == "zero_orw":
            p.phase_zero_orw()
        elif step[0] == "rw":
            p.phase_rw(step[1])
        elif step[0] == "final":
            p.phase_final()
        else:
            raise ValueError(step)
    return nc


def full_plan():
    def cb(l):
        return [("castq", l, "a1"), ("castq", l, "a2"), ("castq", l, "b")]
    plan = [("cast", 0, "a1"), ("castbg", 0, "a2")]
    plan += [("e1", 0), ("castq", 0, "b")] + cb(1) + [("sb", 0)] + cb(2) + [("rw", 0), ("eout", 0), ("ffn", 0)]
    plan += [("odd", 1), ("ffn", 1)]
    plan += [("e1", 2)] + cb(3) + [("sb", 2), ("rw", 2), ("eout", 2), ("ffn", 2)]
    plan += [("odd", 3), ("ffn", 3)]
    plan += [("final",)]
    return plan


def host_inputs(inp):
    x = np.asarray(inp["x"], np.float32)
    gains = np.concatenate([np.asarray(inp["norm_mix"], np.float32),
                            np.asarray(inp["norm_ffn"], np.float32),
                            np.asarray(inp["norm_final"], np.float32)[None]], axis=0)
    gains = np.ascontiguousarray(gains.reshape(9, NC_D, 128).transpose(2, 0, 1).reshape(128, 9 * NC_D))
    wflat = pack_weights({k_: np.asarray(v, np.float32) for k_, v in inp.items()})
    common = {"gains": gains}
    f = lambda a: np.asarray(a, np.float32)
    par = np.zeros((2, 128, 8, 12), np.float32)
    mu = f(inp["even_mu"])
    def hd(a):
        return a.reshape(2, 8, 128).transpose(0, 2, 1)
    par[:, :, :, 0] = hd(mu[:, 0:1024]); par[:, :, :, 1] = hd(mu[:, 1024:2048]); par[:, :, :, 2] = hd(mu[:, 2048:3072])
    par[:, :, :, 3] = hd(f(inp["even_w0"])); par[:, :, :, 4] = hd(f(inp["even_a0"]))
    par[:, :, :, 5] = hd(f(inp["even_k_k"])); par[:, :, :, 6] = hd(f(inp["even_k_a"]))
    par[:, :, :, 7] = hd(f(inp["even_r_k"]).reshape(2, 1024))
    par[:, :, :, 8] = hd(f(inp["even_lnx_w"])); par[:, :, :, 9] = hd(f(inp["even_lnx_b"]))
    common["rw_par"] = par
    mux = np.zeros((2, 128, 4), np.float32)
    mux[:, :64, 0] = mu[:, 3072:3136]; mux[:, :64, 1] = mu[:, 3136:3200]
    mux[:, :, 2] = mu[:, 3200:3328]; mux[:, :32, 3] = mu[:, 3328:3360]
    common["rw_mux"] = mux
    common["rw_wup"] = f(inp["even_w_up"]); common["rw_aup"] = f(inp["even_a_up"]); common["rw_gup"] = f(inp["even_g_up"])
    common["odd_lng"] = np.ascontiguousarray(np.broadcast_to(f(inp["odd_ln_g"])[:, None, :], (2, 128, D)))
    common["odd_lnb"] = np.ascontiguousarray(np.broadcast_to(f(inp["odd_ln_b"])[:, None, :], (2, 128, D)))
    common["odd_wsT"] = np.ascontiguousarray(f(inp["odd_w_s"]).transpose(0, 3, 1, 2))
    common["odd_bs"] = np.ascontiguousarray(f(inp["odd_b_s"]).reshape(2, 1, 1024))
    for l in range(DEPTH):
        common["wflat%d" % l] = wflat[l]
    common.update(host_consts())
    maps = []
    for c in range(8):
        xs = x[c * NB_CORE:(c + 1) * NB_CORE].reshape(NT, D)
        m = dict(common)
        m["xin"] = np.ascontiguousarray(xs.T)
        maps.append(m)
    return maps


def kernel(**inputs):
    maps = host_inputs(inputs)
    nc = build(full_plan())
    res = run_bass_kernel_spmd(nc, maps, core_ids=list(range(8)))
    out = np.empty((8 * NB_CORE, SEQ, D), np.float32)
    for c in range(8):
        oT = np.asarray(res.results[c]["outT"], np.float32)
        out[c * NB_CORE:(c + 1) * NB_CORE] = oT.T.reshape(NB_CORE, SEQ, D)
    return out
```

```python
import contextlib
import numpy as np
import concourse.bass as bass
import concourse.mybir as mybir
from concourse.bass_utils import run_bass_kernel_spmd

F32 = mybir.dt.float32
BF16 = mybir.dt.bfloat16
AF = mybir.ActivationFunctionType
ALU = mybir.AluOpType

D = 2048
SEQ = 2048
NB_CORE = 2
NT = NB_CORE * SEQ
TB = 512
NBLK = NT // TB
DEPTH = 4
HID = 8192
NC_D = D // 128
NJ = HID // 128
RWW = 3360
EIN = 6432
RMS_EPS = 1e-6
LN_EPS = 1e-5
LNX_EPS = 64e-5
C0 = float(np.exp(-0.5))


class Sem:
    def __init__(self, h):
        self.h = h
        self.n = 0


class Tile:
    def __init__(self, t, name):
        self.t = t
        self.name = name
        self.w = []
        self.r = []
        self.dsem = None

    def __getitem__(self, idx):
        return self.t[idx]


class Eng:
    def __init__(self, name, e, sem):
        self.name = name
        self.e = e
        self.sem = sem
        self.seen = {}


class K:
    def __init__(self, nc):
        self.nc = nc
        self.engs = {}
        for name, e in (("pe", nc.tensor), ("act", nc.scalar), ("dve", nc.vector),
                        ("pool", nc.gpsimd), ("sp", nc.sync)):
            self.engs[name] = Eng(name, e, Sem(nc.alloc_semaphore("p_" + name)))
        self.free_dsems = [Sem(nc.alloc_semaphore("d%d" % i)) for i in range(80)]
        self.live_dsems = []
        self.stack = None
        self.tiles = []
        self.uid = 0

    @contextlib.contextmanager
    def phase(self):
        self.stack = contextlib.ExitStack()
        self.tiles = []
        try:
            yield self
        finally:
            pass
        self.barrier()
        self.stack.close()
        self.stack = None
        self.free_dsems.extend(self.live_dsems)
        self.live_dsems = []
        self.tiles = []

    def sb(self, shape, dtype, name=None):
        self.uid += 1
        name = "%s_%d" % (name or "t", self.uid)
        t = self.stack.enter_context(self.nc.sbuf_tensor(name, list(shape), dtype))
        tl = Tile(t, name)
        self.tiles.append(tl)
        return tl

    def ps(self, name=None, dtype=F32, shape=(128, 512)):
        self.uid += 1
        name = "%s_%d" % (name or "ps", self.uid)
        t = self.stack.enter_context(self.nc.psum_tensor(name, list(shape), dtype))
        tl = Tile(t, name)
        self.tiles.append(tl)
        return tl

    def _wait(self, eng, toks):
        best = {}
        for (s, v) in toks:
            if v > best.get(id(s), (s, 0))[1]:
                best[id(s)] = (s, v)
        for (s, v) in best.values():
            if eng.seen.get(id(s), 0) >= v:
                continue
            eng.e.wait_ge(s.h, v)
            eng.seen[id(s)] = v

    def _deps(self, eng, reads, writes):
        toks = []
        for t in reads:
            for tk in t.w:
                if tk[0] is eng.sem and eng.name == "pe":
                    continue
                toks.append(tk)
        for t in writes:
            for tk in t.w + t.r:
                if tk[0] is eng.sem:
                    continue
                toks.append(tk)
        return toks

    @staticmethod
    def _note_read(t, tok):
        t.r = [x for x in t.r if x[0] is not tok[0]] + [tok]

    def op(self, en, reads, writes, fn, inc=True):
        eng = self.engs[en]
        self._wait(eng, self._deps(eng, reads, writes))
        ins = fn(eng.e)
        if inc:
            eng.sem.n += 1
            ins.then_inc(eng.sem.h, 1)
            tok = (eng.sem, eng.sem.n)
        else:
            tok = (eng.sem, eng.sem.n + 1)
        for t in writes:
            t.w = [tok]
            t.r = []
        for t in reads:
            if t not in writes:
                self._note_read(t, tok)
        return ins

    def _dsem(self, t):
        if t.dsem is None:
            t.dsem = self.free_dsems.pop()
            self.live_dsems.append(t.dsem)
        return t.dsem

    def dma(self, q, out, in_, reads=(), writes=(), owner=None):
        eng = self.engs[q]
        self._wait(eng, self._deps(eng, list(reads), list(writes)))
        owner = owner or (writes[0] if writes else reads[0])
        s = self._dsem(owner)
        s.n += 16
        eng.e.dma_start(out=out, in_=in_).then_inc(s.h, 16)
        tok = (s, s.n)
        for t in writes:
            t.w = [tok]
            t.r = []
        for t in reads:
            self._note_read(t, tok)
        return tok

    def barrier(self):
        toks = [(e.sem, e.sem.n) for e in self.engs.values()]
        toks += [(s, s.n) for s in self.live_dsems]
        for e in self.engs.values():
            self._wait(e, [tk for tk in toks if tk[0] is not e.sem and tk[1] > 0])
        for t in self.tiles:
            t.w = []
            t.r = []


def tile_w(W, bw):
    Kd, N = W.shape
    return np.ascontiguousarray(W.reshape(Kd // 128, 128, N // bw, bw).transpose(2, 1, 0, 3))


class WPack:
    def __init__(self):
        self.parts = []
        self.off = {}
        self.rows = 0

    def add(self, name, arr):
        a = np.ascontiguousarray(arr, dtype=np.float32).reshape(-1)
        assert a.size % 2048 == 0, (name, a.size)
        self.off[name] = self.rows
        self.parts.append(a.reshape(-1, 2048))
        self.rows += a.size // 2048

    def build(self):
        return np.concatenate(self.parts, axis=0)


def wpack_layout():
    off = {}
    rows = []
    for l in range(DEPTH):
        r = 0
        if l % 2 == 0:
            off["ein_qk%d" % l] = r; r += 16 * 128 * 16 * 128 // 2048
            off["ein_v%d" % l] = r; r += 2 * 128 * 16 * 512 // 2048
            off["ein_rw%d" % l] = r; r += 27 * 128 * 16 * 128 // 2048
            off["eout%d" % l] = r; r += 16 * 128 * 16 * 128 // 2048
        else:
            off["oin_u%d" % l] = r; r += 16 * 128 * 16 * 128 // 2048
            off["oin_v%d" % l] = r; r += 4 * 128 * 16 * 512 // 2048
            off["oout%d" % l] = r; r += 16 * 128 * 16 * 128 // 2048
        off["w1_%d" % l] = r; r += 64 * 128 * 16 * 128 // 2048
        off["w2_%d" % l] = r; r += 16 * 128 * 64 * 128 // 2048
        rows.append(r)
    return off, rows


def pack_weights(inp):
    off, rows = wpack_layout()
    out = []
    for l in range(DEPTH):
        wp = WPack()
        i = l // 2
        if l % 2 == 0:
            win = inp["even_w_in"][i]
            wp.add("ein_qk%d" % l, tile_w(win[:, :2048], 128))
            wp.add("ein_v%d" % l, tile_w(win[:, 2048:3072], 512))
            wrw = np.zeros((D, 27 * 128), np.float32)
            wrw[:, :RWW] = win[:, 3072:]
            wp.add("ein_rw%d" % l, tile_w(wrw, 128))
            wp.add("eout%d" % l, tile_w(inp["even_w_out"][i], 128))
        else:
            win = inp["odd_w_in"][i]
            wp.add("oin_u%d" % l, tile_w(win[:, :2048], 128))
            wp.add("oin_v%d" % l, tile_w(win[:, 2048:], 512))
            wp.add("oout%d" % l, tile_w(inp["odd_w_out"][i], 128))
        wp.add("w1_%d" % l, tile_w(inp["ffn_w1"][l], 128))
        w2t = tile_w(inp["ffn_w2"][l], 128).reshape(16, 128, 2, 32, 128).transpose(0, 2, 1, 3, 4)
        wp.add("w2_%d" % l, w2t)
        for k_, v in wp.off.items():
            assert off[k_] == v
        assert wp.rows == rows[l]
        out.append(wp.build())
    return out


def wview(wbf, row0, F, n=1):
    a = F // 2048
    v = wbf[row0:row0 + n * 128 * a, :]
    return v.rearrange("(n p a) c -> p n (a c)", n=n, p=128, a=a)


class Prog:
    def __init__(self, nc, k, dr):
        self.nc = nc
        self.k = k
        self.dr = dr

    def load_x(self, xb, blk, src=None):
        k = self.k
        xT = src if src is not None else self.dr["xT"]
        for g in range(4):
            src = xT[g * 512:(g + 1) * 512, blk * TB:(blk + 1) * TB].rearrange("(c p) t -> p c t", p=128)
            k.dma("sp", xb[:, g * 4:(g + 1) * 4, :], src, writes=[xb])

    def store_x(self, xb, blk, dst=None):
        k = self.k
        xT = dst if dst is not None else self.dr["xT"]
        for g in range(4):
            d = xT[g * 512:(g + 1) * 512, blk * TB:(blk + 1) * TB].rearrange("(c p) t -> p c t", p=128)
            k.dma("pool", d, xb[:, g * 4:(g + 1) * 4, :], reads=[xb])

    def rms(self, xb, hT, gcol, ps_ss, sqs, rstd, tmp, onesb, eps_t):
        k = self.k
        for c in range(NC_D):
            sq = sqs[c % len(sqs)]
            k.op("act", [xb], [sq], lambda e, c=c, sq=sq: e.activation(out=sq[:, :], in_=xb[:, c, :], func=AF.Square))
            k.op("pe", [sq, onesb], [ps_ss], lambda e, c=c, sq=sq: e.matmul(
                ps_ss[:, :], lhsT=onesb[:, :], rhs=sq[:, :], start=(c == 0), stop=(c == NC_D - 1)),
                inc=True)
        k.op("act", [ps_ss, eps_t], [tmp], lambda e: e.activation(
            out=tmp[:, :], in_=ps_ss[:, :], func=AF.Sqrt, scale=1.0 / D, bias=eps_t[:, 0:1]))
        k.op("dve", [tmp], [rstd], lambda e: e.reciprocal(out=rstd[:, :], in_=tmp[:, :]))
        for c in range(NC_D):
            k.op("dve", [xb, rstd, gcol], [hT], lambda e, c=c: e.scalar_tensor_tensor(
                out=hT[:, c, :], in0=xb[:, c, :], scalar=gcol[:, c:c + 1], op0=ALU.mult,
                in1=rstd[:, :], op1=ALU.mult))

    def ffn_stage1(self, l, hf, hT, hid, w1b, ps_h, rl, cnt):
        k = self.k
        wbf = self.dr["wbf%d" % l]
        o1 = self.off["w1_%d" % l]
        for jg in range(hf * 16, hf * 16 + 16):
            wt = w1b[cnt[0] % len(w1b)]; cnt[0] += 1
            k.dma("sp", wt[:, :, :], wview(wbf, o1 + jg * 2 * 128, 2048, 2), writes=[wt])
            for jj in range(2):
                j = jg * 2 + jj
                ph = ps_h[j % len(ps_h)]
                for c in range(NC_D):
                    k.op("pe", [wt, hT], [ph], lambda e, c=c, jj=jj, wt=wt, ph=ph: e.matmul(
                        ph[:, :], lhsT=wt[:, jj, c * 128:(c + 1) * 128], rhs=hT[:, c, :],
                        start=(c == 0), stop=(c == NC_D - 1)), inc=(c == NC_D - 1))
                r = rl[j % len(rl)]
                k.op("act", [ph], [r], lambda e, ph=ph, r=r: e.activation(out=r[:, :], in_=ph[:, :], func=AF.Relu))
                k.op("dve", [r], [hid], lambda e, j=j, r=r: e.tensor_tensor(
                    out=hid[:, j - hf * 32, :], in0=r[:, :], in1=r[:, :], op=ALU.mult))

    def ffn_stage2(self, l, hf, crange, xb, hid, w2b, ps_o, cnt):
        k = self.k
        wbf = self.dr["wbf%d" % l]
        o2 = self.off["w2_%d" % l]
        for c in crange:
            po = ps_o[c % len(ps_o)]
            wt = w2b[cnt[0] % len(w2b)]; cnt[0] += 1
            k.dma("sp", wt[:, :, :], wview(wbf, o2 + c * 512 + hf * 256, 4096, 1), writes=[wt])
            for jj in range(32):
                k.op("pe", [wt, hid], [po], lambda e, jj=jj, wt=wt, po=po: e.matmul(
                    po[:, :], lhsT=wt[:, 0, jj * 128:(jj + 1) * 128], rhs=hid[:, jj, :],
                    start=(jj == 0), stop=(jj == 31)), inc=(jj == 31))
            k.op("dve", [po, xb], [xb], lambda e, c=c, po=po: e.tensor_tensor(
                out=xb[:, c, :], in0=xb[:, c, :], in1=po[:, :], op=ALU.add))

    def phase_copyx(self):
        k = self.k
        with k.phase():
            dummy = Tile(None, "cpdummy")
            k.tiles.append(dummy)
            for g in range(8):
                k.dma("sp", self.dr["xT"][g * 256:(g + 1) * 256, :], self.dr["xin"][g * 256:(g + 1) * 256, :], writes=[dummy])

    def phase_cast(self, l, part, bg=False):
        k = self.k
        if not hasattr(self, "cast_tok"):
            self.cast_tok = {}
        with k.phase():
            CH = 1024
            wsrc = self.dr["wflat%d" % l]
            wbf = self.dr["wbf%d" % l]
            split = self.off["w1_%d" % l]
            s1 = self.off["ein_v%d" % l] if l % 2 == 0 else split
            r, r1 = {"a1": (0, s1), "a2": (s1, split), "b": (split, self.wrows[l])}[part]
            sem = k.free_dsems.pop()
            if not bg:
                k.live_dsems.append(sem)
            while r < r1:
                n = min(CH, r1 - r)
                sem.n += 16
                k.engs["pool"].e.dma_start(out=wbf[r:r + n, :], in_=wsrc[r:r + n, :]).then_inc(sem.h, 16)
                r += n
            self.cast_tok[(l, part)] = (sem, sem.n)

    def cast_queue(self, l, part):
        if not hasattr(self, "bg_jobs"):
            self.bg_jobs = []
            self.bg_sems = {}
        split = self.off["w1_%d" % l]
        s1 = self.off["ein_v%d" % l] if l % 2 == 0 else split
        r, r1 = {"a1": (0, s1), "a2": (s1, split), "b": (split, self.wrows[l])}[part]
        while r < r1:
            n = min(1024, r1 - r)
            self.bg_jobs.append((l, part, r, n))
            r += n

    def bg_pump(self, njobs, only=None):
        k = self.k
        if not hasattr(self, "cast_tok"):
            self.cast_tok = {}
        jobs = getattr(self, "bg_jobs", [])
        done = 0
        i = 0
        while i < len(jobs) and done < njobs:
            l, part, r, n = jobs[i]
            if only is not None and (l, part) not in only:
                i += 1
                continue
            jobs.pop(i)
            sem = self.bg_sems.get((l, part))
            if sem is None:
                sem = self.bg_sems[(l, part)] = k.free_dsems.pop()
            sem.n += 16
            k.engs["pool"].e.dma_start(out=self.dr["wbf%d" % l][r:r + n, :],
                                       in_=self.dr["wflat%d" % l][r:r + n, :]).then_inc(sem.h, 16)
            self.cast_tok[(l, part)] = (sem, sem.n)
            done += 1

    def wait_cast(self, l, part="a1"):
        need = [(l, pp) for pp in ("a1", "a2", "b") if pp <= part]
        self.bg_pump(10 ** 6, only=need)
        toks = [t for (ll, pp), t in getattr(self, "cast_tok", {}).items() if ll == l and pp <= part]
        for e in self.k.engs.values():
            self.k._wait(e, toks)

    def consts(self, ph):
        k = self.k
        ones32 = k.sb([128, 128], F32, "ones32")
        k.dma("sp", ones32[:, :], self.dr["c_ones32"], writes=[ones32])
        gains = k.sb([128, 9 * NC_D], F32, "gains")
        k.dma("sp", gains[:, :], self.dr["gains"], writes=[gains])
        eps_t = k.sb([128, 4], F32, "eps")
        k.dma("sp", eps_t[:, :], self.dr["c_eps"], writes=[eps_t])
        self.onesb = k.sb([128, 128], BF16, "onesb16")
        k.op("dve", [], [self.onesb], lambda e: e.memset(self.onesb[:, :], 1.0))
        return ones32, gains, eps_t

    def phase_ffn(self, l):
        self.wait_cast(l, "b")
        k = self.k
        with k.phase() as ph:
            ones32, gains, eps_t = self.consts(ph)
            xbs = [k.sb([128, NC_D, TB], F32, "xb") for _ in range(2)]
            hTs = [k.sb([128, NC_D, TB], BF16, "hT") for _ in range(2)]
            hid = k.sb([128, NJ // 2, TB], BF16, "hid")
            w1b = [k.sb([128, 2, 2048], BF16, "w1") for _ in range(4)]
            w2b = [k.sb([128, 1, 4096], BF16, "w2") for _ in range(4)]
            sqs = [k.sb([128, TB], BF16, "sq") for _ in range(2)]
            rl = [k.sb([128, TB], F32, "rl") for _ in range(2)]
            rstd = k.sb([128, TB], F32, "rstd")
            tmp = k.sb([128, TB], F32, "tmp")
            ps_ss = k.ps("ss")
            ps_h = [k.ps("h") for _ in range(2)]
            ps_o = [k.ps("o") for _ in range(2)]
            g = _GV(gains, (4 + l) * NC_D)
            c1, c2 = [0], [0]
            self.load_x(xbs[0], 0)
            self.rms(xbs[0], hTs[0], g, ps_ss, sqs, rstd, tmp, self.onesb, eps_t)
            for blk in range(self.nblk):
                xb, hT = xbs[blk % 2], hTs[blk % 2]
                nxt = blk + 1 < self.nblk
                if nxt:
                    self.load_x(xbs[(blk + 1) % 2], blk + 1)
                self.ffn_stage1(l, 0, hT, hid, w1b, ps_h, rl, c1)
                self.ffn_stage2(l, 0, range(NC_D), xb, hid, w2b, ps_o, c2)
                self.ffn_stage1(l, 1, hT, hid, w1b, ps_h, rl, c1)
                self.ffn_stage2(l, 1, range(0, 8), xb, hid, w2b, ps_o, c2)
                if nxt:
                    self.rms(xbs[(blk + 1) % 2], hTs[(blk + 1) % 2], g, ps_ss, sqs, rstd, tmp, self.onesb, eps_t)
                self.ffn_stage2(l, 1, range(8, NC_D), xb, hid, w2b, ps_o, c2)
                self.store_x(xb, blk)

    def phase_odd(self, l):
        self.wait_cast(l, "a2")
        k = self.k
        i = l // 2
        wbf = self.dr["wbf%d" % l]
        xT = self.dr["xT"]
        ou, ov, oo = self.off["oin_u%d" % l], self.off["oin_v%d" % l], self.off["oout%d" % l]
        with k.phase() as ph:
            ones32, gains, eps_t = self.consts(ph)
            xb = k.sb([128, NC_D, TB], F32, "xb")
            hTs = [k.sb([128, NC_D, TB], BF16, "hT") for _ in range(2)]
            uT = k.sb([128, NC_D, TB], BF16, "uT")
            vt = k.sb([128, 4, D], F32, "vt")
            vn = k.sb([128, 4, D], BF16, "vn")
            wA = [k.sb([128, 2, 2048], BF16, "wA") for _ in range(2)]
            wv = [k.sb([128, NC_D, 512], BF16, "wv") for _ in range(2)]
            lng = k.sb([128, D], F32, "lng")
            lnb = k.sb([128, D], F32, "lnb")
            wsb = k.sb([128, 8, 128], BF16, "wsb")
            bsb = k.sb([1, 1024], BF16, "bsb")
            onesb = self.onesb
            xr = [k.sb([128, TB], F32, "xr") for _ in range(2)]
            sqs = [k.sb([128, TB], BF16, "sq") for _ in range(2)]
            rstd = k.sb([128, TB], F32, "rstd")
            tmp = k.sb([128, TB], F32, "tmp")
            st = k.sb([128, 8], F32, "st")
            ps_ss = k.ps("ss")
            ps_a = [k.ps("a") for _ in range(2)]
            ps_v = [k.ps("v") for _ in range(2)]
            ps_m = [k.ps("m") for _ in range(2)]
            k.dma("sp", lng[:, :], self.dr["odd_lng"][i], writes=[lng])
            k.dma("sp", lnb[:, :], self.dr["odd_lnb"][i], writes=[lnb])
            k.dma("pool", wsb[:, :, :], self.dr["odd_wsT"][i], writes=[wsb])
            k.dma("pool", bsb[:, :], self.dr["odd_bs"][i], writes=[bsb])
            k.op("dve", [wsb], [wsb], lambda e: e.memset(wsb[64:128, :, 0:64], 0.0))
            g = _GV(gains, l * NC_D)
            na = 0
            nx = 0
            self.load_x(xb, 0)
            self.rms(xb, hTs[0], g, ps_ss, sqs, rstd, tmp, self.onesb, eps_t)
            for blk in range(self.nblk):
                cols = slice(blk * TB, (blk + 1) * TB)
                hT = hTs[blk % 2]
                nxt = blk + 1 < self.nblk
                for nb in range(4):
                    wt = wv[nb % 2]
                    k.dma("sp", wt[:, :, :], wview(wbf, ov + nb * 512, 8192, 1)[:, 0, :].rearrange(
                        "p (c m) -> p c m", c=NC_D), writes=[wt])
                    for tt in range(4):
                        pv = ps_v[(nb * 4 + tt) % 2]
                        for dc in range(NC_D):
                            k.op("pe", [wt, hT], [pv], lambda e, dc=dc, tt=tt, wt=wt, pv=pv: e.matmul(
                                pv[:, :], lhsT=hT[:, dc, tt * 128:(tt + 1) * 128], rhs=wt[:, dc, :],
                                start=(dc == 0), stop=(dc == NC_D - 1)), inc=(dc == NC_D - 1))
                        k.op("act", [pv], [vt], lambda e, nb=nb, tt=tt, pv=pv: e.activation(
                            out=vt[:, tt, nb * 512:(nb + 1) * 512], in_=pv[:, :], func=AF.Gelu))
                if nxt:
                    self.load_x(xb, blk + 1)
                for tt in range(4):
                    k.op("act", [vt], [vn, st], lambda e, tt=tt: e.activation(
                        out=vn[:, tt, :], in_=vt[:, tt, :], func=AF.Identity, accum_out=st[:, 0:1]))
                    k.op("dve", [st], [st], lambda e: e.tensor_scalar(
                        out=st[:, 1:2], in0=st[:, 0:1], scalar1=-1.0 / D, scalar2=None, op0=ALU.mult))
                    k.op("act", [vt, st], [vt], lambda e, tt=tt: e.activation(
                        out=vt[:, tt, :], in_=vt[:, tt, :], func=AF.Identity, bias=st[:, 1:2]))
                    k.op("act", [vt], [vn, st], lambda e, tt=tt: e.activation(
                        out=vn[:, tt, :], in_=vt[:, tt, :], func=AF.Square, accum_out=st[:, 2:3]))
                    k.op("act", [st, eps_t], [st], lambda e: e.activation(
                        out=st[:, 3:4], in_=st[:, 2:3], func=AF.Sqrt, scale=1.0 / D, bias=eps_t[:, 1:2]))
                    k.op("dve", [st], [st], lambda e: e.reciprocal(out=st[:, 4:5], in_=st[:, 3:4]))
                    k.op("dve", [vt, st, lng], [vt], lambda e, tt=tt: e.scalar_tensor_tensor(
                        out=vt[:, tt, :], in0=vt[:, tt, :], scalar=st[:, 4:5], op0=ALU.mult,
                        in1=lng[:, :], op1=ALU.mult))
                    k.op("dve", [vt, lnb], [vn], lambda e, tt=tt: e.tensor_tensor(
                        out=vn[:, tt, :], in0=vt[:, tt, :], in1=lnb[:, :], op=ALU.add))
                for cp in range(8):
                    wt = wA[na % 2]; na += 1
                    k.dma("sp", wt[:, :, :], wview(wbf, ou + cp * 256, 2048, 2), writes=[wt])
                    for jj in range(2):
                        c = cp * 2 + jj
                        pa = ps_a[c % 2]
                        for dc in range(NC_D):
                            k.op("pe", [wt, hT], [pa], lambda e, dc=dc, jj=jj, wt=wt, pa=pa: e.matmul(
                                pa[:, :], lhsT=wt[:, jj, dc * 128:(dc + 1) * 128], rhs=hT[:, dc, :],
                                start=(dc == 0), stop=(dc == NC_D - 1)), inc=(dc == NC_D - 1))
                        k.op("act", [pa], [uT], lambda e, c=c, pa=pa: e.activation(
                            out=uT[:, c, :], in_=pa[:, :], func=AF.Gelu))
                if nxt:
                    self.rms(xb, hTs[(blk + 1) % 2], g, ps_ss, sqs, rstd, tmp, self.onesb, eps_t)
                for c in range(NC_D):
                    gI = c // 2
                    pm = ps_m[c % 2]
                    for tt in range(4):
                        k.op("pe", [vn, wsb], [pm], lambda e, c=c, tt=tt, gI=gI, pm=pm: e.matmul(
                            pm[:, tt * 128:(tt + 1) * 128], lhsT=vn[:, tt, c * 128:(c + 1) * 128],
                            rhs=wsb[:, gI, :], start=True, stop=False), inc=False)
                        k.op("pe", [onesb, bsb], [pm], lambda e, tt=tt, gI=gI, pm=pm: e.matmul(
                            pm[:, tt * 128:(tt + 1) * 128], lhsT=onesb[0:1, :],
                            rhs=bsb[0:1, gI * 128:(gI + 1) * 128], start=False, stop=True), inc=(tt == 3))
                    k.op("dve", [pm, uT], [uT], lambda e, c=c, pm=pm: e.tensor_tensor(
                        out=uT[:, c, :], in0=pm[:, :], in1=uT[:, c, :], op=ALU.mult))
                for cp in range(8):
                    wt = wA[na % 2]; na += 1
                    k.dma("sp", wt[:, :, :], wview(wbf, oo + cp * 256, 2048, 2), writes=[wt])
                    for jj in range(2):
                        c = cp * 2 + jj
                        pa = ps_a[c % 2]
                        xc = xr[nx % 2]; nx += 1
                        k.dma("sp", xc[:, :], xT[c * 128:(c + 1) * 128, cols], writes=[xc])
                        for cc in range(NC_D):
                            k.op("pe", [wt, uT], [pa], lambda e, cc=cc, jj=jj, wt=wt, pa=pa: e.matmul(
                                pa[:, :], lhsT=wt[:, jj, cc * 128:(cc + 1) * 128], rhs=uT[:, cc, :],
                                start=(cc == 0), stop=(cc == NC_D - 1)), inc=(cc == NC_D - 1))
                        k.op("dve", [pa, xc], [xc], lambda e, xc=xc, pa=pa: e.tensor_tensor(
                            out=xc[:, :], in0=xc[:, :], in1=pa[:, :], op=ALU.add))
                        k.dma("pool", xT[c * 128:(c + 1) * 128, cols], xc[:, :], reads=[xc])

    def phase_e1(self, l):
        self.wait_cast(l)
        k = self.k
        wbf = self.dr["wbf%d" % l]
        oq, ov, orw = self.off["ein_qk%d" % l], self.off["ein_v%d" % l], self.off["ein_rw%d" % l]
        qkT, vtok, zrwT = self.dr["qkT"], self.dr["vtok"], self.dr["zrwT"]
        with k.phase() as ph:
            ones32, gains, eps_t = self.consts(ph)
            xbs = [k.sb([128, NC_D, TB], F32, "xb") for _ in range(2)]
            hTs = [k.sb([128, NC_D, TB], BF16, "hT") for _ in range(2)]
            wA = [k.sb([128, 2, 2048], BF16, "wA") for _ in range(5)]
            wv = [k.sb([128, NC_D, 512], BF16, "wv") for _ in range(2)]
            sqk = [k.sb([128, 2, TB], BF16, "sqk") for _ in range(3)]
            sv = [k.sb([128, 4, 512], BF16, "sv") for _ in range(2)]
            srw = [k.sb([128, 2, TB], F32, "srw") for _ in range(3)]
            sqs = [k.sb([128, TB], BF16, "sq") for _ in range(2)]
            rstd = k.sb([128, TB], F32, "rstd")
            tmp = k.sb([128, TB], F32, "tmp")
            ps_ss = k.ps("ss")
            ps_a = [k.ps("a") for _ in range(3)]
            ps_v = [k.ps("v") for _ in range(2)]
            g = _GV(gains, l * NC_D)
            na = 0
            ncp = 0
            xsrc = self.dr["xin"] if l == 0 else None
            self.load_x(xbs[0], 0, src=xsrc)
            self.rms(xbs[0], hTs[0], g, ps_ss, sqs, rstd, tmp, self.onesb, eps_t)
            for blk in range(self.nblk):
                cols = slice(blk * TB, (blk + 1) * TB)
                xb, hT = xbs[blk % 2], hTs[blk % 2]
                nxt = blk + 1 < self.nblk
                if nxt:
                    self.load_x(xbs[(blk + 1) % 2], blk + 1, src=xsrc)
                jobs = [("qk", cp) for cp in range(8)] + [("rw", cp) for cp in range(14)]
                for kind, cp in jobs:
                    if blk == 0 and kind == "rw" and cp == 0:
                        self.wait_cast(l, "a2")
                    wt = wA[na % 5]; na += 1
                    n = 2 if not (kind == "rw" and cp == 13) else 1
                    base = (oq if kind == "qk" else orw) + cp * 256
                    k.dma("sp", wt[:, 0:n, :], wview(wbf, base, 2048, n), writes=[wt])
                    stg = (sqk if kind == "qk" else srw)[ncp % 3]
                    for jj in range(n):
                        pa = ps_a[ncp % 3]
                        for dc in range(NC_D):
                            k.op("pe", [wt, hT], [pa], lambda e, dc=dc, jj=jj, wt=wt, pa=pa: e.matmul(
                                pa[:, :], lhsT=wt[:, jj, dc * 128:(dc + 1) * 128], rhs=hT[:, dc, :],
                                start=(dc == 0), stop=(dc == NC_D - 1)), inc=(dc == NC_D - 1))
                        if (cp + jj) % 2 == 0:
                            k.op("act", [pa], [stg], lambda e, jj=jj, pa=pa, stg=stg: e.activation(
                                out=stg[:, jj, :], in_=pa[:, :], func=AF.Copy))
                        else:
                            k.op("dve", [pa], [stg], lambda e, jj=jj, pa=pa, stg=stg: e.tensor_copy(
                                out=stg[:, jj, :], in_=pa[:, :]))
                        ncp += 1 if jj == n - 1 else 0
                        if jj < n - 1:
                            ncp += 0
                    dst = (qkT if kind == "qk" else zrwT)[cp * 256:cp * 256 + n * 128, cols].rearrange(
                        "(j p) t -> p j t", p=128)
                    k.dma("pool", dst, stg[:, 0:n, :], reads=[stg])
                if nxt:
                    self.rms(xbs[(blk + 1) % 2], hTs[(blk + 1) % 2], g, ps_ss, sqs, rstd, tmp, self.onesb, eps_t)
                for nb in range(2):
                    wt = wv[nb % 2]
                    k.dma("sp", wt[:, :, :], wview(wbf, ov + nb * 512, 8192, 1)[:, 0, :].rearrange(
                        "p (c m) -> p c m", c=NC_D), writes=[wt])
                    stg = sv[nb % 2]
                    for tt in range(4):
                        pv = ps_v[tt % 2]
                        for dc in range(NC_D):
                            k.op("pe", [wt, hT], [pv], lambda e, dc=dc, tt=tt, wt=wt, pv=pv: e.matmul(
                                pv[:, :], lhsT=hT[:, dc, tt * 128:(tt + 1) * 128], rhs=wt[:, dc, :],
                                start=(dc == 0), stop=(dc == NC_D - 1)), inc=(dc == NC_D - 1))
                        k.op("act", [pv], [stg], lambda e, tt=tt, pv=pv, stg=stg: e.activation(
                            out=stg[:, tt, :], in_=pv[:, :], func=AF.Copy))
                    dst = vtok[cols, nb * 512:(nb + 1) * 512].rearrange("(t p) c -> p t c", p=128)
                    k.dma("pool", dst, stg[:, :, :], reads=[stg])

    def phase_sb(self, l):
        k = self.k
        qkT, vtok, oT = self.dr["qkT"], self.dr["vtok"], self.dr["oT"]
        nseq = max(1, self.nblk * TB // SEQ)
        L = min(SEQ, self.nblk * TB)
        nqg = L // 512
        with k.phase() as ph:
            c32 = k.sb([128, 6, 512], F32, "c32")
            k.dma("sp", c32[:, :, :], self.dr["c_sb"], writes=[c32])
            cb = k.sb([128, 6, 512], BF16, "cb")
            k.op("dve", [c32], [cb], lambda e: e.tensor_copy(out=cb[:, :, :], in_=c32[:, :, :]))
            one_t = k.sb([128, 1], F32, "one")
            k.op("dve", [], [one_t], lambda e: e.memset(one_t[:, :], 1.0))
            qs = [k.sb([64, SEQ], BF16, "q") for _ in range(2)]
            ks = [k.sb([64, SEQ], BF16, "k") for _ in range(2)]
            nks = [None, None]
            xcs = [k.sb([128, 512], F32, "xc") for _ in range(2)]
            vs = [k.sb([128, 16, 64], BF16, "v") for _ in range(2)]
            ohs = [k.sb([64, SEQ], BF16, "oh") for _ in range(2)]
            e32 = [k.sb([128, 512], F32, "e32") for _ in range(2)]
            sps = [k.sb([128, 512], BF16, "sp") for _ in range(2)]
            A32 = k.sb([128, 512], F32, "A32")
            Abf = [k.sb([128, 512], BF16, "Abf") for _ in range(2)]
            ws = [k.sb([128, 512], BF16, "w") for _ in range(3)]
            ps_z = [k.ps("z") for _ in range(2)]
            ps_c = [k.ps("c") for _ in range(2)]
            ps_o = [k.ps("o") for _ in range(2)]
            Abf3 = Abf + [k.sb([128, 512], BF16, "Abf")]
            its = []
            hi = 0
            for b in range(nseq):
                for h in range(16):
                    for qg in range(nqg):
                        kbs = list(range(4 * qg + 3, -1, -1))
                        for idx, kb in enumerate(kbs):
                            its.append(dict(b=b, h=h, hi=hi, qg=qg, idx=idx, kb=kb, nkb=len(kbs),
                                            gq=hi * nqg + qg))
                    hi += 1

            def head_tiles(hi_):
                return qs[hi_ % 2], ks[hi_ % 2], nks[hi_ % 2], vs[hi_ % 2], ohs[hi_ % 2]

            def head_load(itd):
                self.bg_pump(2)
                b, h = itd["b"], itd["h"]
                q, kk, nk, v, oh = head_tiles(itd["hi"])
                tc_ = slice(b * SEQ, b * SEQ + L)
                k.dma("sp", q[:, 0:L], qkT[h * 64:(h + 1) * 64, tc_], writes=[q])
                k.dma("sp", kk[:, 0:L], qkT[1024 + h * 64:1024 + (h + 1) * 64, tc_], writes=[kk])
                k.dma("sp", v[:, 0:L // 128, :], vtok[tc_, h * 64:(h + 1) * 64].rearrange(
                    "(s p) c -> p s c", p=128), writes=[v])

            def stage1(n, itd):
                if itd["qg"] == 0 and itd["idx"] == 0:
                    head_load(itd)
                q, kk, nk, v, oh = head_tiles(itd["hi"])
                qg, kb, idx = itd["qg"], itd["kb"], itd["idx"]
                i = kb - 4 * qg
                qsl = slice(qg * 512, (qg + 1) * 512)
                ksl = slice(kb * 128, (kb + 1) * 128)
                pz, e_, sp = ps_z[n % 2], e32[n % 2], sps[n % 2]
                k.op("pe", [kk, q], [pz], lambda e: e.matmul(
                    pz[:, :], lhsT=kk[:, ksl], rhs=q[:, qsl], start=True, stop=True))
                k.op("act", [pz], [e_], lambda e: e.activation(out=e_[:, :], in_=pz[:, :], func=AF.Exp, scale=0.125))
                k.op("act", [e_, one_t], [sp], lambda e: e.activation(
                    out=sp[:, :], in_=e_[:, :], func=AF.Ln, bias=one_t[:, 0:1]))
                if i >= 0:
                    k.op("dve", [sp, cb], [sp], lambda e: e.tensor_tensor(
                        out=sp[:, :], in0=sp[:, :], in1=cb[:, 2 + i, :], op=ALU.mult))
                if idx < itd["nkb"] - 1:
                    abn = Abf3[(n + 1) % 3]
                    if idx == 0:
                        k.op("dve", [sp], [abn], lambda e: e.tensor_copy(out=abn[:, :], in_=sp[:, :]))
                    else:
                        abc = Abf3[n % 3]
                        k.op("dve", [sp, abc], [abn], lambda e: e.tensor_tensor(
                            out=abn[:, :], in0=abc[:, :], in1=sp[:, :], op=ALU.add))

            def stage2(n, itd):
                q, kk, nk, v, oh = head_tiles(itd["hi"])
                qg, kb, idx = itd["qg"], itd["kb"], itd["idx"]
                i = kb - 4 * qg
                qsl = slice(qg * 512, (qg + 1) * 512)
                ksl = slice(kb * 128, (kb + 1) * 128)
                pc, sp, w = ps_c[n % 2], sps[n % 2], ws[n % 3]
                first = idx == 0
                last = idx == itd["nkb"] - 1
                ab = Abf3[n % 3]
                k.op("pe", [cb, sp], [pc], lambda e: e.matmul(
                    pc[:, :], lhsT=cb[:, 0, 0:128], rhs=sp[:, :], start=True, stop=first), inc=first)
                if not first:
                    k.op("pe", [cb, ab], [pc], lambda e: e.matmul(
                        pc[:, :], lhsT=cb[:, 1, 0:128], rhs=ab[:, :], start=False, stop=True))
                xc_, e_ = xcs[n % 2], e32[n % 2]
                k.op("act", [pc], [xc_], lambda e: e.activation(out=xc_[:, :], in_=pc[:, :], func=AF.Exp, scale=-1.0))
                k.op("dve", [xc_, e_], [w], lambda e: e.tensor_tensor(out=w[:, :], in0=xc_[:, :], in1=e_[:, :], op=ALU.mult))
                if i >= 0:
                    k.op("dve", [w, cb], [w], lambda e: e.tensor_tensor(
                        out=w[:, :], in0=w[:, :], in1=cb[:, 2 + i, :], op=ALU.mult))

            def stage3(n, itd):
                q, kk, nk, v, oh = head_tiles(itd["hi"])
                qg, kb, idx = itd["qg"], itd["kb"], itd["idx"]
                qsl = slice(qg * 512, (qg + 1) * 512)
                w = ws[n % 3]
                po = ps_o[itd["gq"] % 2]
                first = idx == 0
                last = idx == itd["nkb"] - 1
                k.op("pe", [v, w], [po], lambda e: e.matmul(
                    po[0:64, :], lhsT=v[:, kb, :], rhs=w[:, :], start=first, stop=last), inc=last)
                if last:
                    k.op("dve", [po], [oh], lambda e: e.tensor_copy(out=oh[:, qsl], in_=po[0:64, :]))
                    if qg == nqg - 1:
                        b, h = itd["b"], itd["h"]
                        tc_ = slice(b * SEQ, b * SEQ + L)
                        k.dma("pool", oT[h * 64:(h + 1) * 64, tc_], oh[:, 0:L], reads=[oh])

            stage1(0, its[0])
            N_ = len(its)
            for n in range(N_ + 1):
                if n + 1 < N_:
                    stage1(n + 1, its[n + 1])
                if n < N_:
                    stage2(n, its[n])
                if n >= 1:
                    stage3(n - 1, its[n - 1])

    def phase_rw(self, l):
        k = self.k
        i = l // 2
        zrwT, oT = self.dr["zrwT"], self.dr["oT"]
        nseq = max(1, self.nblk * TB // SEQ)
        L = min(SEQ, self.nblk * TB)
        nq = L // 512
        NP = 12
        HH = (slice(0, 64), slice(64, 128))
        with k.phase() as ph:
            ones32, gains, eps_t = self.consts(ph)
            crw = k.sb([128, 5, 512], F32, "crw")
            k.dma("sp", crw[:, :, :], self.dr["c_rw"], writes=[crw])
            bd = k.sb([128, 128], F32, "bd")
            k.dma("sp", bd[:, :], self.dr["c_bd"], writes=[bd])
            par = k.sb([128, 8, NP], F32, "par")
            k.dma("sp", par[:, :, :], self.dr["rw_par"][i], writes=[par])
            k.op("dve", [par], [par], lambda e: e.tensor_scalar(
                out=par[:, :, 10:11], in0=par[:, :, 6:7], scalar1=-1.0, scalar2=1.0, op0=ALU.mult, op1=ALU.add))
            mux = k.sb([128, 4], F32, "mux")
            k.dma("sp", mux[:, :], self.dr["rw_mux"][i], writes=[mux])
            wup = k.sb([64, 1024], F32, "wup")
            aup = k.sb([64, 1024], F32, "aup")
            g0 = k.sb([128, 1024], F32, "g0")
            g1 = k.sb([32, 1024], F32, "g1")
            k.dma("sp", wup[:, :], self.dr["rw_wup"][i], writes=[wup])
            k.dma("sp", aup[:, :], self.dr["rw_aup"][i], writes=[aup])
            k.dma("sp", g0[:, :], self.dr["rw_gup"][i, 0:128, :], writes=[g0])
            k.dma("sp", g1[:, :], self.dr["rw_gup"][i, 128:160, :], writes=[g1])
            Sall = k.sb([128, 16, 64], F32, "Sall")
            k.op("dve", [], [Sall], lambda e: e.memset(Sall[:, :, :], 0.0))
            raws = [k.sb([128, 513], F32, "raw") for _ in range(4)]
            nraw = [0]
            SU, SL, IU, IDT, RST = 0, 1, 2, 3, 4
            tmpA = k.sb([128, 512], F32, "tmpA")

            def load_shift(dst, rows, row0, mucol, b, q, reads_mu):
                raw = raws[nraw[0] % 4]; nraw[0] += 1
                t0 = b * SEQ + q * 512
                if q == 0:
                    k.op("dve", [], [raw], lambda e: e.memset(raw[0:rows, 0:1], 0.0))
                    k.dma("sp", raw[0:rows, 1:513], zrwT[row0:row0 + rows, t0:t0 + 512], writes=[raw])
                else:
                    k.dma("sp", raw[0:rows, 0:513], zrwT[row0:row0 + rows, t0 - 1:t0 + 512], writes=[raw])
                T = tmpA
                k.op("dve", [raw], [T], lambda e: e.tensor_tensor(
                    out=T[0:rows, :], in0=raw[0:rows, 0:512], in1=raw[0:rows, 1:513], op=ALU.subtract))
                k.op("dve", [T, raw, reads_mu], [dst], lambda e: e.scalar_tensor_tensor(
                    out=dst[0:rows, 0:512], in0=T[0:rows, :], scalar=mucol(rows), op0=ALU.mult,
                    in1=raw[0:rows, 1:513], op1=ALU.add))

            sh = []
            for b in range(2):
                sh.append(dict(txw=k.sb([64, 512], F32, "txw"), xas=k.sb([64, 512], F32, "xas"),
                               sg0=k.sb([128, 512], F32, "sg0"), sg1=k.sb([32, 512], F32, "sg1")))
            names32 = ["rs", "ks", "vs", "sig", "cum", "gi", "ginv", "ge", "a", "kap", "kmod",
                       "kt", "rt", "bv", "T1", "T2"]
            names16 = ["kt16", "rt16", "vs16", "bt", "ktl",
                       "Nm", "NTm", "Mkk", "Mbr", "Mkr", "X0", "X1", "P0", "P1", "PT0", "PT1",
                       "Btm", "Ktm", "Vtm"]
            id16 = k.sb([128, 64], BF16, "id16")
            k.op("dve", [crw], [id16], lambda e: e.tensor_copy(out=id16[:, :], in_=crw[:, 3, 0:64]))
            ch = []
            for b in range(2):
                C = {}
                for nm in names32:
                    C[nm] = k.sb([128, 512], F32, nm)
                for nm in names16:
                    C[nm] = k.sb([128, 512], BF16, nm)
                C["Yq"] = C["kmod"]
                C["nP"] = [k.sb([128, 64], BF16, "nP") for _ in range(2)]
                C["UT"] = [k.sb([128, 64], BF16, "UT") for _ in range(2)]
                C["Sg"] = [k.sb([128, 64], F32, "Sg") for _ in range(2)]
                C["obf"] = [k.sb([128, 512], BF16, "obf") for _ in range(2)]
                C["pA"] = k.ps("pA")
                C["pB"] = k.ps("pB")
                C["pS"] = k.ps("pS")
                C["pY"] = k.ps("pY")
                C["no"] = 0
                ch.append(C)

            def mm(ps, out_ap, lhsT, rhs, reads, start, stop, inc):
                k.op("pe", reads, [ps], lambda e: e.matmul(out_ap, lhsT=lhsT, rhs=rhs, start=start, stop=stop), inc=inc)

            def dve(reads, writes, fn):
                k.op("dve", reads, writes, fn)

            def pool_(reads, writes, fn):
                k.op("pool", reads, writes, fn)

            def act(reads, writes, fn):
                k.op("act", reads, writes, fn)

            def unit_prep(q, hp, b, C, S):
                hc = slice(hp * 128, (hp + 1) * 128)
                pA, pB = C["pA"], C["pB"]
                rs, ks, vs = C["rs"], C["ks"], C["vs"]
                T1, T2 = C["T1"], C["T2"]
                load_shift(rs, 128, hp * 128, lambda r: par[0:r, hp, 0:1], b, q, par)
                load_shift(ks, 128, 1024 + hp * 128, lambda r: par[0:r, hp, 1:2], b, q, par)
                load_shift(vs, 128, 2048 + hp * 128, lambda r: par[0:r, hp, 2:3], b, q, par)
                yield
                sig, cum, gi, ginv, ge, a = C["sig"], C["cum"], C["gi"], C["ginv"], C["ge"], C["a"]
                kap, kmod, kt, bt, ktl, rt, bv = C["kap"], C["kmod"], C["kt"], C["bt"], C["ktl"], C["rt"], C["bv"]
                mm(pA, pA[:, :], wup[:, hc], S["txw"][:, :], [wup, S["txw"]], True, True, True)
                act([pA, par], [sig], lambda e: e.activation(out=sig[:, :], in_=pA[:, :], func=AF.Sigmoid,
                                                             bias=par[:, hp, 3:4]))
                dve([sig, crw], [cum], lambda e: e.tensor_tensor_scan(
                    out=cum[:, :], data0=crw[:, RST, :], data1=sig[:, :], initial=0.0, op0=ALU.mult, op1=ALU.add))
                act([cum], [gi], lambda e: e.activation(out=gi[:, :], in_=cum[:, :], func=AF.Exp, scale=-C0))
                act([cum], [ginv], lambda e: e.activation(out=ginv[:, :], in_=cum[:, :], func=AF.Exp, scale=C0))
                dve([cum, sig], [T1], lambda e: e.tensor_tensor(out=T1[:, :], in0=cum[:, :], in1=sig[:, :], op=ALU.subtract))
                act([T1], [ge], lambda e: e.activation(out=ge[:, :], in_=T1[:, :], func=AF.Exp, scale=-C0))
                yield
                mm(pB, pB[:, :], aup[:, hc], S["xas"][:, :], [aup, S["xas"]], True, True, True)
                act([pB, par], [a], lambda e: e.activation(out=a[:, :], in_=pB[:, :], func=AF.Sigmoid,
                                                           bias=par[:, hp, 4:5]))
                yield
                dve([ks, par], [T1], lambda e: e.tensor_scalar(out=T1[:, :], in0=ks[:, :], scalar1=par[:, hp, 5:6],
                                                               scalar2=None, op0=ALU.mult))
                dve([T1], [T2], lambda e: e.tensor_tensor(out=T2[:, :], in0=T1[:, :], in1=T1[:, :], op=ALU.mult))
                mm(pA, pA[:, :], bd[:, :], T2[:, :], [bd, T2], True, True, True)
                act([pA], [T2], lambda e: e.activation(out=T2[:, :], in_=pA[:, :], func=AF.Sqrt))
                dve([T2], [T2], lambda e: e.tensor_scalar_max(out=T2[:, :], in0=T2[:, :], scalar1=1e-12))
                dve([T2], [T2], lambda e: e.reciprocal(out=T2[:, :], in_=T2[:, :]))
                dve([T1, T2], [kap], lambda e: e.tensor_tensor(out=kap[:, :], in0=T1[:, :], in1=T2[:, :], op=ALU.mult))
                yield
                dve([a, par], [T1], lambda e: e.tensor_scalar(out=T1[:, :], in0=a[:, :], scalar1=par[:, hp, 6:7],
                                                              scalar2=par[:, hp, 10:11], op0=ALU.mult, op1=ALU.add))
                dve([ks, T1], [kmod], lambda e: e.tensor_tensor(out=kmod[:, :], in0=ks[:, :], in1=T1[:, :], op=ALU.mult))
                pool_([kap, ge], [kt], lambda e: e.tensor_tensor(out=kt[:, :], in0=kap[:, :], in1=ge[:, :], op=ALU.mult))
                kt16, rt16, vs16 = C["kt16"], C["rt16"], C["vs16"]
                k.op("pool", [kt], [kt16], lambda e: e.tensor_copy(out=kt16[:, :], in_=kt[:, :]))
                k.op("pool", [vs], [vs16], lambda e: e.tensor_copy(out=vs16[:, :], in_=vs[:, :]))
                pool_([kap, a], [T1], lambda e: e.tensor_tensor(out=T1[:, :], in0=kap[:, :], in1=a[:, :], op=ALU.mult))
                dve([T1, ginv], [bt], lambda e: e.tensor_tensor(out=bt[:, :], in0=T1[:, :], in1=ginv[:, :], op=ALU.mult))
                pool_([kmod, ginv], [ktl], lambda e: e.tensor_tensor(out=ktl[:, :], in0=kmod[:, :], in1=ginv[:, :], op=ALU.mult))
                pool_([rs, gi], [rt], lambda e: e.tensor_tensor(out=rt[:, :], in0=rs[:, :], in1=gi[:, :], op=ALU.mult))
                k.op("pool", [rt], [rt16], lambda e: e.tensor_copy(out=rt16[:, :], in_=rt[:, :]))
                yield
                dve([rs, kmod, par], [T1], lambda e: e.scalar_tensor_tensor(
                    out=T1[:, :], in0=rs[:, :], scalar=par[:, hp, 7:8], op0=ALU.mult, in1=kmod[:, :], op1=ALU.mult))
                mm(pB, pB[:, :], bd[:, :], T1[:, :], [bd, T1], True, True, True)
                dve([pB, vs], [bv], lambda e: e.tensor_tensor(out=bv[:, :], in0=pB[:, :], in1=vs[:, :], op=ALU.mult))
                yield
                specs = [("Nm", bt, kt16, SU, pA), ("NTm", kt16, bt, SL, pB), ("Mkk", ktl, kt16, SU, pA),
                         ("Mbr", bt, rt16, IU, pB), ("Mkr", ktl, rt16, IU, pA)]
                for nm, Lt, Rt, mk, ps in specs:
                    dst = C[nm]
                    for c in range(8):
                        cs = slice(c * 64, (c + 1) * 64)
                        for hh, pr in enumerate(HH):
                            mm(ps, ps[pr, cs], Lt[pr, cs], Rt[pr, cs], [Lt, Rt], True, True, c == 7 and hh == 1)
                    dve([ps, crw], [dst], lambda e, ps=ps, dst=dst, mk=mk: e.tensor_tensor(
                        out=dst[:, :], in0=ps[:, :], in1=crw[:, mk, :], op=ALU.mult))
                    yield
                X = C["X0"]
                dve([crw, C["Nm"]], [X], lambda e: e.tensor_tensor(out=X[:, :], in0=crw[:, IDT, :], in1=C["Nm"][:, :],
                                                                   op=ALU.subtract))
                P, PT = C["Nm"], C["NTm"]
                Pb = [C["P0"], C["P1"]]
                PTb = [C["PT0"], C["PT1"]]
                Xb = [C["X1"], C["X0"]]
                for j in range(1, 6):
                    Pn, PTn, Xn = Pb[j % 2], PTb[j % 2], Xb[(j - 1) % 2]
                    if j < 5:
                        for c in range(8):
                            cs = slice(c * 64, (c + 1) * 64)
                            for hh, pr in enumerate(HH):
                                mm(pA, pA[pr, cs], PT[pr, cs], P[pr, cs], [PT, P], True, True, c == 7 and hh == 1)
                    for c in range(8):
                        cs = slice(c * 64, (c + 1) * 64)
                        for hh, pr in enumerate(HH):
                            mm(pB, pB[pr, cs], P[pr, cs], PT[pr, cs], [PT, P], True, True, c == 7 and hh == 1)
                    if j < 5:
                        act([pA], [Pn], lambda e, Pn=Pn: e.activation(out=Pn[:, :], in_=pA[:, :], func=AF.Copy))
                    act([pB], [PTn], lambda e, PTn=PTn: e.activation(out=PTn[:, :], in_=pB[:, :], func=AF.Copy))
                    yield
                    for c in range(8):
                        cs = slice(c * 64, (c + 1) * 64)
                        for hh, pr in enumerate(HH):
                            mm(pA, pA[pr, cs], PTn[pr, cs], X[pr, cs], [PTn, X], True, True, c == 7 and hh == 1)
                    dve([pA, X], [Xn], lambda e, X=X, Xn=Xn: e.tensor_tensor(
                        out=Xn[:, :], in0=pA[:, :], in1=X[:, :], op=ALU.add))
                    P, PT, X = Pn, PTn, Xn
                    yield
                C["X"] = X
                for src, dn, ps in ((bt, "Btm", pA), (ktl, "Ktm", pB), (vs16, "Vtm", pA)):
                    dst = C[dn]
                    for c in range(8):
                        cs = slice(c * 64, (c + 1) * 64)
                        for hh, pr in enumerate(HH):
                            mm(ps, ps[pr, cs], src[pr, cs], id16[pr, :], [src, id16], True, True,
                               c == 7 and hh == 1)
                    if dn != "Vtm":
                        act([ps], [dst], lambda e, ps=ps, dst=dst: e.activation(out=dst[:, :], in_=ps[:, :], func=AF.Copy))
                    else:
                        dve([ps], [dst], lambda e, ps=ps, dst=dst: e.tensor_copy(out=dst[:, :], in_=ps[:, :]))
                    yield

            def chunk_a(c, hp, b, C):
                cs = slice(c * 64, (c + 1) * 64)
                pS = C["pS"]
                kt, Mkk, Vtm = C["kt"], C["Mkk"], C["Vtm"]
                si = b * 8 + hp
                nP = C["nP"][c % 2]
                for hh, pr in enumerate(HH):
                    mm(pS, pS[pr, 0:64], kt[pr, cs], Sall[pr, si, :], [kt, Sall], True, False, False)
                    mm(pS, pS[pr, 0:64], Mkk[pr, cs], Vtm[pr, cs], [Mkk, Vtm], False, True, hh == 1)
                act([pS], [nP], lambda e: e.activation(out=nP[:, :], in_=pS[:, 0:64], func=AF.Copy, scale=-1.0))

            def chunk_b(c, hp, b, C):
                cs = slice(c * 64, (c + 1) * 64)
                pS, X = C["pS"], C["X"]
                nP, UT = C["nP"][c % 2], C["UT"][c % 2]
                for hh, pr in enumerate(HH):
                    mm(pS, pS[pr, 64:128], X[pr, cs], nP[pr, :], [X, nP], True, True, hh == 1)
                act([pS], [UT], lambda e: e.activation(out=UT[:, :], in_=pS[:, 64:128], func=AF.Copy))

            def chunk_c(c, hp, b, C):
                cs = slice(c * 64, (c + 1) * 64)
                pS, pY = C["pS"], C["pY"]
                rt, Mbr, Mkr, Btm, Ktm, Vtm, gi = C["rt"], C["Mbr"], C["Mkr"], C["Btm"], C["Ktm"], C["Vtm"], C["gi"]
                si = b * 8 + hp
                UT, Sg = C["UT"][c % 2], C["Sg"][c % 2]
                gcol = gi[:, c * 64 + 63:c * 64 + 64]
                for hh, pr in enumerate(HH):
                    mm(pY, pY[pr, cs], Sall[pr, si, :], rt[pr, cs], [Sall, rt], True, False, False)
                    mm(pY, pY[pr, cs], UT[pr, :], Mbr[pr, cs], [UT, Mbr], False, False, False)
                    mm(pY, pY[pr, cs], Vtm[pr, cs], Mkr[pr, cs], [Vtm, Mkr], False, True, hh == 1)
                for hh, pr in enumerate(HH):
                    mm(pS, pS[pr, 128:192], Btm[pr, cs], UT[pr, :], [Btm, UT], True, False, False)
                    mm(pS, pS[pr, 128:192], Ktm[pr, cs], Vtm[pr, cs], [Ktm, Vtm], False, True, hh == 1)
                dve([Sall, gi], [Sg], lambda e: e.tensor_scalar(out=Sg[:, :], in0=Sall[:, si, :], scalar1=gcol,
                                                                scalar2=None, op0=ALU.mult))
                dve([pS, Sg, gi], [Sall], lambda e: e.scalar_tensor_tensor(
                    out=Sall[:, si, :], in0=pS[:, 128:192], scalar=gcol, op0=ALU.mult, in1=Sg[:, :], op1=ALU.add))

            def unit_post(q, hp, b, C, S):
                hc = slice(hp * 128, (hp + 1) * 128)
                pA, pB, pY = C["pA"], C["pB"], C["pY"]
                Yq, T1, T2, bv = C["Yq"], C["T1"], C["T2"], C["bv"]
                act([pY], [Yq], lambda e: e.activation(out=Yq[:, :], in_=pY[:, :], func=AF.Copy))
                mm(pA, pA[:, :], bd[:, :], Yq[:, :], [bd, Yq], True, True, True)
                dve([pA, Yq], [T1], lambda e: e.scalar_tensor_tensor(
                    out=T1[:, :], in0=pA[:, :], scalar=-1.0 / 64, op0=ALU.mult, in1=Yq[:, :], op1=ALU.add))
                dve([T1], [T2], lambda e: e.tensor_tensor(out=T2[:, :], in0=T1[:, :], in1=T1[:, :], op=ALU.mult))
                mm(pB, pB[:, :], bd[:, :], T2[:, :], [bd, T2], True, True, True)
                act([pB, eps_t], [T2], lambda e: e.activation(out=T2[:, :], in_=pB[:, :], func=AF.Sqrt,
                                                              scale=1.0 / 64, bias=eps_t[:, 2:3]))
                yield
                dve([T2], [T2], lambda e: e.reciprocal(out=T2[:, :], in_=T2[:, :]))
                dve([T1, T2], [T1], lambda e: e.tensor_tensor(out=T1[:, :], in0=T1[:, :], in1=T2[:, :], op=ALU.mult))
                dve([T1, par], [T1], lambda e: e.tensor_scalar(out=T1[:, :], in0=T1[:, :], scalar1=par[:, hp, 8:9],
                                                               scalar2=par[:, hp, 9:10], op0=ALU.mult, op1=ALU.add))
                dve([T1, bv], [T1], lambda e: e.tensor_tensor(out=T1[:, :], in0=T1[:, :], in1=bv[:, :], op=ALU.add))
                mm(pA, pA[:, :], g0[:, hc], S["sg0"][:, :], [g0, S["sg0"]], True, False, False)
                mm(pA, pA[:, :], g1[:, hc], S["sg1"][:, :], [g1, S["sg1"]], False, True, True)
                ob = C["obf"][C["no"] % 2]; C["no"] += 1
                dve([T1, pA], [ob], lambda e: e.tensor_tensor(out=ob[:, :], in0=T1[:, :], in1=pA[:, :], op=ALU.mult))
                t0 = b * SEQ + q * 512
                k.dma("pool", oT[1024 + hp * 128:1024 + (hp + 1) * 128, t0:t0 + 512], ob[:, :], reads=[ob])

            def chain(b):
                C, S = ch[b], sh[b]
                for q in range(nq):
                    load_shift(S["txw"], 64, 3072, lambda r: mux[0:r, 0:1], b, q, mux)
                    act([S["txw"]], [S["txw"]], lambda e: e.activation(out=S["txw"][:, :], in_=S["txw"][:, :], func=AF.Tanh))
                    load_shift(S["xas"], 64, 3136, lambda r: mux[0:r, 1:2], b, q, mux)
                    load_shift(S["sg0"], 128, 3200, lambda r: mux[0:r, 2:3], b, q, mux)
                    act([S["sg0"]], [S["sg0"]], lambda e: e.activation(out=S["sg0"][:, :], in_=S["sg0"][:, :], func=AF.Sigmoid))
                    load_shift(S["sg1"], 32, 3328, lambda r: mux[0:r, 3:4], b, q, mux)
                    act([S["sg1"]], [S["sg1"]], lambda e: e.activation(out=S["sg1"][:, :], in_=S["sg1"][:, :], func=AF.Sigmoid))
                    yield
                    for hp in range(8):
                        if b == 0:
                            self.bg_pump(1)
                        yield from unit_prep(q, hp, b, C, S)
                        for c in range(8):
                            chunk_a(c, hp, b, C)
                            yield
                            chunk_b(c, hp, b, C)
                            yield
                            chunk_c(c, hp, b, C)
                            yield
                        yield from unit_post(q, hp, b, C, S)

            streams = [chain(b) for b in range(nseq)]
            live = [True] * nseq
            for _ in range(30):
                next(streams[0])
            while any(live):
                for b in range(nseq):
                    if live[b]:
                        try:
                            next(streams[b])
                        except StopIteration:
                            live[b] = False

    def phase_eout(self, l):
        self.wait_cast(l, "a2")
        k = self.k
        wbf = self.dr["wbf%d" % l]
        oo = self.off["eout%d" % l]
        oT = self.dr["oT"]
        with k.phase() as ph:
            xbs = [k.sb([128, NC_D, TB], F32, "xb") for _ in range(2)]
            obs = [k.sb([128, NC_D, TB], BF16, "ob") for _ in range(2)]
            wA = [k.sb([128, 2, 2048], BF16, "wA") for _ in range(6)]
            ps_a = [k.ps("a") for _ in range(2)]
            na = 0
            xsrc = self.dr["xin"] if l == 0 else None

            def ld(blk):
                cols = slice(blk * TB, (blk + 1) * TB)
                self.load_x(xbs[blk % 2], blk, src=xsrc)
                for g4 in range(4):
                    k.dma("sp", obs[blk % 2][:, g4 * 4:(g4 + 1) * 4, :], oT[g4 * 512:(g4 + 1) * 512, cols].rearrange(
                        "(c p) t -> p c t", p=128), writes=[obs[blk % 2]])
            ld(0)
            for blk in range(self.nblk):
                xb, ob = xbs[blk % 2], obs[blk % 2]
                if blk + 1 < self.nblk:
                    ld(blk + 1)
                for cp in range(8):
                    wt = wA[na % 6]; na += 1
                    k.dma("sp", wt[:, :, :], wview(wbf, oo + cp * 256, 2048, 2), writes=[wt])
                    for jj in range(2):
                        c = cp * 2 + jj
                        pa = ps_a[c % 2]
                        for cc in range(NC_D):
                            k.op("pe", [wt, ob], [pa], lambda e, cc=cc, jj=jj, wt=wt, pa=pa: e.matmul(
                                pa[:, :], lhsT=wt[:, jj, cc * 128:(cc + 1) * 128], rhs=ob[:, cc, :],
                                start=(cc == 0), stop=(cc == NC_D - 1)), inc=(cc == NC_D - 1))
                        k.op("dve", [pa, xb], [xb], lambda e, c=c, pa=pa: e.tensor_tensor(
                            out=xb[:, c, :], in0=xb[:, c, :], in1=pa[:, :], op=ALU.add))
                self.store_x(xb, blk)

    def phase_zero_orw(self):
        k = self.k
        with k.phase():
            z = k.sb([128, 8, TB], BF16, "z")
            k.op("dve", [], [z], lambda e: e.memset(z[:, :, :], 0.0))
            for blk in range(self.nblk):
                k.dma("sp", self.dr["oT"][1024:2048, blk * TB:(blk + 1) * TB].rearrange(
                    "(c p) t -> p c t", p=128), z[:, :, :], reads=[z])

    def phase_final(self):
        k = self.k
        with k.phase() as ph:
            ones32, gains, eps_t = self.consts(ph)
            xbs = [k.sb([128, NC_D, TB], F32, "xb") for _ in range(2)]
            obs = [k.sb([128, NC_D, TB], F32, "ob") for _ in range(2)]
            sqs = [k.sb([128, TB], BF16, "sq") for _ in range(2)]
            rstd = k.sb([128, TB], F32, "rstd")
            tmp = k.sb([128, TB], F32, "tmp")
            ps_ss = k.ps("ss")
            g = _GV(gains, 8 * NC_D)
            for blk in range(self.nblk):
                xb = xbs[blk % 2]
                ob = obs[blk % 2]
                self.load_x(xb, blk)
                self.rms(xb, ob, g, ps_ss, sqs, rstd, tmp, self.onesb, eps_t)
                self.store_x(ob, blk, dst=self.dr["outT"])


class _GV:
    def __init__(self, base, off):
        self.base = base
        self.off = off
        self.t = base.t

    @property
    def w(self):
        return self.base.w

    @w.setter
    def w(self, v):
        self.base.w = v

    @property
    def r(self):
        return self.base.r

    @r.setter
    def r(self, v):
        self.base.r = v

    def __getitem__(self, idx):
        p, c = idx
        if isinstance(c, slice):
            c = slice(c.start + self.off, c.stop + self.off)
        else:
            c = c + self.off
        return self.t[p, c]


def host_consts():
    c = {}
    c["c_ones32"] = np.ones((128, 128), np.float32)
    sb = np.zeros((128, 6, 512), np.float32)
    jj = np.arange(128)[:, None]
    ss = np.arange(128)[None, :]
    sb[:, 0, :128] = (jj >= ss)
    sb[:, 1, :128] = 1.0
    tq = np.arange(512)[None, :]
    for i in range(4):
        sb[:, 2 + i, :] = (tq > jj + 128 * i)
    c["c_sb"] = sb
    rw = np.zeros((128, 5, 512), np.float32)
    s_ = (np.arange(128) % 64)[:, None]
    t_ = np.arange(64)[None, :]
    rw[:, 0, :] = np.tile((s_ < t_), (1, 8))
    rw[:, 1, :] = np.tile((s_ > t_), (1, 8))
    rw[:, 2, :] = np.tile((s_ <= t_), (1, 8))
    rw[:, 3, :] = np.tile((s_ == t_), (1, 8))
    rw[:, 4, :] = (np.arange(512) % 64 != 0)[None, :]
    c["c_rw"] = rw
    bdm = np.zeros((128, 128), np.float32)
    bdm[:64, :64] = 1.0
    bdm[64:, 64:] = 1.0
    c["c_bd"] = bdm
    c["c_eps"] = np.tile(np.array([[RMS_EPS, LN_EPS, LNX_EPS, 1.0]], np.float32), (128, 1))
    return c


def build(plan, nblk=NBLK, debug_out=()):
    nc = bass.Bass("TRN2", target_bir_lowering=False)
    off, wrows = wpack_layout()
    dr = {}
    dr["xin"] = nc.dram_tensor("xin", [D, NT], F32, kind="ExternalInput").ap()
    for l in range(DEPTH):
        dr["wflat%d" % l] = nc.dram_tensor("wflat%d" % l, [wrows[l], 2048], F32, kind="ExternalInput").ap()
        dr["wbf%d" % l] = nc.dram_tensor("wbf%d" % l, [wrows[l], 2048], BF16, kind="Internal").ap()
    dr["gains"] = nc.dram_tensor("gains", [128, 9 * NC_D], F32, kind="ExternalInput").ap()
    for name, arr in host_consts().items():
        dr[name] = nc.dram_tensor(name, list(arr.shape), F32, kind="ExternalInput").ap()
    dr["odd_lng"] = nc.dram_tensor("odd_lng", [2, 128, D], F32, kind="ExternalInput").ap()
    dr["odd_lnb"] = nc.dram_tensor("odd_lnb", [2, 128, D], F32, kind="ExternalInput").ap()
    dr["odd_wsT"] = nc.dram_tensor("odd_wsT", [2, 128, 8, 128], F32, kind="ExternalInput").ap()
    dr["odd_bs"] = nc.dram_tensor("odd_bs", [2, 1, 1024], F32, kind="ExternalInput").ap()
    def _scr(name, shape, dt):
        return nc.dram_tensor(name, shape, dt, kind=("ExternalOutput" if name in debug_out else "Internal")).ap()
    dr["qkT"] = _scr("qkT", [2048, NT], BF16)
    dr["vtok"] = _scr("vtok", [NT, 1024], BF16)
    dr["zrwT"] = _scr("zrwT", [27 * 128, NT], F32)
    dr["oT"] = _scr("oT", [2048, NT], BF16)
    dr["rw_par"] = nc.dram_tensor("rw_par", [2, 128, 8, 12], F32, kind="ExternalInput").ap()
    dr["rw_mux"] = nc.dram_tensor("rw_mux", [2, 128, 4], F32, kind="ExternalInput").ap()
    dr["rw_wup"] = nc.dram_tensor("rw_wup", [2, 64, 1024], F32, kind="ExternalInput").ap()
    dr["rw_aup"] = nc.dram_tensor("rw_aup", [2, 64, 1024], F32, kind="ExternalInput").ap()
    dr["rw_gup"] = nc.dram_tensor("rw_gup", [2, 160, 1024], F32, kind="ExternalInput").ap()
    dr["outT"] = nc.dram_tensor("outT", [D, NT], F32, kind="ExternalOutput").ap()
    dr["xT"] = dr["xin"] if False else nc.dram_tensor("xT", [D, NT], F32, kind=("ExternalOutput" if "xT" in debug_out else "Internal")).ap()
    k = K(nc)
    p = Prog(nc, k, dr)
    p.off = off
    p.wrows = wrows
    p.nblk = nblk
    for step in plan:
        if step[0] == "castq":
            p.cast_queue(step[1], step[2])
        elif step[0] == "castbg":
            p.phase_cast(step[1], step[2], bg=True)
        elif step[0] == "cast":
            p.phase_cast(step[1], step[2])
        elif step[0] == "copyx":
            p.phase_copyx()
        elif step[0] == "ffn":
            p.phase_ffn(step[1])
        elif step[0] == "odd":
            p.phase_odd(step[1])
        elif step[0] == "e1":
            p.phase_e1(step[1])
        elif step[0] == "sb":
            p.phase_sb(step[1])
        elif step[0] == "eout":
            p.phase_eout(step[1])
        elif step[0] == "zero_orw":
            p.phase_zero_orw()
        elif step[0] == "rw":
            p.phase_rw(step[1])
        elif step[0] == "final":
            p.phase_final()
        else:
            raise ValueError(step)
    return nc


def full_plan():
    def cb(l):
        return [("castq", l, "a1"), ("castq", l, "a2"), ("castq", l, "b")]
    plan = [("cast", 0, "a1"), ("castbg", 0, "a2")]
    plan += [("e1", 0), ("castq", 0, "b")] + cb(1) + [("sb", 0)] + cb(2) + [("rw", 0), ("eout", 0), ("ffn", 0)]
    plan += [("odd", 1), ("ffn", 1)]
    plan += [("e1", 2)] + cb(3) + [("sb", 2), ("rw", 2), ("eout", 2), ("ffn", 2)]
    plan += [("odd", 3), ("ffn", 3)]
    plan += [("final",)]
    return plan


def host_inputs(inp):
    x = np.asarray(inp["x"], np.float32)
    gains = np.concatenate([np.asarray(inp["norm_mix"], np.float32),
                            np.asarray(inp["norm_ffn"], np.float32),
                            np.asarray(inp["norm_final"], np.float32)[None]], axis=0)
    gains = np.ascontiguousarray(gains.reshape(9, NC_D, 128).transpose(2, 0, 1).reshape(128, 9 * NC_D))
    wflat = pack_weights({k_: np.asarray(v, np.float32) for k_, v in inp.items()})
    common = {"gains": gains}
    f = lambda a: np.asarray(a, np.float32)
    par = np.zeros((2, 128, 8, 12), np.float32)
    mu = f(inp["even_mu"])
    def hd(a):
        return a.reshape(2, 8, 128).transpose(0, 2, 1)
    par[:, :, :, 0] = hd(mu[:, 0:1024]); par[:, :, :, 1] = hd(mu[:, 1024:2048]); par[:, :, :, 2] = hd(mu[:, 2048:3072])
    par[:, :, :, 3] = hd(f(inp["even_w0"])); par[:, :, :, 4] = hd(f(inp["even_a0"]))
    par[:, :, :, 5] = hd(f(inp["even_k_k"])); par[:, :, :, 6] = hd(f(inp["even_k_a"]))
    par[:, :, :, 7] = hd(f(inp["even_r_k"]).reshape(2, 1024))
    par[:, :, :, 8] = hd(f(inp["even_lnx_w"])); par[:, :, :, 9] = hd(f(inp["even_lnx_b"]))
    common["rw_par"] = par
    mux = np.zeros((2, 128, 4), np.float32)
    mux[:, :64, 0] = mu[:, 3072:3136]; mux[:, :64, 1] = mu[:, 3136:3200]
    mux[:, :, 2] = mu[:, 3200:3328]; mux[:, :32, 3] = mu[:, 3328:3360]
    common["rw_mux"] = mux
    common["rw_wup"] = f(inp["even_w_up"]); common["rw_aup"] = f(inp["even_a_up"]); common["rw_gup"] = f(inp["even_g_up"])
    common["odd_lng"] = np.ascontiguousarray(np.broadcast_to(f(inp["odd_ln_g"])[:, None, :], (2, 128, D)))
    common["odd_lnb"] = np.ascontiguousarray(np.broadcast_to(f(inp["odd_ln_b"])[:, None, :], (2, 128, D)))
    common["odd_wsT"] = np.ascontiguousarray(f(inp["odd_w_s"]).transpose(0, 3, 1, 2))
    common["odd_bs"] = np.ascontiguousarray(f(inp["odd_b_s"]).reshape(2, 1, 1024))
    for l in range(DEPTH):
        common["wflat%d" % l] = wflat[l]
    common.update(host_consts())
    maps = []
    for c in range(8):
        xs = x[c * NB_CORE:(c + 1) * NB_CORE].reshape(NT, D)
        m = dict(common)
        m["xin"] = np.ascontiguousarray(xs.T)
        maps.append(m)
    return maps


def kernel(**inputs):
    maps = host_inputs(inputs)
    nc = build(full_plan())
    res = run_bass_kernel_spmd(nc, maps, core_ids=list(range(8)))
    out = np.empty((8 * NB_CORE, SEQ, D), np.float32)
    for c in range(8):
        oT = np.asarray(res.results[c]["outT"], np.float32)
        out[c * NB_CORE:(c + 1) * NB_CORE] = oT.T.reshape(NB_CORE, SEQ, D)
    return out
```
